# Optimizing a Trainium2 kernel written in Bass

```python
import math
import jax, jax.numpy as jnp
from jax import lax
import numpy as np

D_MODEL = 1024
BATCH = 32
SEQ = 2048
DEPTH = 4

N_MIXERS = 4
LN_EPS = 1e-5
NEG = -1e30
DEEPNORM_ALPHA = (2 * DEPTH) ** 0.25
DEEPNORM_BETA = (8 * DEPTH) ** -0.25

MEM_LEN = 256
MEM_HEADS = 4
MEM_HEAD_DIM = 64
MEM_WIDTH = MEM_HEADS * MEM_HEAD_DIM

S5_WIDTH = D_MODEL // 2
S5_GROUP = 16
S5_GROUPS = S5_WIDTH // S5_GROUP
S5_STATE = 64

DIL_PATTERNS = ((128, 1), (512, 4), (2048, 16))
DIL_HEADS = 8
DIL_HEAD_DIM = 64
DIL_WIDTH = DIL_HEADS * DIL_HEAD_DIM
DIL_IN = 3 * len(DIL_PATTERNS) * DIL_WIDTH
DIL_BLOCK = 64

WIN_Q_HEADS = 12
WIN_KV_HEADS = 4
WIN_GROUP = WIN_Q_HEADS // WIN_KV_HEADS
WIN_HEAD_DIM = 64
WIN_RADIUS = 128
WIN_BLOCK = 128
WIN_WIDTH = WIN_Q_HEADS * WIN_HEAD_DIM
WIN_IN = (WIN_Q_HEADS + 2 * WIN_KV_HEADS) * WIN_HEAD_DIM

MLSTM_HEADS = 4
MLSTM_WIDTH = 3 * D_MODEL // 4
MLSTM_HEAD_DIM = MLSTM_WIDTH // MLSTM_HEADS
MLSTM_CHUNK = 64
MLSTM_IN = 4 * MLSTM_WIDTH + 4 * MLSTM_HEADS

MOE_GROUPS = 4
MOE_EXPERTS_PER_GROUP = 4
MOE_EXPERTS = MOE_GROUPS * MOE_EXPERTS_PER_GROUP
MOE_HIDDEN = D_MODEL // 4
MOE_TOPK = 2

kernel_name = "hybrid_s5_dilated_window_mlstm_hmoe_encoder"


def _n_uses(mixer):
    return len(range(mixer, DEPTH, N_MIXERS))


def _layer_norm(x, g, b):
    xf = x.astype(jnp.float32)
    mu = jnp.mean(xf, -1, keepdims=True)
    var = jnp.mean(jnp.square(xf - mu), -1, keepdims=True)
    return ((xf - mu) * lax.rsqrt(var + LN_EPS) * g.astype(jnp.float32) + b.astype(jnp.float32)).astype(x.dtype)


def _alibi_slopes(n):
    return 2.0 ** (-8.0 * jnp.arange(1, n + 1, dtype=jnp.float32) / n)


def _ssm_combine(left, right):
    a_l, b_l = left
    a_r, b_r = right
    return a_r * a_l, a_r * b_l + b_r


def _s5_mixer(u, lam_re, lam_im, log_step, b_re, b_im, c_re, c_im, d_skip, w_glu, b_glu):
    bsz, L, _ = u.shape
    f32 = jnp.float32
    uf = u.astype(f32)
    ug = uf.reshape(bsz, L, S5_GROUPS, S5_GROUP).astype(jnp.complex64)
    bmat = lax.complex(b_re.astype(f32), b_im.astype(f32))
    cmat = lax.complex(c_re.astype(f32), c_im.astype(f32))
    y = d_skip.astype(f32) * uf
    for direction in range(2):
        lam = lax.complex(lam_re[direction].astype(f32), lam_im[direction].astype(f32))
        step = jnp.exp(log_step[direction].astype(f32))[:, None]
        lam_bar = jnp.exp(lam * step)
        b_bar = ((lam_bar - 1.0) / lam)[..., None] * bmat
        bu = jnp.einsum('blgc,gpc->blgp', ug, b_bar)
        a = jnp.broadcast_to(lam_bar, (1, L) + lam_bar.shape)
        _, states = lax.associative_scan(_ssm_combine, (a, bu), reverse=(direction == 1), axis=1)
        y = y + jnp.real(jnp.einsum('gcp,blgp->blgc', cmat, states)).reshape(bsz, L, S5_WIDTH)
    g = jax.nn.gelu(y)
    return (g * jax.nn.sigmoid(g @ w_glu.astype(f32) + b_glu.astype(f32))).astype(u.dtype)


def _banded_attention(q, k, v, radius, block, dist_unit, slopes, sink):
    n, L, hk, gq, hd = q.shape
    nb = -(-L // block)
    lp = nb * block
    qb = jnp.pad(q, ((0, 0), (0, lp - L), (0, 0), (0, 0), (0, 0))).reshape(n, nb, block, hk, gq, hd)
    kpad = ((0, 0), (block, lp - L + block), (0, 0), (0, 0))
    kp, vp = jnp.pad(k, kpad), jnp.pad(v, kpad)

    def windows(t):
        return jnp.concatenate([t[:, i * block:i * block + lp].reshape(n, nb, block, hk, hd) for i in range(3)], axis=2)

    kw, vw = windows(kp), windows(vp)
    bi = jnp.arange(nb)[:, None, None]
    qi = jnp.arange(block)[None, :, None]
    kj = jnp.arange(3 * block)[None, None, :]
    rel = kj - block - qi
    kpos = (bi - 1) * block + kj
    valid = (jnp.abs(rel) <= radius) & (kpos >= 0) & (kpos < L)
    dist = (jnp.abs(rel[0]) * dist_unit).astype(jnp.float32)
    s = jnp.einsum('nbqhgd,nbkhd->nbhgqk', qb, kw).astype(jnp.float32) * (hd ** -0.5)
    s = s - slopes[:, :, None, None] * dist
    s = jnp.where(valid[None, :, None, None], s, NEG)
    m = jnp.max(s, -1)
    if sink is not None:
        m = jnp.maximum(m, sink[:, :, None])
    p = jnp.exp(s - m[..., None])
    denom = jnp.sum(p, -1)
    if sink is not None:
        denom = denom + jnp.exp(sink[:, :, None] - m)
    out = jnp.einsum('nbhgqk,nbkhd->nbqhgd', (p / denom[..., None]).astype(v.dtype), vw)
    lse = m + jnp.log(denom)
    out = out.reshape(n, lp, hk, gq, hd)[:, :L]
    lse = jnp.moveaxis(lse, -1, 2).reshape(n, lp, hk, gq)[:, :L]
    return out, lse


def _dilated_mixer(proj):
    bsz, L, _ = proj.shape
    qkv = proj.reshape(bsz, L, 3, len(DIL_PATTERNS), DIL_HEADS, DIL_HEAD_DIM)
    slopes = _alibi_slopes(DIL_HEADS).reshape(DIL_HEADS, 1)
    outs, lses = [], []
    for p, (window, dil) in enumerate(DIL_PATTERNS):
        ls = L // dil

        def strided(t):
            return t.reshape(bsz, ls, dil, DIL_HEADS, DIL_HEAD_DIM).swapaxes(1, 2).reshape(bsz * dil, ls, DIL_HEADS, DIL_HEAD_DIM)

        q = strided(qkv[:, :, 0, p])[:, :, :, None]
        k = strided(qkv[:, :, 1, p])
        v = strided(qkv[:, :, 2, p])
        o, lse = _banded_attention(q, k, v, (window // 2) // dil, DIL_BLOCK, dil, slopes, None)
        outs.append(o.reshape(bsz, dil, ls, DIL_HEADS, DIL_HEAD_DIM).swapaxes(1, 2).reshape(bsz, L, DIL_HEADS, DIL_HEAD_DIM))
        lses.append(lse.reshape(bsz, dil, ls, DIL_HEADS).swapaxes(1, 2).reshape(bsz, L, DIL_HEADS))
    wts = jax.nn.softmax(jnp.stack(lses), axis=0)
    out = jnp.sum(wts[..., None] * jnp.stack(outs).astype(jnp.float32), axis=0)
    return out.reshape(bsz, L, DIL_WIDTH).astype(proj.dtype)


def _window_mixer(proj, sink):
    bsz, L, _ = proj.shape
    nq = WIN_Q_HEADS * WIN_HEAD_DIM
    nkv = WIN_KV_HEADS * WIN_HEAD_DIM
    q = proj[..., :nq].reshape(bsz, L, WIN_KV_HEADS, WIN_GROUP, WIN_HEAD_DIM)
    k = proj[..., nq:nq + nkv].reshape(bsz, L, WIN_KV_HEADS, WIN_HEAD_DIM)
    v = proj[..., nq + nkv:].reshape(bsz, L, WIN_KV_HEADS, WIN_HEAD_DIM)
    slopes = _alibi_slopes(WIN_Q_HEADS).reshape(WIN_KV_HEADS, WIN_GROUP)
    sink_l = sink.astype(jnp.float32).reshape(WIN_KV_HEADS, WIN_GROUP)
    out, _ = _banded_attention(q, k, v, WIN_RADIUS, WIN_BLOCK, 1, slopes, sink_l)
    return out.reshape(bsz, L, WIN_WIDTH).astype(proj.dtype)


def _mlstm_chunkwise(q, k, v, li, lf):
    bsz, nh, L, dh = q.shape
    nc = L // MLSTM_CHUNK

    def to_chunks(t):
        return jnp.moveaxis(t.reshape(bsz, nh, nc, MLSTM_CHUNK, *t.shape[3:]), 2, 0)

    xs = (to_chunks(q), to_chunks(k), to_chunks(v), to_chunks(li), to_chunks(lf))
    tril = jnp.tril(jnp.ones((MLSTM_CHUNK, MLSTM_CHUNK), bool))

    def step(carry, inp):
        c_st, n_st, m_st = carry
        qt, kt, vt, it, ft = inp
        b = jnp.cumsum(ft, axis=-1)
        g = b[..., -1]
        dmat = jnp.where(tril, b[..., :, None] - b[..., None, :] + it[..., None, :], NEG)
        inter = b + m_st[..., None]
        m_t = jnp.maximum(jnp.max(dmat, -1), inter)
        w = jnp.exp(dmat - m_t[..., None])
        w_inter = jnp.exp(inter - m_t)
        qk = jnp.einsum('bhtd,bhsd->bhts', qt, kt) * w
        num = jnp.einsum('bhts,bhsd->bhtd', qk, vt) + w_inter[..., None] * jnp.einsum('bhvk,bhtk->bhtv', c_st, qt)
        den = jnp.sum(qk, -1) + w_inter * jnp.einsum('bhk,bhtk->bht', n_st, qt)
        h = num / jnp.maximum(jnp.abs(den), jnp.exp(-m_t))[..., None]
        decay_s = g[..., None] - b + it
        m_new = jnp.maximum(g + m_st, jnp.max(decay_s, -1))
        ws = jnp.exp(decay_s - m_new[..., None])
        carry_scale = jnp.exp(g + m_st - m_new)
        c_new = carry_scale[..., None, None] * c_st + jnp.einsum('bhs,bhsv,bhsk->bhvk', ws, vt, kt)
        n_new = carry_scale[..., None] * n_st + jnp.einsum('bhs,bhsk->bhk', ws, kt)
        return (c_new, n_new, m_new), h

    init = (jnp.zeros((bsz, nh, dh, dh), jnp.float32), jnp.zeros((bsz, nh, dh), jnp.float32),
            jnp.full((bsz, nh), NEG, jnp.float32))
    _, hs = lax.scan(step, init, xs)
    return jnp.moveaxis(hs, 0, 2).reshape(bsz, nh, L, dh)


def _mlstm_mixer(proj, gate_b, norm_g):
    bsz, L, _ = proj.shape
    w = MLSTM_WIDTH
    f32 = jnp.float32

    def heads(t):
        return t.reshape(bsz, L, MLSTM_HEADS, MLSTM_HEAD_DIM).transpose(0, 2, 1, 3).astype(f32)

    q = heads(proj[..., :w])
    k = heads(proj[..., w:2 * w]) * (MLSTM_HEAD_DIM ** -0.5)
    v = heads(proj[..., 2 * w:3 * w])
    o = proj[..., 3 * w:4 * w].astype(f32)
    gates = proj[..., 4 * w:].astype(f32).reshape(bsz, L, 4, MLSTM_HEADS) + gate_b.astype(f32)
    gates = gates.transpose(2, 0, 3, 1)
    h_fwd = _mlstm_chunkwise(q, k, v, gates[0], jax.nn.log_sigmoid(gates[1]))
    flip = lambda t: jnp.flip(t, axis=2)
    h_bwd = flip(_mlstm_chunkwise(flip(q), flip(k), flip(v), flip(gates[2]), flip(jax.nn.log_sigmoid(gates[3]))))
    h = h_fwd + h_bwd
    mu = jnp.mean(h, -1, keepdims=True)
    var = jnp.mean(jnp.square(h - mu), -1, keepdims=True)
    hn = ((h - mu) * lax.rsqrt(var + LN_EPS)).transpose(0, 2, 1, 3).reshape(bsz, L, w) * norm_g.astype(f32)
    return (hn * jax.nn.sigmoid(o)).astype(proj.dtype)


def _memory_attention(q_mem, mem, w_kv):
    bsz, L, _ = q_mem.shape
    kv = (mem @ w_kv).reshape(bsz, mem.shape[1], 2, MEM_HEADS, MEM_HEAD_DIM)
    q = q_mem.reshape(bsz, L, MEM_HEADS, MEM_HEAD_DIM)
    s = jnp.einsum('blhd,bmhd->bhlm', q, kv[:, :, 0]).astype(jnp.float32) * (MEM_HEAD_DIM ** -0.5)
    p = jax.nn.softmax(s, axis=-1)
    out = jnp.einsum('bhlm,bmhd->blhd', p.astype(q_mem.dtype), kv[:, :, 1])
    return out.reshape(bsz, L, MEM_WIDTH)


def _hier_moe(x, wg, bg, we, be, w_gate, w_up, w_down):
    bsz, L, d = x.shape
    t = x.reshape(-1, d)
    n_tok = t.shape[0]
    f32 = jnp.float32
    gl = (t @ wg).astype(f32) + bg.astype(f32)
    gp = jax.nn.softmax(gl, axis=-1)
    gsel = jnp.argmax(gl, axis=-1)
    gw = jnp.take_along_axis(gp, gsel[:, None], axis=-1)
    el = ((t @ we).astype(f32) + be.astype(f32)).reshape(n_tok, MOE_GROUPS, MOE_EXPERTS_PER_GROUP)
    idx = jnp.broadcast_to(gsel[:, None, None], (n_tok, 1, MOE_EXPERTS_PER_GROUP))
    el = jnp.take_along_axis(el, idx, axis=1)[:, 0]
    top_v, top_i = lax.top_k(el, MOE_TOPK)
    ew = jax.nn.softmax(top_v, axis=-1) * gw
    eid = gsel[:, None] * MOE_EXPERTS_PER_GROUP + top_i
    gate = jnp.sum(jax.nn.one_hot(eid, MOE_EXPERTS, dtype=f32) * ew[..., None], axis=1)
    h = jax.nn.silu(jnp.einsum('td,edf->tef', t, w_gate)) * jnp.einsum('td,edf->tef', t, w_up)
    y = jnp.einsum('tef,efd->td', h * gate[..., None].astype(h.dtype), w_down)
    return y.reshape(bsz, L, d).astype(x.dtype)


def setup_inputs(seed: int = 0) -> dict:
    key = jax.random.key(seed)
    ks = iter(jax.random.split(key, 48))
    f32 = jnp.float32
    d = D_MODEL
    beta = DEEPNORM_BETA

    def nrm(shape, scale):
        return jax.random.normal(next(ks), shape, f32) * scale

    na, nb, nc, nd = (_n_uses(m) for m in range(N_MIXERS))
    x = nrm((BATCH, SEQ, d), 1.0)
    mem = nrm((BATCH, MEM_LEN, d), 1.0)
    s5_w_in = nrm((na, d, S5_WIDTH + MEM_WIDTH), d ** -0.5)
    s5_lam_re = -0.5 + nrm((na, 2, S5_GROUPS, S5_STATE), 0.01)
    s5_lam_im = math.pi * jnp.arange(S5_STATE, dtype=f32) + nrm((na, 2, S5_GROUPS, S5_STATE), 0.01)
    s5_log_step = jax.random.uniform(next(ks), (na, 2, S5_GROUPS), f32, math.log(1e-3), math.log(1e-1))
    s5_b_re = nrm((na, S5_GROUPS, S5_STATE, S5_GROUP), (2 * S5_GROUP) ** -0.5)
    s5_b_im = nrm((na, S5_GROUPS, S5_STATE, S5_GROUP), (2 * S5_GROUP) ** -0.5)
    s5_c_re = nrm((na, S5_GROUPS, S5_GROUP, S5_STATE), 0.5)
    s5_c_im = nrm((na, S5_GROUPS, S5_GROUP, S5_STATE), 0.5)
    s5_d = nrm((na, S5_WIDTH), 1.0)
    s5_w_glu = nrm((na, S5_WIDTH, S5_WIDTH), S5_WIDTH ** -0.5)
    s5_b_glu = nrm((na, S5_WIDTH), 0.01)
    s5_w_out = nrm((na, S5_WIDTH + MEM_WIDTH, d), (S5_WIDTH + MEM_WIDTH) ** -0.5 * beta)
    dil_w_in = nrm((nb, d, DIL_IN + MEM_WIDTH), d ** -0.5)
    dil_w_out = nrm((nb, DIL_WIDTH + MEM_WIDTH, d), (DIL_WIDTH + MEM_WIDTH) ** -0.5 * beta)
    win_w_in = nrm((nc, d, WIN_IN + MEM_WIDTH), d ** -0.5)
    win_sink = nrm((nc, WIN_Q_HEADS), 0.5)
    win_w_out = nrm((nc, WIN_WIDTH + MEM_WIDTH, d), (WIN_WIDTH + MEM_WIDTH) ** -0.5 * beta)
    mlstm_w_in = nrm((nd, d, MLSTM_IN + MEM_WIDTH), d ** -0.5)
    i_bias = nrm((nd, 2, 1, MLSTM_HEADS), 0.1)
    f_bias = jnp.linspace(3.0, 6.0, MLSTM_HEADS, dtype=f32) + nrm((nd, 2, 1, MLSTM_HEADS), 0.01)
    mlstm_gate_b = jnp.concatenate([i_bias, f_bias], axis=2).reshape(nd, 4, MLSTM_HEADS)
    mlstm_norm_g = 1.0 + nrm((nd, MLSTM_WIDTH), 0.01)
    mlstm_w_out = nrm((nd, MLSTM_WIDTH + MEM_WIDTH, d), (MLSTM_WIDTH + MEM_WIDTH) ** -0.5 * beta)
    mem_w_kv = nrm((DEPTH, d, 2 * MEM_WIDTH), d ** -0.5)
    ln1_g = 1.0 + nrm((DEPTH, d), 0.01)
    ln1_b = nrm((DEPTH, d), 0.01)
    ln2_g = 1.0 + nrm((DEPTH, d), 0.01)
    ln2_b = nrm((DEPTH, d), 0.01)
    router_g_w = nrm((DEPTH, d, MOE_GROUPS), d ** -0.5)
    router_g_b = nrm((DEPTH, MOE_GROUPS), 0.01)
    router_e_w = nrm((DEPTH, d, MOE_EXPERTS), d ** -0.5)
    router_e_b = nrm((DEPTH, MOE_EXPERTS), 0.01)
    moe_w_gate = nrm((DEPTH, MOE_EXPERTS, d, MOE_HIDDEN), d ** -0.5)
    moe_w_up = nrm((DEPTH, MOE_EXPERTS, d, MOE_HIDDEN), d ** -0.5)
    moe_w_down = nrm((DEPTH, MOE_EXPERTS, MOE_HIDDEN, d), MOE_HIDDEN ** -0.5 * beta)
    return {"x": x, "mem": mem,
            "s5_w_in": s5_w_in, "s5_lam_re": s5_lam_re, "s5_lam_im": s5_lam_im, "s5_log_step": s5_log_step,
            "s5_b_re": s5_b_re, "s5_b_im": s5_b_im, "s5_c_re": s5_c_re, "s5_c_im": s5_c_im, "s5_d": s5_d,
            "s5_w_glu": s5_w_glu, "s5_b_glu": s5_b_glu, "s5_w_out": s5_w_out,
            "dil_w_in": dil_w_in, "dil_w_out": dil_w_out,
            "win_w_in": win_w_in, "win_sink": win_sink, "win_w_out": win_w_out,
            "mlstm_w_in": mlstm_w_in, "mlstm_gate_b": mlstm_gate_b, "mlstm_norm_g": mlstm_norm_g,
            "mlstm_w_out": mlstm_w_out,
            "mem_w_kv": mem_w_kv, "ln1_g": ln1_g, "ln1_b": ln1_b, "ln2_g": ln2_g, "ln2_b": ln2_b,
            "router_g_w": router_g_w, "router_g_b": router_g_b, "router_e_w": router_e_w, "router_e_b": router_e_b,
            "moe_w_gate": moe_w_gate, "moe_w_up": moe_w_up, "moe_w_down": moe_w_down}


def reference(x, mem, s5_w_in, s5_lam_re, s5_lam_im, s5_log_step, s5_b_re, s5_b_im, s5_c_re, s5_c_im, s5_d,
              s5_w_glu, s5_b_glu, s5_w_out, dil_w_in, dil_w_out, win_w_in, win_sink, win_w_out,
              mlstm_w_in, mlstm_gate_b, mlstm_norm_g, mlstm_w_out, mem_w_kv, ln1_g, ln1_b, ln2_g, ln2_b,
              router_g_w, router_g_b, router_e_w, router_e_b, moe_w_gate, moe_w_up, moe_w_down):
    for i in range(DEPTH):
        kind, j = i % N_MIXERS, i // N_MIXERS
        if kind == 0:
            proj = x @ s5_w_in[j]
            mix = _s5_mixer(proj[..., :S5_WIDTH], s5_lam_re[j], s5_lam_im[j], s5_log_step[j], s5_b_re[j],
                            s5_b_im[j], s5_c_re[j], s5_c_im[j], s5_d[j], s5_w_glu[j], s5_b_glu[j])
            w_out = s5_w_out[j]
        elif kind == 1:
            proj = x @ dil_w_in[j]
            mix = _dilated_mixer(proj[..., :DIL_IN])
            w_out = dil_w_out[j]
        elif kind == 2:
            proj = x @ win_w_in[j]
            mix = _window_mixer(proj[..., :WIN_IN], win_sink[j])
            w_out = win_w_out[j]
        else:
            proj = x @ mlstm_w_in[j]
            mix = _mlstm_mixer(proj[..., :MLSTM_IN], mlstm_gate_b[j], mlstm_norm_g[j])
            w_out = mlstm_w_out[j]
        mem_out = _memory_attention(proj[..., -MEM_WIDTH:], mem, mem_w_kv[i])
        y = jnp.concatenate([mix.astype(x.dtype), mem_out.astype(x.dtype)], axis=-1) @ w_out
        x = _layer_norm(DEEPNORM_ALPHA * x + y, ln1_g[i], ln1_b[i])
        moe = _hier_moe(x, router_g_w[i], router_g_b[i], router_e_w[i], router_e_b[i],
                        moe_w_gate[i], moe_w_up[i], moe_w_down[i])
        x = _layer_norm(DEEPNORM_ALPHA * x + moe, ln2_g[i], ln2_b[i])
    return x
```

```python
import math
import numpy as np
from contextlib import ExitStack
import concourse.bass as bass
import concourse.mybir as mybir
from concourse.bass_utils import run_bass_kernel_spmd

F32 = mybir.dt.float32
BF16 = mybir.dt.bfloat16
ALU = mybir.AluOpType
AF = mybir.ActivationFunctionType
AX = mybir.AxisListType

SAME_ENGINE_SYNC = {"pe": False, "act": True, "dve": True, "pool": True, "sp": True}
NDMA_SEMS = {"sp": 20, "pool": 12, "act": 4}

L = 2048
D = 1024
NT = 16
ALPHA = 8.0 ** 0.25
EPS = 1e-5
NCORES = 8


class _Op:
    __slots__ = ("eng", "fn", "dma", "deps", "sig", "sem", "val", "prewait")

    def __init__(self, eng, fn, dma):
        self.eng, self.fn, self.dma = eng, fn, dma
        self.deps = set()
        self.sig = False
        self.sem = None
        self.val = 0
        self.prewait = None


class Prog:
    def __init__(self, nc, es):
        self.nc, self.es = nc, es
        self.ops = []
        self.lastw = {}
        self.readers = {}
        self.n = 0

    def sb(self, shape, dt, name=None):
        self.n += 1
        return self.es.enter_context(self.nc.sbuf_tensor(name or f"sb{self.n}", list(shape), dt))

    def ps(self, shape, dt, name=None):
        self.n += 1
        return self.es.enter_context(self.nc.psum_tensor(name or f"ps{self.n}", list(shape), dt))

    @staticmethod
    def _norm(k):
        if isinstance(k, tuple):
            return (k[0], k[1:] if len(k) > 1 else None)
        return (k, None)

    def _conf(self, table, root, sub):
        d = table.get(root)
        if not d:
            return []
        if sub is None:
            return list(d.values())
        out = []
        if sub in d:
            out.append(d[sub])
        if None in d:
            out.append(d[None])
        return out

    def add(self, eng, fn, reads=(), writes=(), dma=False):
        op = _Op(eng, fn, dma)
        i = len(self.ops)
        for k in reads:
            root, sub = self._norm(k)
            for w in self._conf(self.lastw, root, sub):
                op.deps.add(w)
        for k in writes:
            root, sub = self._norm(k)
            for w in self._conf(self.lastw, root, sub):
                op.deps.add(w)
            for rl in self._conf(self.readers, root, sub):
                op.deps.update(rl)
        for k in reads:
            root, sub = self._norm(k)
            self.readers.setdefault(root, {}).setdefault(sub, []).append(i)
        for k in writes:
            root, sub = self._norm(k)
            d = self.lastw.setdefault(root, {})
            r = self.readers.setdefault(root, {})
            if sub is None:
                d.clear()
                r.clear()
            d[sub] = i
            r[sub] = []
        op.deps.discard(i)
        self.ops.append(op)
        return i

    def emit(self):
        nc, ops = self.nc, self.ops
        for i, op in enumerate(ops):
            keep = set()
            for j in op.deps:
                p = ops[j]
                if p.eng == op.eng and not p.dma and not op.dma and not SAME_ENGINE_SYNC[op.eng]:
                    continue
                p.sig = True
                keep.add(j)
            op.deps = keep
        for op in ops:
            if op.dma:
                op.sig = True
        engs = ["pe", "act", "dve", "pool", "sp"]
        esem = {e: self.es.enter_context(nc.semaphore(f"s_{e}")) for e in engs}
        dsems = {e: [self.es.enter_context(nc.semaphore(f"d_{e}{k}")) for k in range(n)]
                 for e, n in NDMA_SEMS.items()}
        cnt = {e: 0 for e in engs}
        dcnt = {e: [0] * n for e, n in NDMA_SEMS.items()}
        drr = {e: 0 for e in NDMA_SEMS}
        for op in ops:
            if op.dma:
                k = drr[op.eng]
                drr[op.eng] = (k + 1) % NDMA_SEMS[op.eng]
                op.sem = dsems[op.eng][k]
                if dcnt[op.eng][k] > 0:
                    op.prewait = (op.sem, dcnt[op.eng][k])
                dcnt[op.eng][k] += 16
                op.val = dcnt[op.eng][k]
            elif op.sig:
                cnt[op.eng] += 1
                op.sem = esem[op.eng]
                op.val = cnt[op.eng]
        per = {e: [] for e in engs}
        for op in ops:
            per[op.eng].append(op)
        self.stats = {e: len(per[e]) for e in engs}

        def run(e, engine):
            seen = {}
            for op in per[e]:
                waits = {}
                if op.prewait is not None:
                    waits[op.prewait[0]] = op.prewait[1]
                for j in op.deps:
                    p = ops[j]
                    if waits.get(p.sem, 0) < p.val:
                        waits[p.sem] = p.val
                for s, v in waits.items():
                    if seen.get(s, 0) >= v:
                        continue
                    seen[s] = v
                    engine.wait_ge(s, v)
                inst = op.fn(engine)
                if op.sig and inst is not None:
                    inst.then_inc(op.sem, 16 if op.dma else 1)

        with nc.Block() as block:
            @block.tensor
            def _(t):
                run("pe", t)

            @block.scalar
            def _(t):
                run("act", t)

            @block.vector
            def _(t):
                run("dve", t)

            @block.gpsimd
            def _(t):
                run("pool", t)

            @block.sync
            def _(t):
                run("sp", t)


class Rot:
    def __init__(self, items):
        self.items, self.i = list(items), 0

    def next(self):
        v = self.items[self.i % len(self.items)]
        self.i += 1
        return v


def _slopes(n):
    return 2.0 ** (-8.0 * np.arange(1, n + 1, dtype=np.float64) / n)


def _emask(radius, unit, slopes):
    kl = np.arange(128)[:, None]
    ql = np.arange(128)[None, :]
    out = np.zeros((len(slopes), 3, 128, 128), np.float32)
    for di, dlt in enumerate((-1, 0, 1)):
        delta = np.abs(kl - ql + 128 * dlt).astype(np.float64)
        for h, s in enumerate(slopes):
            m = np.exp(-s * unit * delta)
            m[delta > radius] = 0.0
            out[h, di] = m
    return out


def _consts():
    c = {}
    c["c_ident"] = np.eye(128, dtype=np.float32)
    dm = np.stack([_emask(64, dil, _slopes(8)) for dil in (1, 4, 16)])
    dm = dm.reshape(3, 4, 2, 3, 128, 128)
    c["c_dilmask"] = np.ascontiguousarray(dm.transpose(4, 1, 0, 2, 3, 5)).reshape(128, 72 * 128)
    wm = _emask(128, 1, _slopes(12))
    wm = wm.reshape(2, 2, 3, 3, 128, 128)
    c["c_winmask"] = np.ascontiguousarray(wm.transpose(4, 0, 2, 1, 3, 5)).reshape(128, 36 * 128)
    s_ = np.arange(128)[:, None]
    t_ = np.arange(128)[None, :]
    tri = np.stack([(s_ <= t_), (s_ >= t_), np.ones((128, 128), bool)], axis=1).astype(np.float32)
    c["c_tri"] = np.ascontiguousarray(tri).reshape(128, 3 * 128)
    sel = np.zeros((128, 8, 8, 128), np.float32)
    selT = np.zeros((128, 8, 8, 128), np.float32)
    for j in range(8):
        for tau in range(8):
            for cch in range(16):
                sel[16 * j + cch, j, tau, tau * 16 + cch] = 1.0
                selT[tau * 16 + cch, j, tau, 16 * j + cch] = 1.0
    c["c_sel"] = sel.reshape(128, 8192)
    c["c_selT"] = selT.reshape(128, 8192)
    ev = np.arange(-7, 9, dtype=np.float32)
    c["c_evec"] = np.ascontiguousarray(np.broadcast_to(np.concatenate([ev, ev[::-1]])[None, :], (128, 32))).astype(np.float32)
    tau = (np.arange(128) // 16)
    mF = (tau[None, :] >= tau[:, None]).astype(np.float32)
    mB = (tau[None, :] <= tau[:, None]).astype(np.float32)
    c["c_s5mask"] = np.ascontiguousarray(np.stack([mF, mB], axis=1)).reshape(128, 256)
    return c


CONST_SHAPES = {"c_sel": [128, 8192], "c_selT": [128, 8192], "c_evec": [128, 32], "c_s5mask": [128, 256], "c_tri": [128, 384], "c_ident": [128, 128], "c_dilmask": [128, 72 * 128], "c_winmask": [128, 36 * 128]}

WEIGHT_SHAPES = {
    "s5_w_in": [1, 1024, 768], "s5_lam_re": [1, 2, 32, 64], "s5_lam_im": [1, 2, 32, 64],
    "s5_log_step": [1, 2, 32], "s5_b_re": [1, 32, 64, 16], "s5_b_im": [1, 32, 64, 16],
    "s5_c_re": [1, 32, 16, 64], "s5_c_im": [1, 32, 16, 64], "s5_d": [1, 512],
    "s5_w_glu": [1, 512, 512], "s5_b_glu": [1, 512], "s5_w_out": [1, 768, 1024],
    "dil_w_in": [1, 1024, 4864], "dil_w_out": [1, 768, 1024],
    "win_w_in": [1, 1024, 1536], "win_sink": [1, 12], "win_w_out": [1, 1024, 1024],
    "mlstm_w_in": [1, 1024, 3344], "mlstm_gate_b": [1, 4, 4], "mlstm_norm_g": [1, 768],
    "mlstm_w_out": [1, 1024, 1024],
    "mem_w_kv": [4, 1024, 512], "ln1_g": [4, 1024], "ln1_b": [4, 1024], "ln2_g": [4, 1024],
    "ln2_b": [4, 1024], "router_g_w": [4, 1024, 4], "router_g_b": [4, 4],
    "router_e_w": [4, 1024, 16], "router_e_b": [4, 16],
    "moe_w_gate": [4, 16, 1024, 256], "moe_w_up": [4, 16, 1024, 256], "moe_w_down": [4, 16, 256, 1024],
}


def build(nseq, layers=(0, 1, 2, 3), do_moe=True, debug=()):
    nc = bass.Bass("TRN2", target_bir_lowering=False)
    W = {k: nc.dram_tensor(k, s, F32, kind="ExternalInput").ap() for k, s in WEIGHT_SHAPES.items()}
    CT = {k: nc.dram_tensor(k, s, F32, kind="ExternalInput").ap() for k, s in CONST_SHAPES.items()}
    x_d = nc.dram_tensor("x", [nseq, L, D], F32, kind="ExternalInput").ap()
    mem_d = nc.dram_tensor("mem", [nseq, 256, D], F32, kind="ExternalInput").ap()
    out_d = nc.dram_tensor("out", [nseq, L, D], F32, kind="ExternalOutput").ap()

    with ExitStack() as es:
        P = Prog(nc, es)
        x_tm = P.sb([128, NT, D], F32, "x_tm")
        xT = P.sb([128, 8, L], BF16, "xT")
        mixT = P.sb([128, 8, L], BF16, "mixT")
        memT = P.sb([128, 8, 256], BF16, "memT")
        ident = P.sb([128, 128], BF16, "ident")
        onesAB = P.sb([128, 2, 128], BF16, "onesAB")
        lnp = P.sb([128, 2, D], F32, "lnp")
        xb = [P.sb([128, D], BF16, f"xb{i}") for i in range(1)]
        sttA = P.sb([128, NT, 2, 6], F32, "sttA")
        mvA = P.sb([128, NT, 4], F32, "mvA")
        esk = P.sb([128, 8], F32, "esk")
        dummy = P.sb([128, 8], F32, "dummyt")
        SCR_BYTES = 56832
        scr = P.sb([128, SCR_BYTES // 2], BF16, "scr")

        def view(off, shape, dt=BF16):
            n = int(np.prod(shape))
            if dt == BF16:
                v = scr[:, off // 2:off // 2 + n]
            else:
                v = scr[:, off // 2:off // 2 + 2 * n].bitcast(F32)
            if len(shape) == 1:
                return v
            names = " ".join(f"a{i}" for i in range(len(shape)))
            return v.rearrange(f"p ({names}) -> p {names}", **{f"a{i}": shape[i] for i in range(1, len(shape))})

        wo = view(0, [8, 512])
        wkv = view(8192, [8, 256])
        KTm = view(12288, [2, 256])
        vm = view(13312, [2, 2, 2, 128])
        rec = [view(15360, [512], F32)]
        wbuf = view(17408, [8, 384])
        QT = view(23552, [L])
        KT = view(27648, [L])
        vAB = view(31744, [NT, 2, 128])
        pts = [view(39936 + 1024 * i, [512]) for i in range(8)] + [view(52736 + 1024 * i, [512]) for i in range(4)]
        masks = view(48128, [18 * 128])
        accN = view(0, [L], F32)
        accD = view(8192, [L], F32)
        ATT_KEYS = ["wo", "wkv", "KTm", "vm", "rec", "wbuf", "QT", "KT", "vAB", "pt", "masks"]
        ACC_KEYS = ["accN", "accD"]
        wg = [view(4096 * i, [8, 256]) for i in range(2)]
        wu = [view(8192 + 4096 * i, [8, 256]) for i in range(2)]
        wd = [view(16384 + 4096 * i, [2, 1024]) for i in range(2)]
        hT = [view(24576 + 8192 * i, [2, L]) for i in range(2)]
        sl = [view(40960 + 1024 * i, [512]) for i in range(2)]
        MOE_KEYS = ["wg", "wu", "wd", "hT", "sl"]
        banks = [P.ps([128, 512], F32, f"bank{i}") for i in range(8)]
        rot = Rot(range(4))
        acc = Rot([(4, 5), (6, 7)])
        ptr = Rot(range(12))
        recr = Rot(range(1))
        xbr = Rot(range(1))

        def barrier(rd, wr):
            P.add("dve", lambda e: e.memset(dummy[:, 0:1], 0.0), reads=list(rd), writes=list(wr) + ["dummy"])

        def bk(i):
            return ("bank", i)

        dbg_seen = set()

        def dbg(name, ap, keys):
            if name not in debug or name in dbg_seen:
                return
            dbg_seen.add(name)
            dt_ = nc.dram_tensor("dbg_" + name, list(ap.shape), ap.dtype, kind="ExternalOutput").ap()
            P.add("sp", lambda e: e.dma_start(out=dt_, in_=ap), reads=keys, writes=[("dbgout", name)], dma=True)

        def dma(eng, out, in_, reads, writes):
            P.add(eng, lambda e: e.dma_start(out=out, in_=in_), reads=reads, writes=writes, dma=True)

        def load_w(dst, key, src, c0, n, col0=0, nk=8):
            dma("pool", dst[:, 0:nk, col0:col0 + n],
                src[:, c0:c0 + n].rearrange("(kc p) n -> p kc n", p=128), [], [key])

        lnT = P.sb([128, 4, 2, 2, 8], F32, "lnT")
        lnstage = view(0, [128], F32)
        identf32 = view(512, [128], F32)
        dma("sp", identf32, CT["c_ident"], [], ["wo"])
        for li_ in range(4):
            for wi_, (gn, bn) in enumerate((("ln1_g", "ln1_b"), ("ln2_g", "ln2_b"))):
                for gi_, nm in enumerate((gn, bn)):
                    r0 = ((li_ * 2 + wi_) * 2 + gi_) * 8
                    dma("sp", lnstage[r0:r0 + 8, :], W[nm][li_].rearrange("(c p) -> c p", p=128), [], ["wo"])
        b_ = rot.next()
        P.add("pe", lambda e: e.transpose(banks[b_][:, 0:128], lnstage, identf32), reads=["wo"], writes=[bk(b_)])
        P.add("act", lambda e: e.activation(out=lnT[:].rearrange("p a b c d -> p (a b c d)"), in_=banks[b_][:, 0:128], func=AF.Copy), reads=[bk(b_)], writes=["lnT"])
        dma("pool", ident[:], CT["c_ident"], [], ["ident"])
        P.add("pool", lambda e: e.memset(onesAB[:], 0.0), writes=["onesAB"])
        P.add("pool", lambda e: e.memset(onesAB[:, 0, 0:64], 1.0), writes=["onesAB"])
        P.add("pool", lambda e: e.memset(onesAB[:, 1, 64:128], 1.0), writes=["onesAB"])

        def transposes_to(src_bf, nchunk, dst_fn, src_key, dst_key, affine=None, c0=0):
            b = rot.next()
            pv = banks[b][:].bitcast(BF16)

            def f(e):
                r = None
                for c in range(nchunk):
                    r = e.transpose(pv[:, c * 128:(c + 1) * 128], src_bf[:, c * 128:(c + 1) * 128], ident[:])
                return r
            P.add("pe", f, reads=[src_key, "ident"], writes=[bk(b)])
            if affine is None:
                src = pv[:, 0:nchunk * 128].rearrange("p (c t) -> p c t", t=128)
                P.add("act", lambda e: e.activation(out=dst_fn(), in_=src, func=AF.Copy), reads=[bk(b)], writes=[dst_key])
            else:
                gc, bc = affine
                for c in range(nchunk):
                    P.add("act", lambda e, c=c: e.activation(out=dst_fn()[:, c, :], in_=pv[:, c * 128:(c + 1) * 128], func=AF.Identity,
                                                           scale=gc[:, c0 + c:c0 + c + 1], bias=bc[:, c0 + c:c0 + c + 1]),
                          reads=[bk(b), "lnT"], writes=[tuple(dst_key) + (c,)])

        def make_xT_tile(t, norm=None):
            for hc in range(2):
                cs = slice(512 * hc, 512 * hc + 512)
                xh = xb[0][:, cs]
                if norm is None:
                    if hc == 0:
                        P.add("act", lambda e, xh=xh, cs=cs: e.activation(out=xh, in_=x_tm[:, t, cs], func=AF.Copy), reads=[("x_tm", t)], writes=[("xb", hc)])
                    else:
                        P.add("pool", lambda e, xh=xh, cs=cs: e.tensor_copy(out=xh, in_=x_tm[:, t, cs]), reads=[("x_tm", t)], writes=[("xb", hc)])
                    aff = None
                else:
                    P.add("act", lambda e, xh=xh, cs=cs: e.activation(out=xh, in_=x_tm[:, t, cs], func=AF.Identity, bias=norm[1], scale=norm[0]),
                          reads=[("x_tm", t), "mv"], writes=[("xb", hc)])
                    aff = (norm[2], norm[3])
                transposes_to(xh, 4, lambda hc=hc: xT[:, 4 * hc:4 * hc + 4, t * 128:(t + 1) * 128], ("xb", hc), ("xT", t, hc), affine=aff, c0=4 * hc)

        def linear_fm(dst, dst_key, w_lhsT, w_key, ntok=L, rhs_fn=None, rhs_key="xT", scale=1.0, nk=8, M=128, perm=1):
            for n in range(ntok // 512):
                b = rot.next()

                def f(e, n=n, b=b):
                    r = None
                    for kc in range(nk):
                        rhs = rhs_fn(kc, n) if rhs_fn else xT[:, kc, n * 512:(n + 1) * 512]
                        r = e.matmul(banks[b][0:M, :], lhsT=w_lhsT(kc), rhs=rhs, start=(kc == 0), stop=(kc == nk - 1))
                    return r
                rk = [rhs_key] if (rhs_fn or rhs_key != "xT") else [("xT", t_, hc_) for t_ in range(4 * n, 4 * n + 4) for hc_ in range(2)]
                P.add("pe", f, reads=[w_key] + rk, writes=[bk(b)])
                if perm > 1:
                    o = dst.rearrange("p (r s) -> p s r", r=perm)[:, n * 512 // perm:(n + 1) * 512 // perm, :]
                    src_ = banks[b][0:M, :].rearrange("p (s r) -> p s r", r=perm)
                else:
                    o = dst[:, n * 512:(n + 1) * 512]
                    src_ = banks[b][0:M, :]
                P.add("act", lambda e, o=o, b=b, src_=src_: e.activation(out=o, in_=src_, func=AF.Copy, scale=scale),
                      reads=[bk(b)], writes=[dst_key])

        def load_ln(li, which):
            for i, nm in enumerate((f"ln{which}_g", f"ln{which}_b")):
                dma("sp", lnp[:, i, :], W[nm][li].partition_broadcast(128), [], ["lnp"])

        def layer_norm_all(li, which, after4=None, make=True):
            gcol = lnT[:, li, which - 1, 0, :]
            bcol = lnT[:, li, which - 1, 1, :]
            for t in range(NT):
                for hf in range(2):
                    jb = rot.next()
                    P.add("act", lambda e, t=t, hf=hf, jb=jb: e.activation(out=banks[jb][:], in_=x_tm[:, t, hf * 512:(hf + 1) * 512], func=AF.Square,
                                                                         accum_out=sttA[:, t, hf, 1:2]),
                          reads=[("x_tm", t)], writes=[bk(jb), ("stt", t, hf, "q")])
            SV = ["stt", "mv"]
            P.add("dve", lambda e: e.tensor_tensor(out=mvA[:, :, 0], in0=sttA[:, :, 0, 0], in1=sttA[:, :, 1, 0], op=ALU.add), reads=SV, writes=["mv"])
            P.add("dve", lambda e: e.tensor_scalar(out=mvA[:, :, 0], in0=mvA[:, :, 0], scalar1=1.0 / 1024, scalar2=None, op0=ALU.mult), reads=SV, writes=["mv"])
            P.add("dve", lambda e: e.tensor_tensor(out=mvA[:, :, 1], in0=sttA[:, :, 0, 1], in1=sttA[:, :, 1, 1], op=ALU.add), reads=SV, writes=["mv"])
            P.add("dve", lambda e: e.tensor_scalar(out=mvA[:, :, 1], in0=mvA[:, :, 1], scalar1=1.0 / 1024, scalar2=EPS, op0=ALU.mult, op1=ALU.add), reads=SV, writes=["mv"])
            P.add("dve", lambda e: e.tensor_tensor(out=mvA[:, :, 2], in0=mvA[:, :, 0], in1=mvA[:, :, 0], op=ALU.mult), reads=SV, writes=["mv"])
            P.add("dve", lambda e: e.tensor_tensor(out=mvA[:, :, 1], in0=mvA[:, :, 1], in1=mvA[:, :, 2], op=ALU.subtract), reads=SV, writes=["mv"])
            P.add("act", lambda e: e.activation(out=mvA[:, :, 2], in_=mvA[:, :, 1], func=AF.Sqrt), reads=["mv"], writes=["mv"])
            P.add("dve", lambda e: e.reciprocal(out=mvA[:, :, 2], in_=mvA[:, :, 2]), reads=["mv"], writes=["mv"])
            P.add("dve", lambda e: e.scalar_tensor_tensor(out=mvA[:, :, 3], in0=mvA[:, :, 0], scalar=-1.0, in1=mvA[:, :, 2], op0=ALU.mult, op1=ALU.mult), reads=["mv"], writes=["mv"])
            for t in range(NT):
                P.add("act", lambda e, t=t: e.activation(out=x_tm[:, t, :], in_=x_tm[:, t, :], func=AF.Identity, bias=mvA[:, t, 3:4], scale=mvA[:, t, 2:3]),
                      reads=[("x_tm", t), "mv"], writes=[("x_tm", t)])
            for t in range(NT):
                P.add("dve", lambda e, t=t: e.tensor_tensor(out=x_tm[:, t, :], in0=x_tm[:, t, :], in1=lnp[:, 0, :], op=ALU.mult),
                      reads=[("x_tm", t), "lnp"], writes=[("x_tm", t)])
                P.add("dve", lambda e, t=t: e.tensor_tensor(out=x_tm[:, t, :], in0=x_tm[:, t, :], in1=lnp[:, 1, :], op=ALU.add),
                      reads=[("x_tm", t), "lnp"], writes=[("x_tm", t)])
            for t in range(NT if make else 0):
                make_xT_tile(t)
                if after4 is not None and t % 4 == 3:
                    after4(t // 4)

        def attn_epi_norm(chunk, sink_col=None):
            def epi(g, nb, db):
                r = recr.next()
                if sink_col is not None:
                    P.add("dve", lambda e: e.tensor_scalar(out=rec[r], in0=banks[db][:], scalar1=esk[:, sink_col:sink_col + 1], scalar2=None, op0=ALU.add),
                          reads=[bk(db), "esk"], writes=[("rec", r)])
                    P.add("dve", lambda e: e.reciprocal(out=rec[r], in_=rec[r]), reads=[("rec", r)], writes=[("rec", r)])
                else:
                    P.add("dve", lambda e: e.reciprocal(out=rec[r], in_=banks[db][:]), reads=[bk(db)], writes=[("rec", r)])
                P.add("dve", lambda e: e.tensor_tensor(out=mixT[:, chunk, g * 512:(g + 1) * 512], in0=banks[nb][:], in1=rec[r], op=ALU.mult),
                      reads=[bk(nb), ("rec", r)], writes=[("mixT", chunk, g)])
            return epi

        def attn_banded(nsub, mask_fn, scale, epi):
            def stageA(g):
                lst = []
                for ab in (0, 1):
                    hp0 = 64 * ab
                    for di, dlt in enumerate((-1, 0, 1)):
                        valid = [i for i in range(4 * g, 4 * g + 4)
                                 if 0 <= i + dlt < NT and (i // nsub) == ((i + dlt) // nsub)]
                        if not valid:
                            continue
                        b = rot.next()
                        pi = ptr.next()
                        c0, c1 = (valid[0] - 4 * g) * 128, (valid[-1] + 1 - 4 * g) * 128

                        def mm(e, valid=valid, b=b, hp0=hp0, dlt=dlt):
                            r = None
                            for i in valid:
                                r = e.matmul(banks[b][:, (i - 4 * g) * 128:(i - 4 * g + 1) * 128],
                                             lhsT=KT[hp0:hp0 + 64, (i + dlt) * 128:(i + dlt + 1) * 128],
                                             rhs=QT[hp0:hp0 + 64, i * 128:(i + 1) * 128], start=True, stop=True)
                            return r
                        P.add("pe", mm, reads=["QT", "KT"], writes=[bk(b)])
                        P.add("act", lambda e, b=b, pi=pi, c0=c0, c1=c1: e.activation(out=pts[pi][:, c0:c1], in_=banks[b][:, c0:c1], func=AF.Exp, scale=scale),
                              reads=[bk(b)], writes=[("pt", pi)])
                        m = mask_fn(ab, di)
                        nt_ = (c1 - c0) // 128
                        P.add("dve", lambda e, pi=pi, c0=c0, c1=c1, m=m, nt_=nt_: e.tensor_tensor(
                            out=pts[pi][:, c0:c1].rearrange("p (t q) -> p t q", q=128),
                            in0=pts[pi][:, c0:c1].rearrange("p (t q) -> p t q", q=128),
                            in1=m.unsqueeze(1).to_broadcast([128, nt_, 128]), op=ALU.mult),
                            reads=[("pt", pi), "masks"], writes=[("pt", pi)])
                        lst.append((ab, dlt, valid, pi))
                return lst

            def stageB(g, lst):
                nb, db = acc.next()

                def pv(e):
                    r = None
                    for i in range(4 * g, 4 * g + 4):
                        cs = slice((i - 4 * g) * 128, (i - 4 * g + 1) * 128)
                        con = [(ab, dlt, pi) for (ab, dlt, valid, pi) in lst if i in valid]
                        for idx, (ab, dlt, pi) in enumerate(con):
                            e.matmul(banks[nb][:, cs], lhsT=vAB[:, i + dlt, ab, :], rhs=pts[pi][:, cs],
                                     start=(idx == 0), stop=(idx == len(con) - 1))
                            r = e.matmul(banks[db][:, cs], lhsT=onesAB[:, ab, :], rhs=pts[pi][:, cs],
                                         start=(idx == 0), stop=(idx == len(con) - 1))
                    return r
                P.add("pe", pv, reads=["vAB", "onesAB"] + [("pt", pi) for (_, _, _, pi) in lst], writes=[bk(nb), bk(db)])
                epi(g, nb, db)

            prev = stageA(0)
            for g in range(4):
                nxt = stageA(g + 1) if g < 3 else None
                stageB(g, prev)
                prev = nxt

        def mem_kv(li):
            P.add("pool", lambda e: e.memset(vm, 0.0), writes=["vm"])
            load_w(wkv, "wkv", W["mem_w_kv"][li], 0, 256)
            for c in range(2):
                b = rot.next()

                def f(e, c=c, b=b):
                    r = None
                    for kc in range(8):
                        r = e.matmul(banks[b][:, 0:256], lhsT=wkv[:, kc, c * 128:(c + 1) * 128], rhs=memT[:, kc, :], start=(kc == 0), stop=(kc == 7))
                    return r
                P.add("pe", f, reads=["wkv", "memT"], writes=[bk(b)])
                P.add("act", lambda e, c=c, b=b: e.activation(out=KTm[:, c, :], in_=banks[b][:, 0:256], func=AF.Copy), reads=[bk(b)], writes=["KTm"])
            load_w(wkv, "wkv", W["mem_w_kv"][li], 256, 256)
            for j in range(2):
                b = rot.next()

                def f(e, j=j, b=b):
                    r = None
                    for kc in range(8):
                        r = e.matmul(banks[b][:, 0:256], lhsT=memT[:, kc, j * 128:(j + 1) * 128], rhs=wkv[:, kc, :], start=(kc == 0), stop=(kc == 7))
                    return r
                P.add("pe", f, reads=["wkv", "memT"], writes=[bk(b)])
                for c in range(2):
                    for ab in range(2):
                        P.add("act", lambda e, j=j, b=b, c=c, ab=ab: e.activation(
                            out=vm[:, j, c, ab, 64 * ab:64 * ab + 64], in_=banks[b][:, c * 128 + 64 * ab:c * 128 + 64 * ab + 64], func=AF.Copy),
                            reads=[bk(b)], writes=["vm"])

        def mem_attention_pair(c, out_chunk):
            epi = attn_epi_norm(out_chunk)

            def stageA(g):
                lst = []
                for ab in (0, 1):
                    hp0 = 64 * ab
                    for j in range(2):
                        b = rot.next()
                        pi = ptr.next()
                        P.add("pe", lambda e, b=b, hp0=hp0, j=j: e.matmul(
                            banks[b][:], lhsT=KTm[hp0:hp0 + 64, c, j * 128:(j + 1) * 128], rhs=QT[hp0:hp0 + 64, g * 512:(g + 1) * 512], start=True, stop=True),
                            reads=["QT", "KTm"], writes=[bk(b)])
                        P.add("act", lambda e, b=b, pi=pi: e.activation(out=pts[pi], in_=banks[b][:], func=AF.Exp, scale=0.125),
                              reads=[bk(b)], writes=[("pt", pi)])
                        lst.append((ab, j, pi))
                return lst

            def stageB(g, lst):
                nb, db = acc.next()

                def pv(e):
                    r = None
                    for idx, (ab, j, pi) in enumerate(lst):
                        e.matmul(banks[nb][:], lhsT=vm[:, j, c, ab, :], rhs=pts[pi], start=(idx == 0), stop=(idx == len(lst) - 1))
                        r = e.matmul(banks[db][:], lhsT=onesAB[:, ab, :], rhs=pts[pi], start=(idx == 0), stop=(idx == len(lst) - 1))
                    return r
                P.add("pe", pv, reads=["vm", "onesAB"] + [("pt", pi) for (_, _, pi) in lst], writes=[bk(nb), bk(db)])
                epi(g, nb, db)
            prev = stageA(0)
            for g in range(4):
                nxt = stageA(g + 1) if g < 3 else None
                stageB(g, prev)
                prev = nxt

        def do_mem(li, w_in, qcol0, mix_chunk0):
            mem_kv(li)
            for c in range(2):
                load_w(wbuf, "wbuf", w_in, qcol0 + c * 128, 128)
                linear_fm(QT, "QT", lambda kc: wbuf[:, kc, 0:128], "wbuf")
                mem_attention_pair(c, mix_chunk0 + c)

        wbuf2 = lnp[:].rearrange("p a b -> p (a b)")[:, 0:1536].bitcast(BF16).rearrange("p (k n) -> p k n", n=384)
        WB = {"i": 0}

        def next_wb():
            WB["i"] += 1
            return (wbuf, "wbuf") if WB["i"] % 2 else (wbuf2, "lnp")
        wo1 = view(27648, [8, 512])
        wos = [wo, wo1]
        WOP = {"done": False}

        def prefetch_wo(w_out, nin_chunks, rowmap=None):
            for hf in range(2):
                for fc in range(nin_chunks):
                    pieces = rowmap(fc) if rowmap else [(0, fc * 128, 128)]
                    for (p0, r0, n) in pieces:
                        dma("pool", wos[hf][p0:p0 + n, fc, :], w_out[r0:r0 + n, hf * 512:(hf + 1) * 512], [], [("wo", hf)] + (["KT", "vAB"] if hf == 1 else []))
            WOP["done"] = True

        def out_proj_ln(li, w_out, nin_chunks, rowmap=None):
            load_ln(li, 1)
            if not WOP["done"]:
                prefetch_wo(w_out, nin_chunks, rowmap)
            WOP["done"] = False
            for hf in range(2):
                wo = wos[hf]
                for t in range(NT):
                    b = acc.next()[t % 2]

                    def f(e, b=b, t=t, wo=wo):
                        r = None
                        for fc in range(nin_chunks):
                            r = e.matmul(banks[b][:], lhsT=mixT[:, fc, t * 128:(t + 1) * 128], rhs=wo[:, fc, :],
                                         start=(fc == 0), stop=(fc == nin_chunks - 1))
                        return r
                    P.add("pe", f, reads=["mixT", ("wo", hf)] + (["KT", "vAB"] if hf == 1 else []), writes=[bk(b)])
                    P.add("dve", lambda e, b=b, hf=hf, t=t: e.scalar_tensor_tensor(
                        out=x_tm[:, t, hf * 512:(hf + 1) * 512], in0=x_tm[:, t, hf * 512:(hf + 1) * 512], scalar=ALPHA,
                        in1=banks[b][:], op0=ALU.mult, op1=ALU.add, accum_out=sttA[:, t, hf, 0:1]), reads=[bk(b), ("x_tm", t)], writes=[("x_tm", t), ("stt", t, hf)])
            if do_moe:
                moe_prefetch(li)
                layer_norm_all(li, 1, after4=lambda n: (MOE["gu_unit"](0, n), MOE["gu_unit"](0, 4 + n)))
            else:
                layer_norm_all(li, 1)

        wr = P.sb([128, 8, 20], BF16, "wr")
        rb = P.sb([128, 20], F32, "rb")
        lg = P.sb([128, NT, 20], F32, "lg")
        gate = P.sb([128, NT, 16], F32, "gate")
        rt = [P.sb([128, NT, 16], F32, f"rt{i}") for i in range(3)]
        rs = P.sb([128, 8, NT], F32, "rs")

        MOE = {}

        def moe_prefetch(li):
            barrier(ATT_KEYS + ACC_KEYS, MOE_KEYS)

            def load_gu(ex):
                k = ex % 2
                dma("pool", wg[k], W["moe_w_gate"][li, ex].rearrange("(kc p) n -> p kc n", p=128), [], [("wg", k)])
                dma("pool", wu[k], W["moe_w_up"][li, ex].rearrange("(kc p) n -> p kc n", p=128), [], [("wu", k)])

            def load_d(ex):
                k = ex % 2
                dma("pool", wd[k], W["moe_w_down"][li, ex].rearrange("(kc p) n -> p kc n", p=128), [], [("wd", k)])

            def gu_unit(ex, i):
                k = ex % 2
                fc, n = i // 4, i % 4
                bg, bu = rot.next(), rot.next()

                def f(e):
                    r = None
                    for kc in range(8):
                        e.matmul(banks[bg][:], lhsT=wg[k][:, kc, fc * 128:(fc + 1) * 128], rhs=xT[:, kc, n * 512:(n + 1) * 512], start=(kc == 0), stop=(kc == 7))
                    for kc in range(8):
                        r = e.matmul(banks[bu][:], lhsT=wu[k][:, kc, fc * 128:(fc + 1) * 128], rhs=xT[:, kc, n * 512:(n + 1) * 512], start=(kc == 0), stop=(kc == 7))
                    return r
                P.add("pe", f, reads=[("xT", t_, hc_) for t_ in range(4 * n, 4 * n + 4) for hc_ in range(2)] + [("wg", k), ("wu", k)], writes=[bk(bg), bk(bu)])
                si = i % 2
                P.add("act", lambda e: e.activation(out=sl[si], in_=banks[bg][:], func=AF.Silu), reads=[bk(bg)], writes=[("sl", si)])
                P.add("dve", lambda e: e.tensor_tensor(out=hT[k][:, fc, n * 512:(n + 1) * 512], in0=banks[bu][:], in1=sl[si], op=ALU.mult),
                      reads=[bk(bu), ("sl", si)], writes=[("hT", k, fc, n)])
            MOE.update(load_gu=load_gu, load_d=load_d, gu_unit=gu_unit)
            load_gu(0)
            load_d(0)
            load_gu(1)

        def moe(li):
            load_gu, load_d, gu_unit = MOE["load_gu"], MOE["load_d"], MOE["gu_unit"]
            load_w(wr, "wr", W["router_g_w"][li], 0, 4, col0=0)
            load_w(wr, "wr", W["router_e_w"][li], 0, 16, col0=4)
            dma("sp", rb[:, 0:4], W["router_g_b"][li].partition_broadcast(128), [], ["rb"])
            dma("sp", rb[:, 4:20], W["router_e_b"][li].partition_broadcast(128), [], ["rb"])
            b = rot.next()

            def f(e):
                r = None
                for t in range(NT):
                    for kc in range(8):
                        r = e.matmul(banks[b][:, t * 20:(t + 1) * 20], lhsT=xT[:, kc, t * 128:(t + 1) * 128], rhs=wr[:, kc, :], start=(kc == 0), stop=(kc == 7))
                return r
            P.add("pe", f, reads=["xT", "wr"], writes=[bk(b)])

            def D(fn, rd, wr):
                P.add("dve", fn, reads=rd, writes=wr)
            gmax, gsum, m1, m2, dd, w1, w2 = (rs[:, i, :] for i in range(7))
            lv = banks[b][:, 0:NT * 20].rearrange("p (t c) -> p t c", c=20)
            gl = lg[:, :, 0:4]
            r0s, r1s = rt[0][:, :, 0:4], rt[1][:, :, 0:4]
            elm = rt[2][:].rearrange("p t (g x) -> p t g x", x=4)
            RK = ["lg", "rs", "rt"]
            D(lambda e: e.tensor_tensor(out=lg[:], in0=lv, in1=rb[:].unsqueeze(1).to_broadcast([128, NT, 20]), op=ALU.add), [bk(b), "rb"], RK)
            D(lambda e: e.tensor_reduce(out=gmax, in_=gl, axis=AX.X, op=ALU.max), RK, RK)
            D(lambda e: e.tensor_tensor(out=r0s, in0=gl, in1=gmax.unsqueeze(2).to_broadcast([128, NT, 4]), op=ALU.is_equal), RK, RK)
            D(lambda e: e.tensor_tensor(out=r1s, in0=gl, in1=gmax.unsqueeze(2).to_broadcast([128, NT, 4]), op=ALU.subtract), RK, RK)
            D(lambda e: e.tensor_scalar(out=r0s, in0=r0s, scalar1=-1.0, scalar2=1e30, op0=ALU.add, op1=ALU.mult), RK, RK)
            P.add("act", lambda e: e.activation(out=r1s, in_=r1s, func=AF.Exp), reads=RK, writes=RK)
            D(lambda e: e.tensor_reduce(out=gsum, in_=r1s, axis=AX.X, op=ALU.add), RK, RK)
            D(lambda e: e.reciprocal(out=gsum, in_=gsum), RK, RK)
            D(lambda e: e.tensor_tensor(out=elm, in0=lg[:, :, 4:20].rearrange("p t (g x) -> p t g x", x=4),
                                        in1=r0s.unsqueeze(3).to_broadcast([128, NT, 4, 4]), op=ALU.add), RK, RK)
            D(lambda e: e.tensor_reduce(out=m1, in_=rt[2][:], axis=AX.X, op=ALU.max), RK, RK)
            D(lambda e: e.tensor_tensor(out=rt[0][:], in0=rt[2][:], in1=m1.unsqueeze(2).to_broadcast([128, NT, 16]), op=ALU.is_equal), RK, RK)
            D(lambda e: e.scalar_tensor_tensor(out=rt[2][:], in0=rt[0][:], scalar=-1e30, in1=rt[2][:], op0=ALU.mult, op1=ALU.add), RK, RK)
            D(lambda e: e.tensor_reduce(out=m2, in_=rt[2][:], axis=AX.X, op=ALU.max), RK, RK)
            D(lambda e: e.tensor_tensor(out=rt[1][:], in0=rt[2][:], in1=m2.unsqueeze(2).to_broadcast([128, NT, 16]), op=ALU.is_equal), RK, RK)
            D(lambda e: e.tensor_tensor(out=dd, in0=m2, in1=m1, op=ALU.subtract), RK, RK)
            P.add("act", lambda e: e.activation(out=dd, in_=dd, func=AF.Exp), reads=RK, writes=RK)
            D(lambda e: e.tensor_scalar(out=w1, in0=dd, scalar1=1.0, scalar2=None, op0=ALU.add), RK, RK)
            D(lambda e: e.reciprocal(out=w1, in_=w1), RK, RK)
            D(lambda e: e.tensor_tensor(out=w1, in0=w1, in1=gsum, op=ALU.mult), RK, RK)
            D(lambda e: e.tensor_tensor(out=w2, in0=w1, in1=dd, op=ALU.mult), RK, RK)
            D(lambda e: e.tensor_tensor(out=rt[0][:], in0=rt[0][:], in1=w1.unsqueeze(2).to_broadcast([128, NT, 16]), op=ALU.mult), RK, RK)
            D(lambda e: e.tensor_tensor(out=rt[1][:], in0=rt[1][:], in1=w2.unsqueeze(2).to_broadcast([128, NT, 16]), op=ALU.mult), RK, RK)
            D(lambda e: e.tensor_tensor(out=gate[:], in0=rt[0][:], in1=rt[1][:], op=ALU.add), RK, ["gate"])

            for t in range(NT):
                P.add("act", lambda e, t=t: e.activation(out=x_tm[:, t, :], in_=x_tm[:, t, :], func=AF.Copy, scale=ALPHA),
                      reads=[("x_tm", t)], writes=[("x_tm", t)])

            def down_unit(ex, t):
                k = ex % 2
                pair = acc.next()
                for hf in range(2):
                    bq = pair[hf]

                    def f(e, bq=bq, hf=hf):
                        r = None
                        for fc in range(2):
                            r = e.matmul(banks[bq][:], lhsT=hT[k][:, fc, t * 128:(t + 1) * 128], rhs=wd[k][:, fc, hf * 512:(hf + 1) * 512], start=(fc == 0), stop=(fc == 1))
                        return r
                    P.add("pe", f, reads=[("hT", k, 0, t // 4), ("hT", k, 1, t // 4), ("wd", k)], writes=[bk(bq)])
                    if ex == 15:
                        P.add("dve", lambda e, bq=bq, hf=hf: e.scalar_tensor_tensor(
                            out=x_tm[:, t, hf * 512:(hf + 1) * 512], in0=banks[bq][:], scalar=gate[:, t, ex:ex + 1],
                            in1=x_tm[:, t, hf * 512:(hf + 1) * 512], op0=ALU.mult, op1=ALU.add, accum_out=sttA[:, t, hf, 0:1]),
                            reads=[bk(bq), "gate", ("x_tm", t)], writes=[("x_tm", t), ("stt", t, hf)])
                    else:
                        P.add("dve", lambda e, bq=bq, hf=hf: e.scalar_tensor_tensor(
                            out=x_tm[:, t, hf * 512:(hf + 1) * 512], in0=banks[bq][:], scalar=gate[:, t, ex:ex + 1],
                            in1=x_tm[:, t, hf * 512:(hf + 1) * 512], op0=ALU.mult, op1=ALU.add),
                            reads=[bk(bq), "gate", ("x_tm", t)], writes=[("x_tm", t)])

            for ex in range(0, 16):
                if ex + 2 < 16:
                    load_gu(ex + 2)
                if ex + 1 < 16:
                    load_d(ex + 1)
                for i in range(8):
                    if ex + 1 < 16:
                        gu_unit(ex + 1, i)
                    down_unit(ex, 2 * i)
                    down_unit(ex, 2 * i + 1)
            load_ln(li, 2)
            layer_norm_all(li, 2, make=(li != layers[-1]))
            barrier(MOE_KEYS, ATT_KEYS + ACC_KEYS)

        def v_padded(lhs_fn, wcols, wbuf=None, wkey="wbuf"):
            wbuf = wbuf if wbuf is not None else view(17408, [8, 384])
            for t in range(NT):
                b = rot.next()

                def f(e, b=b, t=t):
                    r = None
                    for kc in range(8):
                        r = e.matmul(banks[b][:, 0:128], lhsT=lhs_fn(kc, t), rhs=wbuf[:, kc, wcols], start=(kc == 0), stop=(kc == 7))
                    return r
                P.add("pe", f, reads=["xT", wkey], writes=[bk(b)])
                for ab in range(2):
                    P.add("act", lambda e, b=b, t=t, ab=ab: e.activation(out=vAB[:, t, ab, 64 * ab:64 * ab + 64], in_=banks[b][:, 64 * ab:64 * ab + 64], func=AF.Copy),
                          reads=[bk(b)], writes=["vAB"])

        def layer_win(li):
            w_in = W["win_w_in"][0]
            P.add("pool", lambda e: e.memset(vAB, 0.0), writes=["vAB"])
            for a in range(2):
                for g3 in range(3):
                    cc = a * 3 + g3
                    for ab in range(2):
                        h = (2 * a + ab) * 3 + g3
                        dma("sp", esk[64 * ab:64 * ab + 64, cc:cc + 1], W["win_sink"][0, h:h + 1].partition_broadcast(64), [], ["esk"])
            P.add("act", lambda e: e.activation(out=esk[:, 0:6], in_=esk[:, 0:6], func=AF.Exp), reads=["esk"], writes=["esk"])
            for a in range(2):
                dma("pool", masks, CT["c_winmask"][:, a * 18 * 128:(a + 1) * 18 * 128], [], ["masks"])
                wb_, wk_ = next_wb()
                load_w(wb_, wk_, w_in, 768 + a * 128, 128, col0=0)
                load_w(wb_, wk_, w_in, 1024 + a * 128, 128, col0=128)
                linear_fm(KT, "KT", lambda kc, wb_=wb_: wb_[:, kc, 0:128], wk_)
                v_padded(lambda kc, t: xT[:, kc, t * 128:(t + 1) * 128], slice(128, 256), wb_, wk_)
                for g3 in range(3):
                    cc = a * 3 + g3
                    hA, hB = (2 * a) * 3 + g3, (2 * a + 1) * 3 + g3
                    wb_, wk_ = next_wb()
                    load_w(wb_, wk_, w_in, hA * 64, 64, col0=256)
                    load_w(wb_, wk_, w_in, hB * 64, 64, col0=320)
                    linear_fm(QT, "QT", lambda kc, wb_=wb_: wb_[:, kc, 256:384], wk_)

                    def mfn(ab, di, g3=g3):
                        o = ((g3 * 2 + ab) * 3 + di) * 128
                        return masks[:, o:o + 128]
                    dbg("QT", QT, ["QT"])
                    dbg("KT", KT, ["KT"])
                    dbg("vAB", vAB, ["vAB"])
                    dbg("masks", masks, ["masks"])
                    dbg("esk", esk[:], ["esk"])
                    attn_banded(NT, mfn, 0.125, attn_epi_norm(cc, sink_col=cc))
                    dbg("pt0", pts[0], ["pt"])
            def rowmap(fc):
                if fc >= 6:
                    return [(0, fc * 128, 128)]
                a, g3 = fc // 3, fc % 3
                return [(0, ((2 * a) * 3 + g3) * 64, 64), (64, ((2 * a + 1) * 3 + g3) * 64, 64)]
            prefetch_wo(W["win_w_out"][0], 8, rowmap)
            do_mem(li, w_in, 1536 - 256, 6)
            out_proj_ln(li, W["win_w_out"][0], 8, rowmap)

        def layer_dil(li):
            w_in = W["dil_w_in"][0]
            P.add("pool", lambda e: e.memset(vAB, 0.0), writes=["vAB"])
            for c in range(4):
                dma("pool", masks, CT["c_dilmask"][:, c * 18 * 128:(c + 1) * 18 * 128], [], ["masks"])
                for p, dil in enumerate((1, 4, 16)):
                    ls = L // dil
                    xv = xT[:].rearrange("p k (s r) -> p k r s", r=dil)

                    def rhs_fn(kc, n, ls=ls, xv=xv):
                        if ls >= 512:
                            r, s0 = (512 * n) // ls, (512 * n) % ls
                            return xv[:, kc, r, s0:s0 + 512]
                        nr = 512 // ls
                        return xv[:, kc, n * nr:(n + 1) * nr, :]
                    qc = ((0 * 3 + p) * 8 + 2 * c) * 64
                    kc_ = ((1 * 3 + p) * 8 + 2 * c) * 64
                    vc = ((2 * 3 + p) * 8 + 2 * c) * 64
                    wb_, wk_ = next_wb()
                    load_w(wb_, wk_, w_in, qc, 128, col0=0)
                    load_w(wb_, wk_, w_in, kc_, 128, col0=128)
                    load_w(wb_, wk_, w_in, vc, 128, col0=256)
                    linear_fm(QT, "QT", lambda kc, wb_=wb_: wb_[:, kc, 0:128], wk_, perm=dil)
                    linear_fm(KT, "KT", lambda kc, wb_=wb_: wb_[:, kc, 128:256], wk_, perm=dil)
                    v_padded(lambda kc, t, xv=xv, ls=ls: xv[:, kc, (128 * t) // ls, (128 * t) % ls:(128 * t) % ls + 128], slice(256, 384), wb_, wk_)

                    def mfn(ab, di, p=p):
                        o = ((p * 2 + ab) * 3 + di) * 128
                        return masks[:, o:o + 128]

                    def epi(g, nb, db, p=p, dil=dil, ls=ls):
                        def vw(tl):
                            v = tl.rearrange("p (s r) -> p r s", r=dil)
                            if ls >= 512:
                                r, s0 = (512 * g) // ls, (512 * g) % ls
                                return v[:, r, s0:s0 + 512], None
                            nr = 512 // ls
                            return v[:, g * nr:(g + 1) * nr, :], nr
                        for bkk, tl, nm in ((nb, accN, "accN"), (db, accD, "accD")):
                            ov, nr = vw(tl)
                            src = banks[bkk][:] if nr is None else banks[bkk][:].rearrange("p (r s) -> p r s", r=nr)
                            if p == 0:
                                P.add("act", lambda e, ov=ov, src=src: e.activation(out=ov, in_=src, func=AF.Copy), reads=[bk(bkk)], writes=[nm])
                            else:
                                P.add("dve", lambda e, ov=ov, src=src: e.tensor_tensor(out=ov, in0=ov, in1=src, op=ALU.add), reads=[bk(bkk), nm], writes=[nm])
                    attn_banded(max(1, ls // 128), mfn, 0.125, epi)
                P.add("dve", lambda e: e.reciprocal(out=accD, in_=accD), reads=["accD"], writes=["accD"])
                P.add("dve", lambda e, c=c: e.tensor_tensor(out=mixT[:, c, :], in0=accN, in1=accD, op=ALU.mult), reads=["accN", "accD"], writes=[("mixT", c)])
            barrier(ACC_KEYS, ["wo", "wkv", "KTm", "vm", "rec"])
            prefetch_wo(W["dil_w_out"][0], 6)
            do_mem(li, w_in, 4864 - 256, 4)
            out_proj_ln(li, W["dil_w_out"][0], 6)
            barrier(["wo", "wkv", "KTm", "vm", "rec"], ACC_KEYS)

        PI = float(np.pi)
        S5 = {}

        def view_on(base, off, shape, dt=F32):
            n = int(np.prod(shape))
            if dt == BF16:
                v = base[:, off // 2:off // 2 + n]
            else:
                v = base[:, off // 2:off // 2 + 2 * n].bitcast(dt)
            if len(shape) == 1:
                return v
            names = " ".join(f"a{i}" for i in range(len(shape)))
            return v.rearrange(f"p ({names}) -> p {names}", **{f"a{i}": shape[i] for i in range(1, len(shape))})

        def s5_setup():
            MI_d = nc.dram_tensor("s5_MI", [128, 32, 128], BF16, kind="Internal").ap()
            MINP_d = nc.dram_tensor("s5_MINP", [128, 16, 2, 4, 128], BF16, kind="Internal").ap()
            MOUT_d = nc.dram_tensor("s5_MOUT", [128, 16, 2, 2, 128], BF16, kind="Internal").ap()
            MU_d = nc.dram_tensor("s5_MU", [128, 2, 3, 16, 8], F32, kind="Internal").ap()
            S5.update(MI=MI_d, MINP=MINP_d, MOUT=MOUT_d, MU=MU_d)
            xw = xT[:].rearrange("p a b -> p (a b)")
            mw = mixT[:].rearrange("p a b -> p (a b)")
            I32 = mybir.dt.int32

            def vx(off, shape, dt=F32):
                return view_on(xw, off, shape, dt)

            def vmx(off, shape, dt=F32):
                return view_on(mw, off, shape, dt)

            def vs(off, shape, dt=F32):
                return view_on(scr, off, shape, dt)
            PW = {("a", "re"): vx(0, [32, 16]), ("a", "im"): vx(2048, [32, 16]), ("d", "re"): vx(4096, [32, 16]), ("d", "im"): vx(6144, [32, 16])}
            bbre, bbim = vx(8192, [32, 16]), vx(10240, [32, 16])
            Cre, Cim = vx(12288, [32, 16]), vx(14336, [32, 16])
            Bre, Bim = vx(16384, [32, 16]), vx(18432, [32, 16])
            t1, t2, t3 = vx(20480, [32, 16]), vx(22528, [32, 16]), vx(24576, [32, 16])
            ki = vx(26624, [32, 16], I32)
            sm = vx(28672, [24, 32])
            evec = vx(31744, [2, 16])
            stg = vmx(0, [4, 128], BF16)
            L32 = vmx(1024, [2, 64])
            CL = vmx(1536, [2, 64])
            maskFB = vmx(2048, [2, 128])
            identf = vmx(3072, [128])
            D8 = vmx(3584, [8, 16])
            dcol = vmx(4096, [32])
            lsb = vmx(4224, [32])
            MUt = vmx(4352, [3, 16, 8])
            T2re = vmx(8192, [16, 128])
            T2im = vmx(16384, [16, 128])
            TX = vs(0, [32, 128])
            TY = vs(16384, [32, 128])
            MiA = vs(32768, [32, 128])
            K = "s5w"

            def DV(fn, rd=(), wr=()):
                P.add("dve", fn, reads=[K] + list(rd), writes=[K] + list(wr))

            def AC(fn, rd=(), wr=()):
                P.add("act", fn, reads=[K] + list(rd), writes=[K] + list(wr))
            dma("sp", evec, CT["c_evec"].rearrange("p (a b) -> p a b", a=2), [], [K])
            dma("sp", maskFB, CT["c_s5mask"].rearrange("p (a b) -> p a b", a=2), [], [K])
            dma("sp", identf, CT["c_ident"], [], [K])
            P.add("pool", lambda e: e.memset(stg, 0.0), reads=[K], writes=[K])
            for nm, dst in (("s5_c_re", Cre), ("s5_c_im", Cim)):
                src = W[nm][0].rearrange("g c p -> (g c) p")
                for i in range(4):
                    for du in range(2):
                        dma("sp", CL[:, du, :], src[i * 128:(i + 1) * 128, :], [], [K])
                    b = rot.next()
                    P.add("pe", lambda e, b=b: e.transpose(banks[b][:, 0:128], CL.rearrange("p a b -> p (a b)"), identf), reads=[K], writes=[bk(b)])
                    AC(lambda e, b=b, dst=dst, i=i: e.activation(out=dst[:, 8 * i:8 * i + 8, :], in_=banks[b][:, 0:128].rearrange("p (g c) -> p g c", c=16), func=AF.Copy), [bk(b)])
            for nm, dst in (("s5_b_re", Bre), ("s5_b_im", Bim)):
                for du in range(2):
                    dma("sp", dst[64 * du:64 * du + 64], W[nm][0].rearrange("g p c -> p g c"), [], [K])
            for tau in range(8):
                dma("sp", D8[0:32, tau, :], W["s5_d"][0].rearrange("(g c) -> g c", c=16), [], [K])
            b = rot.next()
            P.add("pe", lambda e, b=b: e.transpose(banks[b][:, 0:32], D8[0:32].rearrange("p a b -> p (a b)"), identf[0:32, 0:32]), reads=[K], writes=[bk(b)])
            AC(lambda e, b=b: e.activation(out=dcol, in_=banks[b][:, 0:32], func=AF.Copy), [bk(b)])

            def sml(i):
                return sm[:, i, :]
            lre, lim, st, a_, th, den, cr, ci, nr, u1, u2 = (sml(i) for i in range(11))

            def rr_sin(dst, src, shift):
                DV(lambda e: e.tensor_scalar(out=t3, in0=src, scalar1=1.0 / (2 * PI), scalar2=32.5 + shift / (2 * PI), op0=ALU.mult, op1=ALU.add))
                DV(lambda e: e.tensor_copy(out=ki, in_=t3))
                DV(lambda e: e.tensor_copy(out=t3, in_=ki))
                DV(lambda e: e.tensor_scalar(out=dst, in0=src, scalar1=64 * PI + shift, scalar2=None, op0=ALU.add))
                DV(lambda e: e.scalar_tensor_tensor(out=dst, in0=t3, scalar=-2 * PI, in1=dst, op0=ALU.mult, op1=ALU.add))
                DV(lambda e: e.tensor_scalar(out=t3, in0=dst, scalar1=-PI, scalar2=None, op0=ALU.is_lt))
                DV(lambda e: e.scalar_tensor_tensor(out=dst, in0=t3, scalar=2 * PI, in1=dst, op0=ALU.mult, op1=ALU.add))
                DV(lambda e: e.tensor_scalar(out=dst, in0=dst, scalar1=PI, scalar2=-PI, op0=ALU.min, op1=ALU.max))
                AC(lambda e: e.activation(out=dst, in_=dst, func=AF.Sin))

            def bc_g(x):
                return x.unsqueeze(2).to_broadcast([128, 32, 16])

            def table(TT, half, pr_, pi_, e0, Tr, Ti, kind, gsl=None):
                for j in range(8):
                    if gsl is None:
                        wr_ = pr_[half, :, e0 + j:e0 + j + 1].to_broadcast([64, 32, 16])
                        wi_ = pi_[half, :, e0 + j:e0 + j + 1].to_broadcast([64, 32, 16])
                        xr, xi, q1, q2 = Tr[half], Ti[half], t1[half], t2[half]
                    else:
                        wr_ = pr_[half, gsl, e0 + j:e0 + j + 1].to_broadcast([64, 16, 16])
                        wi_ = pi_[half, gsl, e0 + j:e0 + j + 1].to_broadcast([64, 16, 16])
                        xr, xi, q1, q2 = Tr[half, gsl, :], Ti[half, gsl, :], t1[half, 0:16, :], t2[half, 0:16, :]
                    o = TT[half, :, j, :]
                    if kind == "re":
                        DV(lambda e, wr_=wr_, xr=xr, q1=q1: e.tensor_tensor(out=q1, in0=wr_, in1=xr, op=ALU.mult))
                        DV(lambda e, wi_=wi_, xi=xi, q2=q2: e.tensor_tensor(out=q2, in0=wi_, in1=xi, op=ALU.mult))
                        DV(lambda e, o=o, q1=q1, q2=q2: e.tensor_tensor(out=o, in0=q1, in1=q2, op=ALU.subtract), wr=["s5T"])
                    else:
                        DV(lambda e, wr_=wr_, xi=xi, q1=q1: e.tensor_tensor(out=q1, in0=wr_, in1=xi, op=ALU.mult))
                        DV(lambda e, wi_=wi_, xr=xr, q2=q2: e.tensor_tensor(out=q2, in0=wi_, in1=xr, op=ALU.mult))
                        if kind == "im":
                            DV(lambda e, o=o, q1=q1, q2=q2: e.tensor_tensor(out=o, in0=q1, in1=q2, op=ALU.add), wr=["s5T"])
                        else:
                            DV(lambda e, o=o, q1=q1, q2=q2: e.scalar_tensor_tensor(out=o, in0=q1, scalar=-1.0, in1=q2, op0=ALU.mult, op1=ALU.subtract), wr=["s5T"])

            LO, HI = slice(0, 64), slice(64, 128)
            TX4 = TX.rearrange("p g (j c) -> p g j c", c=16)
            TY4 = TY.rearrange("p g (j c) -> p g j c", c=16)
            T2re4 = T2re.rearrange("p g (j c) -> p g j c", c=16)
            T2im4 = T2im.rearrange("p g (j c) -> p g j c", c=16)
            for d_ in range(2):
                for nm, dst in (("s5_lam_re", lre), ("s5_lam_im", lim)):
                    for du in range(2):
                        dma("sp", L32[0:32, du, :], W[nm][0, d_], [], [K])
                    b = rot.next()
                    P.add("pe", lambda e, b=b: e.transpose(banks[b][:, 0:32], L32[0:32].rearrange("p a b -> p (a b)"), identf[0:32, 0:32]), reads=[K], writes=[bk(b)])
                    AC(lambda e, b=b, dst=dst: e.activation(out=dst, in_=banks[b][:, 0:32], func=AF.Copy), [bk(b)])
                dma("sp", lsb, W["s5_log_step"][0, d_].partition_broadcast(128), [], [K])
                AC(lambda e: e.activation(out=st, in_=lsb, func=AF.Exp))
                DV(lambda e: e.tensor_tensor(out=a_, in0=lre, in1=st, op=ALU.mult))
                DV(lambda e: e.tensor_tensor(out=th, in0=lim, in1=st, op=ALU.mult))
                for oi, on in enumerate(("a", "d")):
                    ev = evec[:, oi, :].unsqueeze(1).to_broadcast([128, 32, 16])
                    pre, pim = PW[(on, "re")], PW[(on, "im")]
                    DV(lambda e, ev=ev: e.tensor_tensor(out=t1, in0=bc_g(a_), in1=ev, op=ALU.mult))
                    AC(lambda e: e.activation(out=t1, in_=t1, func=AF.Exp))
                    DV(lambda e, ev=ev: e.tensor_tensor(out=t2, in0=bc_g(th), in1=ev, op=ALU.mult))
                    rr_sin(pim, t2, 0.0)
                    rr_sin(pre, t2, PI / 2)
                    DV(lambda e, pim=pim: e.tensor_tensor(out=pim, in0=pim, in1=t1, op=ALU.mult))
                    DV(lambda e, pre=pre: e.tensor_tensor(out=pre, in0=pre, in1=t1, op=ALU.mult))
                pAr, pAi, pDr, pDi = PW[("a", "re")], PW[("a", "im")], PW[("d", "re")], PW[("d", "im")]
                DV(lambda e: e.tensor_scalar(out=nr, in0=pAr[:, :, 8], scalar1=-1.0, scalar2=None, op0=ALU.add))
                DV(lambda e: e.tensor_tensor(out=den, in0=lre, in1=lre, op=ALU.mult))
                DV(lambda e: e.tensor_tensor(out=u1, in0=lim, in1=lim, op=ALU.mult))
                DV(lambda e: e.tensor_tensor(out=den, in0=den, in1=u1, op=ALU.add))
                DV(lambda e: e.reciprocal(out=den, in_=den))
                DV(lambda e: e.tensor_tensor(out=u1, in0=nr, in1=lre, op=ALU.mult))
                DV(lambda e: e.tensor_tensor(out=u2, in0=pAi[:, :, 8], in1=lim, op=ALU.mult))
                DV(lambda e: e.tensor_tensor(out=cr, in0=u1, in1=u2, op=ALU.add))
                DV(lambda e: e.tensor_tensor(out=cr, in0=cr, in1=den, op=ALU.mult))
                DV(lambda e: e.tensor_tensor(out=u1, in0=pAi[:, :, 8], in1=lre, op=ALU.mult))
                DV(lambda e: e.tensor_tensor(out=u2, in0=nr, in1=lim, op=ALU.mult))
                DV(lambda e: e.tensor_tensor(out=ci, in0=u1, in1=u2, op=ALU.subtract))
                DV(lambda e: e.tensor_tensor(out=ci, in0=ci, in1=den, op=ALU.mult))
                DV(lambda e: e.tensor_tensor(out=t1, in0=bc_g(cr), in1=Bre, op=ALU.mult))
                DV(lambda e: e.tensor_tensor(out=t2, in0=bc_g(ci), in1=Bim, op=ALU.mult))
                DV(lambda e: e.tensor_tensor(out=bbre, in0=t1, in1=t2, op=ALU.subtract))
                DV(lambda e: e.tensor_tensor(out=t1, in0=bc_g(cr), in1=Bim, op=ALU.mult))
                DV(lambda e: e.tensor_tensor(out=t2, in0=bc_g(ci), in1=Bre, op=ALU.mult))
                DV(lambda e: e.tensor_tensor(out=bbim, in0=t1, in1=t2, op=ALU.add))
                if d_ == 0:
                    xs, ys = (pDr, pDi, 8), (pAr, pAi, 7)
                else:
                    xs, ys = (pAr, pAi, 7), (pDr, pDi, 8)
                table(TX4, LO, xs[0], xs[1], xs[2], bbre, bbim, "re")
                table(TX4, HI, xs[0], xs[1], xs[2], bbre, bbim, "im")
                table(TY4, LO, ys[0], ys[1], ys[2], Cre, Cim, "re")
                table(TY4, HI, ys[0], ys[1], ys[2], Cre, Cim, "imn")
                for g in range(32):
                    b = rot.next()
                    P.add("pe", lambda e, b=b, g=g: e.matmul(banks[b][:, 0:128], lhsT=TX[:, g, :], rhs=TY[:, g, :], start=True, stop=True), reads=["s5T", K], writes=[bk(b)])
                    if d_ == 0:
                        P.add("dve", lambda e, b=b, g=g: e.tensor_tensor(out=MiA[:, g, :], in0=banks[b][:, 0:128], in1=maskFB[:, 0, :], op=ALU.mult), reads=[bk(b), K], writes=[("s5Mi", g)])
                    else:
                        P.add("dve", lambda e, b=b, g=g: e.tensor_tensor(out=t1[:, 0:8, :].rearrange("p a b -> p (a b)"), in0=banks[b][:, 0:128], in1=maskFB[:, 1, :], op=ALU.mult), reads=[bk(b), K], writes=[K])
                        P.add("dve", lambda e, g=g: e.tensor_tensor(out=MiA[:, g, :], in0=MiA[:, g, :], in1=t1[:, 0:8, :].rearrange("p a b -> p (a b)"), op=ALU.add), reads=[K, ("s5Mi", g)], writes=[("s5Mi", g)])
                        P.add("dve", lambda e, g=g: e.scalar_tensor_tensor(out=MiA[:, g, :], in0=identf, scalar=dcol[:, g:g + 1], in1=MiA[:, g, :], op0=ALU.mult, op1=ALU.add), reads=[K, ("s5Mi", g)], writes=[("s5Mi", g)])
                if d_ == 1:
                    dma("pool", MI_d, MiA, ["s5Mi"], ["s5dram"])
                if d_ == 0:
                    table(TX4, LO, pDr, pDi, 1, bbre, bbim, "re")
                    table(TX4, HI, pDr, pDi, 1, bbre, bbim, "im")
                for pr in range(16):
                    for g2 in range(2):
                        g = 2 * pr + g2
                        b = rot.next()
                        P.add("pe", lambda e, b=b, g=g: e.transpose(banks[b][:, 0:128], TX[:, g, :], identf), reads=["s5T", K], writes=[bk(b)])
                        for ri in range(2):
                            P.add("act", lambda e, b=b, ri=ri, g2=g2: e.activation(out=stg[:, ri * 2 + g2, 64 * g2:64 * g2 + 64], in_=banks[b][:, 64 * ri:64 * ri + 64], func=AF.Copy),
                                  reads=[bk(b)], writes=["s5stg"])
                    dma("sp", MINP_d[:, pr, d_], stg, ["s5stg"], ["s5dram"])
                ms = (pAr, pAi, 8) if d_ == 0 else (pDr, pDi, 0)
                for half, gsl in ((LO, slice(0, 32, 2)), (HI, slice(1, 32, 2))):
                    table(T2re4, half, ms[0], ms[1], ms[2], Cre, Cim, "re", gsl=gsl)
                    table(T2im4, half, ms[0], ms[1], ms[2], Cre, Cim, "imn", gsl=gsl)
                dma("pool", MOUT_d[:, :, d_, 0, :], T2re, ["s5T", K], ["s5dram"])
                dma("pool", MOUT_d[:, :, d_, 1, :], T2im, ["s5T", K], ["s5dram"])
                for half, gsl in ((LO, slice(0, 32, 2)), (HI, slice(1, 32, 2))):
                    DV(lambda e, half=half, gsl=gsl: e.tensor_copy(out=MUt[half, 0, :, 0], in_=pAr[half, gsl, 15]))
                    DV(lambda e, half=half, gsl=gsl: e.tensor_copy(out=MUt[half, 1, :, 0], in_=pAi[half, gsl, 15]))
                for j in range(7):
                    r0, i0, r1, i1 = MUt[:, 0, :, j], MUt[:, 1, :, j], MUt[:, 0, :, j + 1], MUt[:, 1, :, j + 1]
                    DV(lambda e, r0=r0: e.tensor_tensor(out=u1[:, 0:16], in0=r0, in1=r0, op=ALU.mult))
                    DV(lambda e, i0=i0: e.tensor_tensor(out=u2[:, 0:16], in0=i0, in1=i0, op=ALU.mult))
                    DV(lambda e, r1=r1: e.tensor_tensor(out=r1, in0=u1[:, 0:16], in1=u2[:, 0:16], op=ALU.subtract))
                    DV(lambda e, r0=r0, i0=i0: e.tensor_tensor(out=u1[:, 0:16], in0=r0, in1=i0, op=ALU.mult))
                    DV(lambda e, i1=i1: e.tensor_scalar(out=i1, in0=u1[:, 0:16], scalar1=2.0, scalar2=None, op0=ALU.mult))
                DV(lambda e: e.tensor_scalar(out=MUt[:, 2, :, :], in0=MUt[:, 1, :, :], scalar1=-1.0, scalar2=None, op0=ALU.mult))
                dma("sp", MU_d[:, d_], MUt, [K], ["s5dram"])
            barrier([K, "s5T", "s5Mi", "s5stg", "xT", "mixT", "scr_all"], ["xT", "mixT"] + ATT_KEYS + ACC_KEYS)

        def layer_s5(li):
            w_in = W["s5_w_in"][0]
            S5K = ["s5sel", "s5uT", "s5U", "s5minp", "s5mout", "s5mi", "s5sbf", "s5mu"]
            barrier(ATT_KEYS + ACC_KEYS, S5K)
            sel = view(0, [64, 128])
            wbu = view(0, [8, 512])
            uT = view(16384, [4, L])
            Uall = view(32768, [32, 256])
            MINP_t = view(49152, [2, 4, 128])
            MOUT_t = view(51200, [2, 2, 128])
            MI_t = view(52224, [2, 128])
            Sbf = view(52736, [2, 2, 256])
            MU_t = view(54784, [2, 3, 8], F32)
            lnf = lnp[:].rearrange("p a b -> p (a b)")
            SSd = [[[lnf[:, 1024 * dd + (2 * ab + ri) * 256:1024 * dd + (2 * ab + ri + 1) * 256] for ri in range(2)] for ab in range(2)] for dd in range(2)]
            wglu = view(0, [4, 512])
            Gall = mixT[:, 4:8, :].rearrange("p a b -> p (a b)").rearrange("p (g k) -> p g k", k=256)
            gtmp = [rt[i][:].rearrange("p a b -> p (a b)") for i in range(3)]
            load_w(wbu, "s5sel", w_in, 0, 512)
            for fc in range(4):
                linear_fm(uT[:, fc, :], "s5uT", lambda kc, fc=fc: wbu[:, kc, fc * 128:(fc + 1) * 128], "s5sel", perm=8)
            dma("pool", sel, CT["c_sel"].rearrange("p (a b) -> p a b", b=128), [], ["s5sel"])
            for g in range(32):
                b = rot.next()

                def f(e, b=b, g=g):
                    r = None
                    uv = uT[:, g // 8, :].rearrange("p (t k) -> p t k", t=8)
                    for tau in range(8):
                        r = e.matmul(banks[b][:, 0:256], lhsT=sel[:, (g % 8) * 8 + tau, :], rhs=uv[:, tau, :], start=(tau == 0), stop=(tau == 7))
                    return r
                P.add("pe", f, reads=["s5sel", "s5uT"], writes=[bk(b)])
                P.add("act", lambda e, b=b, g=g: e.activation(out=Uall[:, g, :], in_=banks[b][:, 0:256], func=AF.Copy), reads=[bk(b)], writes=[("s5U", g)])
            dbg("s5uT", uT, ["s5uT"])
            dbg("s5U", Uall, ["s5U"])
            dma("pool", sel, CT["c_selT"].rearrange("p (a b) -> p a b", b=128), [], ["s5sel"])
            for pr in range(16):
                dma("sp", MINP_t, S5["MINP"][:, pr], ["s5dram"], ["s5minp"])
                dma("sp", MOUT_t, S5["MOUT"][:, pr], ["s5dram"], ["s5mout"])
                dma("sp", MI_t, S5["MI"][:, 2 * pr:2 * pr + 2, :], ["s5dram"], ["s5mi"])
                dma("sp", MU_t, S5["MU"][:, :, :, pr, :], ["s5dram"], ["s5mu"])
                for d_ in range(2):
                    b = rot.next()
                    SS = SSd[d_]

                    def f(e, b=b, d_=d_, pr=pr):
                        r = None
                        for ri in range(2):
                            for g2 in range(2):
                                r = e.matmul(banks[b][:, ri * 256:(ri + 1) * 256], lhsT=MINP_t[:, d_, ri * 2 + g2, :], rhs=Uall[:, 2 * pr + g2, :],
                                             start=(g2 == 0), stop=(g2 == 1))
                        return r
                    P.add("pe", f, reads=["s5minp", ("s5U", 2 * pr), ("s5U", 2 * pr + 1)], writes=[bk(b)])
                    for ri in range(2):
                        P.add("act", lambda e, b=b, ri=ri, SS=SS: e.activation(out=SS[0][ri], in_=banks[b][:, ri * 256:(ri + 1) * 256], func=AF.Copy),
                              reads=[bk(b)], writes=[("lnp", "S", d_, 0, ri)])
                cur = 0
                for j in range(8):
                    sh = 1 << j
                    nx = 1 - cur
                    for d_ in range(2):
                        SS = SSd[d_]
                        if d_ == 0:
                            dst, src, keep = slice(sh, 256), slice(0, 256 - sh), slice(0, sh)
                        else:
                            dst, src, keep = slice(0, 256 - sh), slice(sh, 256), slice(256 - sh, 256)
                        cr_, ci_, nr_, ni_ = SS[cur][0], SS[cur][1], SS[nx][0], SS[nx][1]
                        kc_r, kc_i, kn_r, kn_i = ("lnp", "S", d_, cur, 0), ("lnp", "S", d_, cur, 1), ("lnp", "S", d_, nx, 0), ("lnp", "S", d_, nx, 1)
                        a_ = MU_t[:, d_, 0, j:j + 1]
                        b_ = MU_t[:, d_, 1, j:j + 1]
                        nb_ = MU_t[:, d_, 2, j:j + 1]
                        P.add("dve", lambda e, nr_=nr_, cr_=cr_, a_=a_, dst=dst, src=src: e.scalar_tensor_tensor(out=nr_[:, dst], in0=cr_[:, src], scalar=a_, in1=cr_[:, dst], op0=ALU.mult, op1=ALU.add),
                              reads=[kc_r, kc_r + ("k",), "s5mu"], writes=[kn_r])
                        P.add("dve", lambda e, ni_=ni_, ci_=ci_, a_=a_, dst=dst, src=src: e.scalar_tensor_tensor(out=ni_[:, dst], in0=ci_[:, src], scalar=a_, in1=ci_[:, dst], op0=ALU.mult, op1=ALU.add),
                              reads=[kc_i, kc_i + ("k",), "s5mu"], writes=[kn_i])
                        P.add("act", lambda e, nr_=nr_, cr_=cr_, keep=keep: e.activation(out=nr_[:, keep], in_=cr_[:, keep], func=AF.Copy), reads=[kc_r, kc_r + ("k",)], writes=[kn_r + ("k",)])
                        P.add("act", lambda e, ni_=ni_, ci_=ci_, keep=keep: e.activation(out=ni_[:, keep], in_=ci_[:, keep], func=AF.Copy), reads=[kc_i, kc_i + ("k",)], writes=[kn_i + ("k",)])
                    for d_ in range(2):
                        SS = SSd[d_]
                        if d_ == 0:
                            dst, src = slice(sh, 256), slice(0, 256 - sh)
                        else:
                            dst, src = slice(0, 256 - sh), slice(sh, 256)
                        cr_, ci_, nr_, ni_ = SS[cur][0], SS[cur][1], SS[nx][0], SS[nx][1]
                        kc_r, kc_i, kn_r, kn_i = ("lnp", "S", d_, cur, 0), ("lnp", "S", d_, cur, 1), ("lnp", "S", d_, nx, 0), ("lnp", "S", d_, nx, 1)
                        b_ = MU_t[:, d_, 1, j:j + 1]
                        nb_ = MU_t[:, d_, 2, j:j + 1]
                        P.add("dve", lambda e, nr_=nr_, ci_=ci_, nb_=nb_, dst=dst, src=src: e.scalar_tensor_tensor(out=nr_[:, dst], in0=ci_[:, src], scalar=nb_, in1=nr_[:, dst], op0=ALU.mult, op1=ALU.add),
                              reads=[kc_i, kc_i + ("k",), kn_r, "s5mu"], writes=[kn_r])
                        P.add("dve", lambda e, ni_=ni_, cr_=cr_, b_=b_, dst=dst, src=src: e.scalar_tensor_tensor(out=ni_[:, dst], in0=cr_[:, src], scalar=b_, in1=ni_[:, dst], op0=ALU.mult, op1=ALU.add),
                              reads=[kc_r, kc_r + ("k",), kn_i, "s5mu"], writes=[kn_i])
                    cur = nx
                for d_ in range(2):
                    for ri in range(2):
                        P.add("act", lambda e, ri=ri, d_=d_, cur=cur: e.activation(out=Sbf[:, d_, ri, :], in_=SSd[d_][cur][ri], func=AF.Copy),
                              reads=[("lnp", "S", d_, cur, ri), ("lnp", "S", d_, cur, ri, "k")], writes=[("s5sbf", d_, ri)])
                for g2 in range(2):
                    g = 2 * pr + g2
                    b = rot.next()
                    hp = slice(64 * g2, 64 * g2 + 64)

                    def f(e, b=b, g=g, g2=g2, hp=hp):
                        e.matmul(banks[b][:, 0:256], lhsT=MI_t[:, g2, :], rhs=Uall[:, g, :], start=True, stop=False)
                        e.matmul(banks[b][:, 1:256], lhsT=MOUT_t[hp, 0, 0, :], rhs=Sbf[hp, 0, 0, 0:255], start=False, stop=False)
                        e.matmul(banks[b][:, 1:256], lhsT=MOUT_t[hp, 0, 1, :], rhs=Sbf[hp, 0, 1, 0:255], start=False, stop=False)
                        e.matmul(banks[b][:, 0:255], lhsT=MOUT_t[hp, 1, 0, :], rhs=Sbf[hp, 1, 0, 1:256], start=False, stop=False)
                        return e.matmul(banks[b][:, 0:255], lhsT=MOUT_t[hp, 1, 1, :], rhs=Sbf[hp, 1, 1, 1:256], start=False, stop=True)
                    P.add("pe", f, reads=["s5mi", "s5mout", "s5sbf", ("s5U", g)], writes=[bk(b)])
                    yb = banks[b][:, 0:256]
                    gt = gtmp[g2]
                    gk = ("rt", g2)
                    P.add("act", lambda e, yb=yb, gt=gt: e.activation(out=gt, in_=yb, func=AF.Square), reads=[bk(b)], writes=[gk])
                    P.add("dve", lambda e, gt=gt: e.tensor_scalar(out=gt, in0=gt, scalar1=0.044715, scalar2=1.0, op0=ALU.mult, op1=ALU.add), reads=[gk], writes=[gk])
                    P.add("dve", lambda e, yb=yb, gt=gt: e.tensor_tensor(out=gt, in0=gt, in1=yb, op=ALU.mult), reads=[gk, bk(b)], writes=[gk])
                    P.add("act", lambda e, gt=gt: e.activation(out=gt, in_=gt, func=AF.Sigmoid, scale=1.5957691216057308), reads=[gk], writes=[gk])
                    P.add("dve", lambda e, yb=yb, gt=gt, g=g: e.tensor_tensor(out=Gall[:, g, :], in0=gt, in1=yb, op=ALU.mult), reads=[gk, bk(b)], writes=[("mixT", "s5G", g)])
            dbg("s5G", Gall, ["mixT"])
            dbg("s5Sbf", Sbf, ["s5sbf"])
            gT = uT
            for cc in range(4):
                for tp in range(8):
                    b = rot.next()

                    def f(e, b=b, cc=cc, tp=tp):
                        r = None
                        for j in range(8):
                            r = e.matmul(banks[b][:, 0:256], lhsT=sel[:, j * 8 + tp, :], rhs=Gall[:, 8 * cc + j, :], start=(j == 0), stop=(j == 7))
                        return r
                    P.add("pe", f, reads=["s5sel"] + [("mixT", "s5G", 8 * cc + j) for j in range(8)], writes=[bk(b)])
                    P.add("act", lambda e, b=b, cc=cc, tp=tp: e.activation(out=gT[:, cc, :].rearrange("p (k t) -> p t k", t=8)[:, tp, :], in_=banks[b][:, 0:256], func=AF.Copy),
                          reads=[bk(b)], writes=["s5uT"])
            dbg("s5gT", gT, ["s5uT"])
            dma("pool", wglu, W["s5_w_glu"][0].rearrange("(kc p) n -> p kc n", p=128), [], ["s5sel"])
            for fc in range(4):
                dma("sp", esk[:, fc:fc + 1], W["s5_b_glu"][0, fc * 128:(fc + 1) * 128].rearrange("(p o) -> p o", o=1), [], ["esk"])
            sgt = Uall.rearrange("p g k -> p (g k)")
            for fc in range(4):
                for n in range(4):
                    b = rot.next()

                    def f(e, b=b, fc=fc, n=n):
                        r = None
                        for kc in range(4):
                            r = e.matmul(banks[b][:], lhsT=wglu[:, kc, fc * 128:(fc + 1) * 128], rhs=gT[:, kc, n * 512:(n + 1) * 512], start=(kc == 0), stop=(kc == 3))
                        return r
                    P.add("pe", f, reads=["s5sel", "s5uT"], writes=[bk(b)])
                    si = (fc * 4 + n) % 4
                    sg = sgt[:, si * 512:(si + 1) * 512]
                    P.add("act", lambda e, b=b, sg=sg, fc=fc: e.activation(out=sg, in_=banks[b][:], func=AF.Sigmoid, bias=esk[:, fc:fc + 1]), reads=[bk(b), "esk"], writes=[("s5U", "sg", si)])
                    P.add("dve", lambda e, sg=sg, fc=fc, n=n: e.tensor_tensor(out=mixT[:, fc, n * 512:(n + 1) * 512], in0=gT[:, fc, n * 512:(n + 1) * 512], in1=sg, op=ALU.mult),
                          reads=[("s5U", "sg", si), "s5uT"], writes=[("mixT", fc, n)])
            barrier(S5K + ["mixT", "lnp", "rt", "esk"], ATT_KEYS + ACC_KEYS + ["mixT", "lnp", "rt", "esk"])
            prefetch_wo(W["s5_w_out"][0], 6)
            do_mem(li, w_in, 512, 4)
            out_proj_ln(li, W["s5_w_out"][0], 6)


        def layer_mlstm(li):
            w_in = W["mlstm_w_in"][0]
            do_mem(li, w_in, 3344 - 256, 6)
            MK = ["m_qT", "m_kT", "m_ktm", "m_vaug", "m_vt", "m_hacc", "m_Z", "m_Cbf", "m_tmp", "m_Sm", "m_wb", "m_otok",
                  "m_osig", "m_hn", "m_G", "m_lfn", "m_cum", "m_E", "m_ng", "m_sc", "m_sc2"]
            barrier(ATT_KEYS + ACC_KEYS, MK)
            qT = view(0, [2, L])
            kT = view(8192, [2, L])
            k_tm = view(16384, [NT, 192])
            v_aug = view(22528, [NT, 194])
            vt = view(28736, [NT, 194])
            hacc = view(34944, [NT, 192])
            Z = view(41088, [2, 194], F32)
            Cbf = view(42640, [2, 194])
            tmp = view(43424, [194], F32)
            Sm = [view(44224 + 256 * i, [128]) for i in range(2)]
            wb = view(44736, [8, 192])
            otok = view(47808, [256])
            osig = view(48320, [192], F32)
            hn = view(49088, [192], F32)
            Gtm = view(49856, [NT, 16], F32)
            lfn = view(50880, [NT, 8], F32)
            cumS = view(51392, [NT, 16], F32)
            EB = view(52416, [NT, 8], F32)
            EC = view(52928, [NT, 8], F32)
            EG = view(53440, [NT, 8], F32)
            ng = view(53952, [192], F32)
            gb = view(54720, [16], F32)
            sc = view(54784, [8], F32)
            tri = view(54816, [3, 128], F32)
            dma("sp", tri, CT["c_tri"].rearrange("p (a b) -> p a b", a=3), [], ["m_sc"])
            dma("sp", gb, W["mlstm_gate_b"][0].rearrange("a b -> (a b)").partition_broadcast(128), [], ["m_G"])
            load_w(wb, "m_wb", w_in, 3072, 16)
            b = rot.next()

            def f(e):
                r = None
                for t in range(NT):
                    for kc in range(8):
                        r = e.matmul(banks[b][:, t * 16:(t + 1) * 16], lhsT=xT[:, kc, t * 128:(t + 1) * 128], rhs=wb[:, kc, 0:16], start=(kc == 0), stop=(kc == 7))
                return r
            P.add("pe", f, reads=["xT", "m_wb"], writes=[bk(b)])
            P.add("dve", lambda e: e.tensor_tensor(out=Gtm, in0=banks[b][:, 0:256].rearrange("p (t c) -> p t c", c=16),
                                                   in1=gb.unsqueeze(1).to_broadcast([128, NT, 16]), op=ALU.add), reads=[bk(b), "m_G"], writes=["m_G"])
            P.add("act", lambda e: e.activation(out=lfn[:, :, 0:4], in_=Gtm[:, :, 4:8], func=AF.Exp, scale=-1.0), reads=["m_G"], writes=["m_lfn"])
            P.add("act", lambda e: e.activation(out=lfn[:, :, 4:8], in_=Gtm[:, :, 12:16], func=AF.Exp, scale=-1.0), reads=["m_G"], writes=["m_lfn"])
            P.add("act", lambda e: e.activation(out=lfn, in_=lfn, func=AF.Ln, bias=1.0), reads=["m_lfn"], writes=["m_lfn"])
            b2 = rot.next()

            def f2(e):
                r = None
                cv = banks[b2][:, 0:256].rearrange("p (t c) -> p t c", c=16)
                for t in range(NT):
                    e.matmul(cv[:, t, 0:4], lhsT=tri[:, 0, :], rhs=lfn[:, t, 0:4], start=True, stop=True)
                    e.matmul(cv[:, t, 4:8], lhsT=tri[:, 1, :], rhs=lfn[:, t, 4:8], start=True, stop=True)
                    r = e.matmul(cv[:, t, 8:16], lhsT=tri[:, 2, :], rhs=lfn[:, t, 0:8], start=True, stop=True)
                return r
            P.add("pe", f2, reads=["m_lfn", "m_sc"], writes=[bk(b2)])
            P.add("act", lambda e: e.activation(out=cumS, in_=banks[b2][:, 0:256].rearrange("p (t c) -> p t c", c=16), func=AF.Copy), reads=[bk(b2)], writes=["m_cum"])
            P.add("act", lambda e: e.activation(out=EB, in_=cumS[:, :, 0:8], func=AF.Exp, scale=-1.0), reads=["m_cum"], writes=[("m_E", 0)])
            P.add("act", lambda e: e.activation(out=EG, in_=cumS[:, :, 8:16], func=AF.Exp, scale=-1.0), reads=["m_cum"], writes=[("m_E", 1)])
            P.add("dve", lambda e: e.tensor_tensor(out=EC[:, :, 0:4], in0=Gtm[:, :, 0:4], in1=cumS[:, :, 0:4], op=ALU.add), reads=["m_G", "m_cum"], writes=[("m_E", 2)])
            P.add("dve", lambda e: e.tensor_tensor(out=EC[:, :, 4:8], in0=Gtm[:, :, 8:12], in1=cumS[:, :, 4:8], op=ALU.add), reads=["m_G", "m_cum"], writes=[("m_E", 2)])
            P.add("act", lambda e: e.activation(out=EC, in_=EC, func=AF.Exp), reads=[("m_E", 2)], writes=[("m_E", 2)])
            P.add("pool", lambda e: e.memset(otok, 0.0), writes=["m_otok"])
            for h in range(4):
                load_w(wb, "m_wb", w_in, h * 192, 192)
                linear_fm(qT[:, 0, :], "m_qT", lambda kc: wb[:, kc, 0:128], "m_wb")
                linear_fm(qT[0:64, 1, :], "m_qT", lambda kc: wb[:, kc, 128:192], "m_wb", M=64)
                load_w(wb, "m_wb", w_in, 768 + h * 192, 192)
                ksc = 192.0 ** -0.5
                linear_fm(kT[:, 0, :], "m_kT", lambda kc: wb[:, kc, 0:128], "m_wb", scale=ksc)
                linear_fm(kT[0:64, 1, :], "m_kT", lambda kc: wb[:, kc, 128:192], "m_wb", M=64, scale=ksc)

                def tokmaj(dst_fn, dkey, func=AF.Copy, scale=1.0):
                    for t in range(NT):
                        bb = rot.next()

                        def g(e, bb=bb, t=t):
                            r = None
                            for kc in range(8):
                                r = e.matmul(banks[bb][:, 0:192], lhsT=xT[:, kc, t * 128:(t + 1) * 128], rhs=wb[:, kc, 0:192], start=(kc == 0), stop=(kc == 7))
                            return r
                        P.add("pe", g, reads=["xT", "m_wb"], writes=[bk(bb)])
                        P.add("act", lambda e, bb=bb, t=t: e.activation(out=dst_fn(t), in_=banks[bb][:, 0:192], func=func, scale=scale), reads=[bk(bb)], writes=[dkey])
                tokmaj(lambda t: k_tm[:, t, :], "m_ktm", scale=ksc)
                load_w(wb, "m_wb", w_in, 1536 + h * 192, 192)
                tokmaj(lambda t: v_aug[:, t, 0:192], "m_vaug")
                P.add("pool", lambda e: e.memset(v_aug[:, :, 192:194], 1.0), writes=["m_vaug"])
                lnb = lnp[:].rearrange("p a b -> p (a b)")
                vts = [vt, lnb[:, 0:1552].bitcast(BF16).rearrange("p (t c) -> p t c", c=194)]
                Zs = [Z, lnb[:, 1552:1940].rearrange("p (a c) -> p a c", c=194)]
                Cbs = [Cbf, rt[0][:].rearrange("p a b -> p (a b)").bitcast(BF16)[:, 0:388].rearrange("p (a c) -> p a c", c=194)]
                tmps = [tmp, rt[1][:].rearrange("p a b -> p (a b)")[:, 0:194]]
                smf = rt[2][:].rearrange("p a b -> p (a b)").bitcast(BF16)
                Sms = [Sm, [smf[:, 0:128], smf[:, 128:256]]]
                scs = [sc, esk]
                P.add("pool", lambda e: e.memset(hacc, 0.0), writes=["m_hacc"])
                for d_ in range(2):
                    hd = d_ * 4 + h
                    P.add("dve", lambda e, hd=hd, d_=d_: e.tensor_tensor(out=vts[d_][:, :, 0:193], in0=v_aug[:, :, 0:193],
                                                                     in1=EC[:, :, hd:hd + 1].to_broadcast([128, NT, 193]), op=ALU.mult),
                          reads=["m_vaug", ("m_E", 2), "lnp"], writes=[("m_vt", d_), "lnp"] if d_ == 1 else [("m_vt", d_)])
                    P.add("pool", lambda e, d_=d_: e.memset(Zs[d_], 0.0), reads=["lnp"], writes=[("m_Z", d_), "lnp"] if d_ == 1 else [("m_Z", d_)])
                    P.add("pool", lambda e, d_=d_: e.memset(Cbs[d_], 0.0), reads=["rt"], writes=[("m_Cbf", d_), "rt"] if d_ == 1 else [("m_Cbf", d_)])
                kprevs = [0, NT - 1]
                for step in range(NT):
                    for d_ in range(2):
                        hd = d_ * 4 + h
                        k = step if d_ == 0 else NT - 1 - step
                        kprev = kprevs[d_]
                        vt_, Z_, Cb_, tmp_, sc_ = vts[d_], Zs[d_], Cbs[d_], tmps[d_], scs[d_]
                        ts = slice(k * 128, (k + 1) * 128)
                        sb_ = rot.next()
                        ob, ub = acc.next()

                        def fs(e, sb_=sb_, ts=ts, ub=ub, k=k, vt_=vt_):
                            e.matmul(banks[sb_][:, 0:128], lhsT=kT[:, 0, ts], rhs=qT[:, 0, ts], start=True, stop=False)
                            e.matmul(banks[sb_][:, 0:128], lhsT=kT[0:64, 1, ts], rhs=qT[0:64, 1, ts], start=False, stop=True)
                            e.matmul(banks[ub][:, 0:193], lhsT=k_tm[:, k, 0:128], rhs=vt_[:, k, 0:193], start=True, stop=True)
                            return e.matmul(banks[ub][0:64, 256:449], lhsT=k_tm[:, k, 128:192], rhs=vt_[:, k, 0:193], start=True, stop=True)
                        P.add("pe", fs, reads=["m_qT", "m_kT", "m_ktm", ("m_vt", d_)], writes=[bk(sb_), bk(ub)])
                        si = step % 2
                        Sm_ = Sms[d_][si]
                        P.add("dve", lambda e, sb_=sb_, Sm_=Sm_, d_=d_: e.tensor_tensor(out=Sm_, in0=banks[sb_][:, 0:128], in1=tri[:, d_, :], op=ALU.mult),
                              reads=[bk(sb_), "m_sc"], writes=[("m_Sm", d_, si)])

                        def fo(e, ob=ob, Sm_=Sm_, k=k, ts=ts, vt_=vt_, Cb_=Cb_):
                            e.matmul(banks[ob][:, 0:193], lhsT=Sm_, rhs=vt_[:, k, 0:193], start=True, stop=False)
                            e.matmul(banks[ob][:, 0:193], lhsT=qT[:, 0, ts], rhs=Cb_[:, 0, 0:193], start=False, stop=False)
                            return e.matmul(banks[ob][:, 0:193], lhsT=qT[0:64, 1, ts], rhs=Cb_[0:64, 1, 0:193], start=False, stop=True)
                        P.add("pe", fo, reads=[("m_Sm", d_, si), ("m_vt", d_), "m_qT", ("m_Cbf", d_)], writes=[bk(ob)])
                        P.add("dve", lambda e, ub=ub, kprev=kprev, hd=hd, Z_=Z_: e.scalar_tensor_tensor(out=Z_[:, 0, 0:193], in0=Z_[:, 0, 0:193], scalar=EG[:, kprev, hd:hd + 1],
                                                                                                  in1=banks[ub][:, 0:193], op0=ALU.mult, op1=ALU.add),
                              reads=[bk(ub), ("m_E", 1), ("m_Z", d_, 0)], writes=[("m_Z", d_, 0)])
                        P.add("dve", lambda e, ub=ub, kprev=kprev, hd=hd, Z_=Z_: e.scalar_tensor_tensor(out=Z_[0:64, 1, 0:193], in0=Z_[0:64, 1, 0:193], scalar=EG[0:64, kprev, hd:hd + 1],
                                                                                                  in1=banks[ub][0:64, 256:449], op0=ALU.mult, op1=ALU.add),
                              reads=[bk(ub), ("m_E", 1), ("m_Z", d_, 1)], writes=[("m_Z", d_, 1)])
                        P.add("act", lambda e, k=k, hd=hd, Z_=Z_, Cb_=Cb_: e.activation(out=Cb_[:, 0, 0:193], in_=Z_[:, 0, 0:193], func=AF.Copy, scale=EG[:, k, hd:hd + 1]),
                              reads=[("m_Z", d_, 0), ("m_E", 1)], writes=[("m_Cbf", d_, 0)])
                        P.add("act", lambda e, k=k, hd=hd, Z_=Z_, Cb_=Cb_: e.activation(out=Cb_[0:64, 1, 0:193], in_=Z_[0:64, 1, 0:193], func=AF.Copy, scale=EG[0:64, k, hd:hd + 1]),
                              reads=[("m_Z", d_, 1), ("m_E", 1)], writes=[("m_Cbf", d_, 1)])
                        P.add("dve", lambda e, ob=ob, k=k, hd=hd, tmp_=tmp_: e.tensor_scalar(out=tmp_[:, 0:193], in0=banks[ob][:, 0:193], scalar1=EB[:, k, hd:hd + 1], scalar2=None, op0=ALU.mult),
                              reads=[bk(ob), ("m_E", 0)], writes=[("m_tmp", d_)])
                        P.add("act", lambda e, tmp_=tmp_, sc_=sc_: e.activation(out=sc_[:, 0:1], in_=tmp_[:, 192:193], func=AF.Abs), reads=[("m_tmp", d_)], writes=[("m_sc2", d_)])
                        P.add("dve", lambda e, sc_=sc_: e.tensor_scalar(out=sc_[:, 0:1], in0=sc_[:, 0:1], scalar1=1.0, scalar2=None, op0=ALU.max), reads=[("m_sc2", d_)], writes=[("m_sc2", d_)])
                        P.add("dve", lambda e, sc_=sc_: e.reciprocal(out=sc_[:, 1:2], in_=sc_[:, 0:1]), reads=[("m_sc2", d_)], writes=[("m_sc2", d_)])
                        P.add("dve", lambda e, k=k, tmp_=tmp_, sc_=sc_: e.scalar_tensor_tensor(out=hacc[:, k, :], in0=tmp_[:, 0:192], scalar=sc_[:, 1:2], in1=hacc[:, k, :], op0=ALU.mult, op1=ALU.add),
                              reads=[("m_tmp", d_), ("m_sc2", d_), ("m_hacc", k)], writes=[("m_hacc", k)])
                        kprevs[d_] = k
                barrier([("m_vt", 1), ("m_Z", 1), ("m_Cbf", 1), ("m_tmp", 1), ("m_Sm", 1), ("m_sc2", 1)], ["lnp", "rt", "esk"])
                load_w(wb, "m_wb", w_in, 2304 + h * 192, 192)
                dma("sp", ng, W["mlstm_norm_g"][0, h * 192:(h + 1) * 192].partition_broadcast(128), [], ["m_ng"])
                lnq = lnp[:].rearrange("p a b -> p (a b)")
                hns = [hn, lnq[:, 0:192]]
                osigs = [osig, lnq[:, 192:384]]
                otoks = [otok, lnq[:, 384:512].bitcast(BF16)]
                if h == 0:
                    P.add("pool", lambda e: e.memset(otoks[1], 0.0), reads=["lnp"], writes=["lnp", ("m_otok", 1)])
                for t in range(NT):
                    stt, mv = sttA[:, t], mvA[:, t]
                    P.add("dve", lambda e, t=t, stt=stt: e.bn_stats(out=stt[:, 0, :], in_=hacc[:, t, :]), reads=[("m_hacc", t)], writes=[("stt", t)])
                    P.add("dve", lambda e, stt=stt, mv=mv: e.bn_aggr(out=mv[:, 0:2], in_=stt[:, 0, :]), reads=[("stt", t)], writes=["mv"])
                P.add("dve", lambda e: e.tensor_scalar(out=mvA[:, :, 1], in0=mvA[:, :, 1], scalar1=EPS, scalar2=None, op0=ALU.add), reads=["mv"], writes=["mv"])
                P.add("act", lambda e: e.activation(out=mvA[:, :, 2], in_=mvA[:, :, 1], func=AF.Sqrt), reads=["mv"], writes=["mv"])
                P.add("dve", lambda e: e.reciprocal(out=mvA[:, :, 2], in_=mvA[:, :, 2]), reads=["mv"], writes=["mv"])

                def stage_o(t):
                    bb = rot.next()
                    q = t % 2

                    def g(e, bb=bb, t=t):
                        r = None
                        for kc in range(8):
                            r = e.matmul(banks[bb][:, 0:192], lhsT=xT[:, kc, t * 128:(t + 1) * 128], rhs=wb[:, kc, 0:192], start=(kc == 0), stop=(kc == 7))
                        return r
                    P.add("pe", g, reads=["xT", "m_wb"], writes=[bk(bb)])
                    P.add("act", lambda e, bb=bb, q=q: e.activation(out=osigs[q], in_=banks[bb][:, 0:192], func=AF.Sigmoid), reads=[bk(bb)], writes=[("m_osig", q)])

                def stage_h(t):
                    q = t % 2
                    hn_, os_, ot_ = hns[q], osigs[q], otoks[q]
                    P.add("dve", lambda e, t=t, hn_=hn_: e.scalar_tensor_tensor(out=hn_, in0=hacc[:, t, :], scalar=mvA[:, t, 0:1], in1=ng, op0=ALU.subtract, op1=ALU.mult),
                          reads=[("m_hacc", t), "mv", "m_ng"], writes=[("m_hn", q)])
                    P.add("dve", lambda e, t=t, hn_=hn_, os_=os_, ot_=ot_: e.scalar_tensor_tensor(out=ot_[:, 0:128], in0=hn_[:, 0:128], scalar=mvA[:, t, 2:3], in1=os_[:, 0:128], op0=ALU.mult, op1=ALU.mult),
                          reads=[("m_hn", q), ("m_osig", q), "mv"], writes=[("m_otok", q)])
                    ro = 128 + 64 * (h % 2)
                    P.add("dve", lambda e, t=t, hn_=hn_, os_=os_, ot_=ot_, ro=ro: e.scalar_tensor_tensor(out=ot_[:, ro:ro + 64], in0=hn_[:, 128:192], scalar=mvA[:, t, 2:3], in1=os_[:, 128:192], op0=ALU.mult, op1=ALU.mult),
                          reads=[("m_hn", q), ("m_osig", q), "mv"], writes=[("m_otok", q)])
                    tb = rot.next()
                    pv = banks[tb][:].bitcast(BF16)

                    def ft(e, pv=pv, ot_=ot_):
                        e.transpose(pv[:, 0:128], ot_[:, 0:128], ident[:])
                        return e.transpose(pv[:, 128:256], ot_[:, 128:256], ident[:])
                    P.add("pe", ft, reads=[("m_otok", q), "ident"], writes=[bk(tb)])
                    P.add("act", lambda e, pv=pv, t=t, h=h: e.activation(out=mixT[:, h, t * 128:(t + 1) * 128], in_=pv[:, 0:128], func=AF.Copy), reads=[bk(tb)], writes=[("mixT", h, t)])
                    p0 = 64 * (h % 2)
                    P.add("act", lambda e, pv=pv, t=t, h=h, p0=p0: e.activation(out=mixT[p0:p0 + 64, 4 + h // 2, t * 128:(t + 1) * 128], in_=pv[p0:p0 + 64, 128:256], func=AF.Copy),
                          reads=[bk(tb)], writes=[("mixT", 4 + h // 2, t, h % 2)])
                stage_o(0)
                for t in range(NT):
                    if t + 1 < NT:
                        stage_o(t + 1)
                    stage_h(t)
                barrier(["m_hn", "m_osig", "m_otok"], ["lnp"])
            barrier(MK, ATT_KEYS + ACC_KEYS)

            def rowmap(fc):
                if fc >= 6:
                    return [(0, fc * 128, 128)]
                if fc < 4:
                    return [(0, fc * 192, 128)]
                h0 = (fc - 4) * 2
                return [(0, h0 * 192 + 128, 64), (64, (h0 + 1) * 192 + 128, 64)]
            out_proj_ln(li, W["mlstm_w_out"][0], 8, rowmap)


        mixers = [layer_s5, layer_dil, layer_win, layer_mlstm]

        if 0 in layers:
            s5_setup()
            dbg("s5MI", S5["MI"], ["s5dram"])
            dbg("s5MINP", S5["MINP"], ["s5dram"])
            dbg("s5MOUT", S5["MOUT"], ["s5dram"])
            dbg("s5MU", S5["MU"], ["s5dram"])
        for s in range(nseq if "setup_only" not in debug else 0):
            for t4 in range(4):
                dma("sp", x_tm[:, 4 * t4:4 * t4 + 4, :], x_d[s, 512 * t4:512 * (t4 + 1), :].rearrange("(t p) d -> p t d", p=128),
                    [], [("x_tm", 4 * t4 + i) for i in range(4)])
            for j in range(2):
                dma("pool", xb[0][:], mem_d[s, j * 128:(j + 1) * 128, :], [], ["xb"])
                for hc in range(2):
                    transposes_to(xb[0][:, 512 * hc:512 * hc + 512], 4, lambda j=j, hc=hc: memT[:, 4 * hc:4 * hc + 4, j * 128:(j + 1) * 128], ("xb", hc), ("memT", j, hc))
            for t in range(NT):
                make_xT_tile(t)
            dbg("xT", xT[:], ["xT"])
            dbg("memT", memT[:], ["memT"])
            for li in layers:
                mixers[li % 4](li)
                dbg("mixT", mixT[:], ["mixT"])
                if do_moe:
                    moe(li)
            for t4 in range(4):
                dma("sp", out_d[s, 512 * t4:512 * (t4 + 1), :].rearrange("(t p) d -> p t d", p=128), x_tm[:, 4 * t4:4 * t4 + 4, :],
                    [("x_tm", 4 * t4 + i) for i in range(4)], [("out", s, t4)])
        P.add("sp", lambda e: None, reads=["out", "dbgout"])
        P.emit()
        print("ops per engine:", P.stats)
    return nc


_NC_CACHE = {}


def kernel(**inputs):
    nseq = 32 // NCORES
    if "nc" not in _NC_CACHE:
        _NC_CACHE["nc"] = build(nseq)
    nc = _NC_CACHE["nc"]
    consts = _consts()
    x = np.ascontiguousarray(inputs["x"], dtype=np.float32)
    mem = np.ascontiguousarray(inputs["mem"], dtype=np.float32)
    shared = {k: np.ascontiguousarray(inputs[k], dtype=np.float32) for k in WEIGHT_SHAPES}
    shared.update(consts)
    in_maps = []
    for c in range(NCORES):
        m = dict(shared)
        m["x"] = x[c * nseq:(c + 1) * nseq]
        m["mem"] = mem[c * nseq:(c + 1) * nseq]
        in_maps.append(m)
    res = run_bass_kernel_spmd(nc, in_maps, core_ids=list(range(NCORES)))
    return np.concatenate([r["out"] for r in res.results], axis=0)
```

```python
import math
import numpy as np
from contextlib import ExitStack
import concourse.bass as bass
import concourse.mybir as mybir
from concourse.bass_utils import run_bass_kernel_spmd

F32 = mybir.dt.float32
BF16 = mybir.dt.bfloat16
ALU = mybir.AluOpType
AF = mybir.ActivationFunctionType
AX = mybir.AxisListType

SAME_ENGINE_SYNC = {"pe": False, "act": True, "dve": True, "pool": True, "sp": True}
NDMA_SEMS = {"sp": 20, "pool": 12, "act": 4}

L = 2048
D = 1024
NT = 16
ALPHA = 8.0 ** 0.25
EPS = 1e-5
NCORES = 8


class _Op:
    __slots__ = ("eng", "fn", "dma", "deps", "sig", "sem", "val", "prewait")

    def __init__(self, eng, fn, dma):
        self.eng, self.fn, self.dma = eng, fn, dma
        self.deps = set()
        self.sig = False
        self.sem = None
        self.val = 0
        self.prewait = None


class Prog:
    def __init__(self, nc, es):
        self.nc, self.es = nc, es
        self.ops = []
        self.lastw = {}
        self.readers = {}
        self.n = 0

    def sb(self, shape, dt, name=None):
        self.n += 1
        return self.es.enter_context(self.nc.sbuf_tensor(name or f"sb{self.n}", list(shape), dt))

    def ps(self, shape, dt, name=None):
        self.n += 1
        return self.es.enter_context(self.nc.psum_tensor(name or f"ps{self.n}", list(shape), dt))

    @staticmethod
    def _norm(k):
        if isinstance(k, tuple):
            return (k[0], k[1:] if len(k) > 1 else None)
        return (k, None)

    def _conf(self, table, root, sub):
        d = table.get(root)
        if not d:
            return []
        if sub is None:
            return list(d.values())
        out = []
        if sub in d:
            out.append(d[sub])
        if None in d:
            out.append(d[None])
        return out

    def add(self, eng, fn, reads=(), writes=(), dma=False):
        op = _Op(eng, fn, dma)
        i = len(self.ops)
        for k in reads:
            root, sub = self._norm(k)
            for w in self._conf(self.lastw, root, sub):
                op.deps.add(w)
        for k in writes:
            root, sub = self._norm(k)
            for w in self._conf(self.lastw, root, sub):
                op.deps.add(w)
            for rl in self._conf(self.readers, root, sub):
                op.deps.update(rl)
        for k in reads:
            root, sub = self._norm(k)
            self.readers.setdefault(root, {}).setdefault(sub, []).append(i)
        for k in writes:
            root, sub = self._norm(k)
            d = self.lastw.setdefault(root, {})
            r = self.readers.setdefault(root, {})
            if sub is None:
                d.clear()
                r.clear()
            d[sub] = i
            r[sub] = []
        op.deps.discard(i)
        self.ops.append(op)
        return i

    def emit(self):
        nc, ops = self.nc, self.ops
        for i, op in enumerate(ops):
            keep = set()
            for j in op.deps:
                p = ops[j]
                if p.eng == op.eng and not p.dma and not op.dma and not SAME_ENGINE_SYNC[op.eng]:
                    continue
                p.sig = True
                keep.add(j)
            op.deps = keep
        for op in ops:
            if op.dma:
                op.sig = True
        engs = ["pe", "act", "dve", "pool", "sp"]
        esem = {e: self.es.enter_context(nc.semaphore(f"s_{e}")) for e in engs}
        dsems = {e: [self.es.enter_context(nc.semaphore(f"d_{e}{k}")) for k in range(n)]
                 for e, n in NDMA_SEMS.items()}
        cnt = {e: 0 for e in engs}
        dcnt = {e: [0] * n for e, n in NDMA_SEMS.items()}
        drr = {e: 0 for e in NDMA_SEMS}
        for op in ops:
            if op.dma:
                k = drr[op.eng]
                drr[op.eng] = (k + 1) % NDMA_SEMS[op.eng]
                op.sem = dsems[op.eng][k]
                if dcnt[op.eng][k] > 0:
                    op.prewait = (op.sem, dcnt[op.eng][k])
                dcnt[op.eng][k] += 16
                op.val = dcnt[op.eng][k]
            elif op.sig:
                cnt[op.eng] += 1
                op.sem = esem[op.eng]
                op.val = cnt[op.eng]
        per = {e: [] for e in engs}
        for op in ops:
            per[op.eng].append(op)
        self.stats = {e: len(per[e]) for e in engs}

        def run(e, engine):
            seen = {}
            for op in per[e]:
                waits = {}
                if op.prewait is not None:
                    waits[op.prewait[0]] = op.prewait[1]
                for j in op.deps:
                    p = ops[j]
                    if waits.get(p.sem, 0) < p.val:
                        waits[p.sem] = p.val
                for s, v in waits.items():
                    if seen.get(s, 0) >= v:
                        continue
                    seen[s] = v
                    engine.wait_ge(s, v)
                inst = op.fn(engine)
                if op.sig and inst is not None:
                    inst.then_inc(op.sem, 16 if op.dma else 1)

        with nc.Block() as block:
            @block.tensor
            def _(t):
                run("pe", t)

            @block.scalar
            def _(t):
                run("act", t)

            @block.vector
            def _(t):
                run("dve", t)

            @block.gpsimd
            def _(t):
                run("pool", t)

            @block.sync
            def _(t):
                run("sp", t)


class Rot:
    def __init__(self, items):
        self.items, self.i = list(items), 0

    def next(self):
        v = self.items[self.i % len(self.items)]
        self.i += 1
        return v


def _slopes(n):
    return 2.0 ** (-8.0 * np.arange(1, n + 1, dtype=np.float64) / n)


def _emask(radius, unit, slopes):
    kl = np.arange(128)[:, None]
    ql = np.arange(128)[None, :]
    out = np.zeros((len(slopes), 3, 128, 128), np.float32)
    for di, dlt in enumerate((-1, 0, 1)):
        delta = np.abs(kl - ql + 128 * dlt).astype(np.float64)
        for h, s in enumerate(slopes):
            m = np.exp(-s * unit * delta)
            m[delta > radius] = 0.0
            out[h, di] = m
    return out


def _consts():
    c = {}
    c["c_ident"] = np.eye(128, dtype=np.float32)
    dm = np.stack([_emask(64, dil, _slopes(8)) for dil in (1, 4, 16)])
    dm = dm.reshape(3, 4, 2, 3, 128, 128)
    c["c_dilmask"] = np.ascontiguousarray(dm.transpose(4, 1, 0, 2, 3, 5)).reshape(128, 72 * 128)
    wm = _emask(128, 1, _slopes(12))
    wm = wm.reshape(2, 2, 3, 3, 128, 128)
    c["c_winmask"] = np.ascontiguousarray(wm.transpose(4, 0, 2, 1, 3, 5)).reshape(128, 36 * 128)
    s_ = np.arange(128)[:, None]
    t_ = np.arange(128)[None, :]
    tri = np.stack([(s_ <= t_), (s_ >= t_), np.ones((128, 128), bool)], axis=1).astype(np.float32)
    c["c_tri"] = np.ascontiguousarray(tri).reshape(128, 3 * 128)
    sel = np.zeros((128, 8, 8, 128), np.float32)
    selT = np.zeros((128, 8, 8, 128), np.float32)
    for j in range(8):
        for tau in range(8):
            for cch in range(16):
                sel[16 * j + cch, j, tau, tau * 16 + cch] = 1.0
                selT[tau * 16 + cch, j, tau, 16 * j + cch] = 1.0
    c["c_sel"] = sel.reshape(128, 8192)
    c["c_selT"] = selT.reshape(128, 8192)
    ev = np.arange(-7, 9, dtype=np.float32)
    c["c_evec"] = np.ascontiguousarray(np.broadcast_to(np.concatenate([ev, ev[::-1]])[None, :], (128, 32))).astype(np.float32)
    tau = (np.arange(128) // 16)
    mF = (tau[None, :] >= tau[:, None]).astype(np.float32)
    mB = (tau[None, :] <= tau[:, None]).astype(np.float32)
    c["c_s5mask"] = np.ascontiguousarray(np.stack([mF, mB], axis=1)).reshape(128, 256)
    return c


CONST_SHAPES = {"c_sel": [128, 8192], "c_selT": [128, 8192], "c_evec": [128, 32], "c_s5mask": [128, 256], "c_tri": [128, 384], "c_ident": [128, 128], "c_dilmask": [128, 72 * 128], "c_winmask": [128, 36 * 128]}

WEIGHT_SHAPES = {
    "s5_w_in": [1, 1024, 768], "s5_lam_re": [1, 2, 32, 64], "s5_lam_im": [1, 2, 32, 64],
    "s5_log_step": [1, 2, 32], "s5_b_re": [1, 32, 64, 16], "s5_b_im": [1, 32, 64, 16],
    "s5_c_re": [1, 32, 16, 64], "s5_c_im": [1, 32, 16, 64], "s5_d": [1, 512],
    "s5_w_glu": [1, 512, 512], "s5_b_glu": [1, 512], "s5_w_out": [1, 768, 1024],
    "dil_w_in": [1, 1024, 4864], "dil_w_out": [1, 768, 1024],
    "win_w_in": [1, 1024, 1536], "win_sink": [1, 12], "win_w_out": [1, 1024, 1024],
    "mlstm_w_in": [1, 1024, 3344], "mlstm_gate_b": [1, 4, 4], "mlstm_norm_g": [1, 768],
    "mlstm_w_out": [1, 1024, 1024],
    "mem_w_kv": [4, 1024, 512], "ln1_g": [4, 1024], "ln1_b": [4, 1024], "ln2_g": [4, 1024],
    "ln2_b": [4, 1024], "router_g_w": [4, 1024, 4], "router_g_b": [4, 4],
    "router_e_w": [4, 1024, 16], "router_e_b": [4, 16],
    "moe_w_gate": [4, 16, 1024, 256], "moe_w_up": [4, 16, 1024, 256], "moe_w_down": [4, 16, 256, 1024],
}


def build(nseq, layers=(0, 1, 2, 3), do_moe=True, debug=()):
    nc = bass.Bass("TRN2", target_bir_lowering=False)
    W = {k: nc.dram_tensor(k, s, F32, kind="ExternalInput").ap() for k, s in WEIGHT_SHAPES.items()}
    CT = {k: nc.dram_tensor(k, s, F32, kind="ExternalInput").ap() for k, s in CONST_SHAPES.items()}
    x_d = nc.dram_tensor("x", [nseq, L, D], F32, kind="ExternalInput").ap()
    mem_d = nc.dram_tensor("mem", [nseq, 256, D], F32, kind="ExternalInput").ap()
    out_d = nc.dram_tensor("out", [nseq, L, D], F32, kind="ExternalOutput").ap()

    with ExitStack() as es:
        P = Prog(nc, es)
        x_tm = P.sb([128, NT, D], F32, "x_tm")
        xT = P.sb([128, 8, L], BF16, "xT")
        mixT = P.sb([128, 8, L], BF16, "mixT")
        memT = P.sb([128, 8, 256], BF16, "memT")
        ident = P.sb([128, 128], BF16, "ident")
        onesAB = P.sb([128, 2, 128], BF16, "onesAB")
        lnp = P.sb([128, 2, D], F32, "lnp")
        xb = [P.sb([128, D], BF16, f"xb{i}") for i in range(1)]
        sttA = P.sb([128, NT, 2, 6], F32, "sttA")
        mvA = P.sb([128, NT, 4], F32, "mvA")
        esk = P.sb([128, 8], F32, "esk")
        dummy = P.sb([128, 8], F32, "dummyt")
        SCR_BYTES = 56832
        scr = P.sb([128, SCR_BYTES // 2], BF16, "scr")

        def view(off, shape, dt=BF16):
            n = int(np.prod(shape))
            if dt == BF16:
                v = scr[:, off // 2:off // 2 + n]
            else:
                v = scr[:, off // 2:off // 2 + 2 * n].bitcast(F32)
            if len(shape) == 1:
                return v
            names = " ".join(f"a{i}" for i in range(len(shape)))
            return v.rearrange(f"p ({names}) -> p {names}", **{f"a{i}": shape[i] for i in range(1, len(shape))})

        wo = view(0, [8, 512])
        wkv = view(8192, [8, 256])
        KTm = view(12288, [2, 256])
        vm = view(13312, [2, 2, 2, 128])
        rec = [view(15360, [512], F32)]
        wbuf = view(17408, [8, 384])
        QT = view(23552, [L])
        KT = view(27648, [L])
        vAB = view(31744, [NT, 2, 128])
        pts = [view(39936 + 1024 * i, [512]) for i in range(8)] + [view(52736 + 1024 * i, [512]) for i in range(4)]
        masks = view(48128, [18 * 128])
        accN = view(0, [L], F32)
        accD = view(8192, [L], F32)
        ATT_KEYS = ["wo", "wkv", "KTm", "vm", "rec", "wbuf", "QT", "KT", "vAB", "pt", "masks"]
        ACC_KEYS = ["accN", "accD"]
        wg = [view(4096 * i, [8, 256]) for i in range(2)]
        wu = [view(8192 + 4096 * i, [8, 256]) for i in range(2)]
        wd = [view(16384 + 4096 * i, [2, 1024]) for i in range(2)]
        hT = [view(24576 + 8192 * i, [2, L]) for i in range(2)]
        sl = [view(40960 + 1024 * i, [512]) for i in range(2)]
        MOE_KEYS = ["wg", "wu", "wd", "hT", "sl"]
        banks = [P.ps([128, 512], F32, f"bank{i}") for i in range(8)]
        rot = Rot(range(4))
        acc = Rot([(4, 5), (6, 7)])
        ptr = Rot(range(12))
        recr = Rot(range(1))
        xbr = Rot(range(1))

        def barrier(rd, wr):
            P.add("dve", lambda e: e.memset(dummy[:, 0:1], 0.0), reads=list(rd), writes=list(wr) + ["dummy"])

        def bk(i):
            return ("bank", i)

        dbg_seen = set()

        def dbg(name, ap, keys):
            if name not in debug or name in dbg_seen:
                return
            dbg_seen.add(name)
            dt_ = nc.dram_tensor("dbg_" + name, list(ap.shape), ap.dtype, kind="ExternalOutput").ap()
            P.add("sp", lambda e: e.dma_start(out=dt_, in_=ap), reads=keys, writes=[("dbgout", name)], dma=True)

        def dma(eng, out, in_, reads, writes):
            P.add(eng, lambda e: e.dma_start(out=out, in_=in_), reads=reads, writes=writes, dma=True)

        def load_w(dst, key, src, c0, n, col0=0, nk=8):
            dma("pool", dst[:, 0:nk, col0:col0 + n],
                src[:, c0:c0 + n].rearrange("(kc p) n -> p kc n", p=128), [], [key])

        lnT = P.sb([128, 4, 2, 2, 8], F32, "lnT")
        lnstage = view(0, [128], F32)
        identf32 = view(512, [128], F32)
        dma("sp", identf32, CT["c_ident"], [], ["wo"])
        for li_ in range(4):
            for wi_, (gn, bn) in enumerate((("ln1_g", "ln1_b"), ("ln2_g", "ln2_b"))):
                for gi_, nm in enumerate((gn, bn)):
                    r0 = ((li_ * 2 + wi_) * 2 + gi_) * 8
                    dma("sp", lnstage[r0:r0 + 8, :], W[nm][li_].rearrange("(c p) -> c p", p=128), [], ["wo"])
        b_ = rot.next()
        P.add("pe", lambda e: e.transpose(banks[b_][:, 0:128], lnstage, identf32), reads=["wo"], writes=[bk(b_)])
        P.add("act", lambda e: e.activation(out=lnT[:].rearrange("p a b c d -> p (a b c d)"), in_=banks[b_][:, 0:128], func=AF.Copy), reads=[bk(b_)], writes=["lnT"])
        dma("pool", ident[:], CT["c_ident"], [], ["ident"])
        P.add("pool", lambda e: e.memset(onesAB[:], 0.0), writes=["onesAB"])
        P.add("pool", lambda e: e.memset(onesAB[:, 0, 0:64], 1.0), writes=["onesAB"])
        P.add("pool", lambda e: e.memset(onesAB[:, 1, 64:128], 1.0), writes=["onesAB"])

        def transposes_to(src_bf, nchunk, dst_fn, src_key, dst_key, affine=None, c0=0):
            b = rot.next()
            pv = banks[b][:].bitcast(BF16)

            def f(e):
                r = None
                for c in range(nchunk):
                    r = e.transpose(pv[:, c * 128:(c + 1) * 128], src_bf[:, c * 128:(c + 1) * 128], ident[:])
                return r
            P.add("pe", f, reads=[src_key, "ident"], writes=[bk(b)])
            if affine is None:
                src = pv[:, 0:nchunk * 128].rearrange("p (c t) -> p c t", t=128)
                P.add("act", lambda e: e.activation(out=dst_fn(), in_=src, func=AF.Copy), reads=[bk(b)], writes=[dst_key])
            else:
                gc, bc = affine
                for c in range(nchunk):
                    P.add("act", lambda e, c=c: e.activation(out=dst_fn()[:, c, :], in_=pv[:, c * 128:(c + 1) * 128], func=AF.Identity,
                                                           scale=gc[:, c0 + c:c0 + c + 1], bias=bc[:, c0 + c:c0 + c + 1]),
                          reads=[bk(b), "lnT"], writes=[tuple(dst_key) + (c,)])

        def make_xT_tile(t, norm=None):
            for hc in range(2):
                cs = slice(512 * hc, 512 * hc + 512)
                xh = xb[0][:, cs]
                if norm is None:
                    if hc == 0:
                        P.add("act", lambda e, xh=xh, cs=cs: e.activation(out=xh, in_=x_tm[:, t, cs], func=AF.Copy), reads=[("x_tm", t)], writes=[("xb", hc)])
                    else:
                        P.add("pool", lambda e, xh=xh, cs=cs: e.tensor_copy(out=xh, in_=x_tm[:, t, cs]), reads=[("x_tm", t)], writes=[("xb", hc)])
                    aff = None
                else:
                    P.add("act", lambda e, xh=xh, cs=cs: e.activation(out=xh, in_=x_tm[:, t, cs], func=AF.Identity, bias=norm[1], scale=norm[0]),
                          reads=[("x_tm", t), "mv"], writes=[("xb", hc)])
                    aff = (norm[2], norm[3])
                transposes_to(xh, 4, lambda hc=hc: xT[:, 4 * hc:4 * hc + 4, t * 128:(t + 1) * 128], ("xb", hc), ("xT", t, hc), affine=aff, c0=4 * hc)

        def linear_fm(dst, dst_key, w_lhsT, w_key, ntok=L, rhs_fn=None, rhs_key="xT", scale=1.0, nk=8, M=128, perm=1):
            for n in range(ntok // 512):
                b = rot.next()

                def f(e, n=n, b=b):
                    r = None
                    for kc in range(nk):
                        rhs = rhs_fn(kc, n) if rhs_fn else xT[:, kc, n * 512:(n + 1) * 512]
                        r = e.matmul(banks[b][0:M, :], lhsT=w_lhsT(kc), rhs=rhs, start=(kc == 0), stop=(kc == nk - 1))
                    return r
                rk = [rhs_key] if (rhs_fn or rhs_key != "xT") else [("xT", t_, hc_) for t_ in range(4 * n, 4 * n + 4) for hc_ in range(2)]
                P.add("pe", f, reads=[w_key] + rk, writes=[bk(b)])
                if perm > 1:
                    o = dst.rearrange("p (r s) -> p s r", r=perm)[:, n * 512 // perm:(n + 1) * 512 // perm, :]
                    src_ = banks[b][0:M, :].rearrange("p (s r) -> p s r", r=perm)
                else:
                    o = dst[:, n * 512:(n + 1) * 512]
                    src_ = banks[b][0:M, :]
                P.add("act", lambda e, o=o, b=b, src_=src_: e.activation(out=o, in_=src_, func=AF.Copy, scale=scale),
                      reads=[bk(b)], writes=[dst_key])

        def load_ln(li, which):
            for i, nm in enumerate((f"ln{which}_g", f"ln{which}_b")):
                dma("sp", lnp[:, i, :], W[nm][li].partition_broadcast(128), [], ["lnp"])

        def layer_norm_all(li, which, after4=None, make=True):
            gcol = lnT[:, li, which - 1, 0, :]
            bcol = lnT[:, li, which - 1, 1, :]
            for t in range(NT):
                for hf in range(2):
                    jb = rot.next()
                    P.add("act", lambda e, t=t, hf=hf, jb=jb: e.activation(out=banks[jb][:], in_=x_tm[:, t, hf * 512:(hf + 1) * 512], func=AF.Square,
                                                                         accum_out=sttA[:, t, hf, 1:2]),
                          reads=[("x_tm", t)], writes=[bk(jb), ("stt", t, hf, "q")])
            SV = ["stt", "mv"]
            P.add("dve", lambda e: e.tensor_tensor(out=mvA[:, :, 0], in0=sttA[:, :, 0, 0], in1=sttA[:, :, 1, 0], op=ALU.add), reads=SV, writes=["mv"])
            P.add("dve", lambda e: e.tensor_scalar(out=mvA[:, :, 0], in0=mvA[:, :, 0], scalar1=1.0 / 1024, scalar2=None, op0=ALU.mult), reads=SV, writes=["mv"])
            P.add("dve", lambda e: e.tensor_tensor(out=mvA[:, :, 1], in0=sttA[:, :, 0, 1], in1=sttA[:, :, 1, 1], op=ALU.add), reads=SV, writes=["mv"])
            P.add("dve", lambda e: e.tensor_scalar(out=mvA[:, :, 1], in0=mvA[:, :, 1], scalar1=1.0 / 1024, scalar2=EPS, op0=ALU.mult, op1=ALU.add), reads=SV, writes=["mv"])
            P.add("dve", lambda e: e.tensor_tensor(out=mvA[:, :, 2], in0=mvA[:, :, 0], in1=mvA[:, :, 0], op=ALU.mult), reads=SV, writes=["mv"])
            P.add("dve", lambda e: e.tensor_tensor(out=mvA[:, :, 1], in0=mvA[:, :, 1], in1=mvA[:, :, 2], op=ALU.subtract), reads=SV, writes=["mv"])
            P.add("act", lambda e: e.activation(out=mvA[:, :, 2], in_=mvA[:, :, 1], func=AF.Sqrt), reads=["mv"], writes=["mv"])
            P.add("dve", lambda e: e.reciprocal(out=mvA[:, :, 2], in_=mvA[:, :, 2]), reads=["mv"], writes=["mv"])
            P.add("dve", lambda e: e.scalar_tensor_tensor(out=mvA[:, :, 3], in0=mvA[:, :, 0], scalar=-1.0, in1=mvA[:, :, 2], op0=ALU.mult, op1=ALU.mult), reads=["mv"], writes=["mv"])
            for t in range(NT):
                P.add("act", lambda e, t=t: e.activation(out=x_tm[:, t, :], in_=x_tm[:, t, :], func=AF.Identity, bias=mvA[:, t, 3:4], scale=mvA[:, t, 2:3]),
                      reads=[("x_tm", t), "mv"], writes=[("x_tm", t)])
            for t in range(NT):
                P.add("dve", lambda e, t=t: e.tensor_tensor(out=x_tm[:, t, :], in0=x_tm[:, t, :], in1=lnp[:, 0, :], op=ALU.mult),
                      reads=[("x_tm", t), "lnp"], writes=[("x_tm", t)])
                P.add("dve", lambda e, t=t: e.tensor_tensor(out=x_tm[:, t, :], in0=x_tm[:, t, :], in1=lnp[:, 1, :], op=ALU.add),
                      reads=[("x_tm", t), "lnp"], writes=[("x_tm", t)])
            for t in range(NT if make else 0):
                make_xT_tile(t)
                if after4 is not None and t % 4 == 3:
                    after4(t // 4)

        def attn_epi_norm(chunk, sink_col=None):
            def epi(g, nb, db):
                r = recr.next()
                if sink_col is not None:
                    P.add("dve", lambda e: e.tensor_scalar(out=rec[r], in0=banks[db][:], scalar1=esk[:, sink_col:sink_col + 1], scalar2=None, op0=ALU.add),
                          reads=[bk(db), "esk"], writes=[("rec", r)])
                    P.add("dve", lambda e: e.reciprocal(out=rec[r], in_=rec[r]), reads=[("rec", r)], writes=[("rec", r)])
                else:
                    P.add("dve", lambda e: e.reciprocal(out=rec[r], in_=banks[db][:]), reads=[bk(db)], writes=[("rec", r)])
                P.add("dve", lambda e: e.tensor_tensor(out=mixT[:, chunk, g * 512:(g + 1) * 512], in0=banks[nb][:], in1=rec[r], op=ALU.mult),
                      reads=[bk(nb), ("rec", r)], writes=[("mixT", chunk, g)])
            return epi

        def attn_banded(nsub, mask_fn, scale, epi):
            def stageA(g):
                lst = []
                for ab in (0, 1):
                    hp0 = 64 * ab
                    for di, dlt in enumerate((-1, 0, 1)):
                        valid = [i for i in range(4 * g, 4 * g + 4)
                                 if 0 <= i + dlt < NT and (i // nsub) == ((i + dlt) // nsub)]
                        if not valid:
                            continue
                        b = rot.next()
                        pi = ptr.next()
                        c0, c1 = (valid[0] - 4 * g) * 128, (valid[-1] + 1 - 4 * g) * 128

                        def mm(e, valid=valid, b=b, hp0=hp0, dlt=dlt):
                            r = None
                            for i in valid:
                                r = e.matmul(banks[b][:, (i - 4 * g) * 128:(i - 4 * g + 1) * 128],
                                             lhsT=KT[hp0:hp0 + 64, (i + dlt) * 128:(i + dlt + 1) * 128],
                                             rhs=QT[hp0:hp0 + 64, i * 128:(i + 1) * 128], start=True, stop=True)
                            return r
                        P.add("pe", mm, reads=["QT", "KT"], writes=[bk(b)])
                        P.add("act", lambda e, b=b, pi=pi, c0=c0, c1=c1: e.activation(out=pts[pi][:, c0:c1], in_=banks[b][:, c0:c1], func=AF.Exp, scale=scale),
                              reads=[bk(b)], writes=[("pt", pi)])
                        m = mask_fn(ab, di)
                        nt_ = (c1 - c0) // 128
                        P.add("dve", lambda e, pi=pi, c0=c0, c1=c1, m=m, nt_=nt_: e.tensor_tensor(
                            out=pts[pi][:, c0:c1].rearrange("p (t q) -> p t q", q=128),
                            in0=pts[pi][:, c0:c1].rearrange("p (t q) -> p t q", q=128),
                            in1=m.unsqueeze(1).to_broadcast([128, nt_, 128]), op=ALU.mult),
                            reads=[("pt", pi), "masks"], writes=[("pt", pi)])
                        lst.append((ab, dlt, valid, pi))
                return lst

            def stageB(g, lst):
                nb, db = acc.next()

                def pv(e):
                    r = None
                    for i in range(4 * g, 4 * g + 4):
                        cs = slice((i - 4 * g) * 128, (i - 4 * g + 1) * 128)
                        con = [(ab, dlt, pi) for (ab, dlt, valid, pi) in lst if i in valid]
                        for idx, (ab, dlt, pi) in enumerate(con):
                            e.matmul(banks[nb][:, cs], lhsT=vAB[:, i + dlt, ab, :], rhs=pts[pi][:, cs],
                                     start=(idx == 0), stop=(idx == len(con) - 1))
                            r = e.matmul(banks[db][:, cs], lhsT=onesAB[:, ab, :], rhs=pts[pi][:, cs],
                                         start=(idx == 0), stop=(idx == len(con) - 1))
                    return r
                P.add("pe", pv, reads=["vAB", "onesAB"] + [("pt", pi) for (_, _, _, pi) in lst], writes=[bk(nb), bk(db)])
                epi(g, nb, db)

            prev = stageA(0)
            for g in range(4):
                nxt = stageA(g + 1) if g < 3 else None
                stageB(g, prev)
                prev = nxt

        def mem_kv(li):
            P.add("pool", lambda e: e.memset(vm, 0.0), writes=["vm"])
            load_w(wkv, "wkv", W["mem_w_kv"][li], 0, 256)
            for c in range(2):
                b = rot.next()

                def f(e, c=c, b=b):
                    r = None
                    for kc in range(8):
                        r = e.matmul(banks[b][:, 0:256], lhsT=wkv[:, kc, c * 128:(c + 1) * 128], rhs=memT[:, kc, :], start=(kc == 0), stop=(kc == 7))
                    return r
                P.add("pe", f, reads=["wkv", "memT"], writes=[bk(b)])
                P.add("act", lambda e, c=c, b=b: e.activation(out=KTm[:, c, :], in_=banks[b][:, 0:256], func=AF.Copy), reads=[bk(b)], writes=["KTm"])
            load_w(wkv, "wkv", W["mem_w_kv"][li], 256, 256)
            for j in range(2):
                b = rot.next()

                def f(e, j=j, b=b):
                    r = None
                    for kc in range(8):
                        r = e.matmul(banks[b][:, 0:256], lhsT=memT[:, kc, j * 128:(j + 1) * 128], rhs=wkv[:, kc, :], start=(kc == 0), stop=(kc == 7))
                    return r
                P.add("pe", f, reads=["wkv", "memT"], writes=[bk(b)])
                for c in range(2):
                    for ab in range(2):
                        P.add("act", lambda e, j=j, b=b, c=c, ab=ab: e.activation(
                            out=vm[:, j, c, ab, 64 * ab:64 * ab + 64], in_=banks[b][:, c * 128 + 64 * ab:c * 128 + 64 * ab + 64], func=AF.Copy),
                            reads=[bk(b)], writes=["vm"])

        def mem_attention_pair(c, out_chunk):
            epi = attn_epi_norm(out_chunk)

            def stageA(g):
                lst = []
                for ab in (0, 1):
                    hp0 = 64 * ab
                    for j in range(2):
                        b = rot.next()
                        pi = ptr.next()
                        P.add("pe", lambda e, b=b, hp0=hp0, j=j: e.matmul(
                            banks[b][:], lhsT=KTm[hp0:hp0 + 64, c, j * 128:(j + 1) * 128], rhs=QT[hp0:hp0 + 64, g * 512:(g + 1) * 512], start=True, stop=True),
                            reads=["QT", "KTm"], writes=[bk(b)])
                        P.add("act", lambda e, b=b, pi=pi: e.activation(out=pts[pi], in_=banks[b][:], func=AF.Exp, scale=0.125),
                              reads=[bk(b)], writes=[("pt", pi)])
                        lst.append((ab, j, pi))
                return lst

            def stageB(g, lst):
                nb, db = acc.next()

                def pv(e):
                    r = None
                    for idx, (ab, j, pi) in enumerate(lst):
                        e.matmul(banks[nb][:], lhsT=vm[:, j, c, ab, :], rhs=pts[pi], start=(idx == 0), stop=(idx == len(lst) - 1))
                        r = e.matmul(banks[db][:], lhsT=onesAB[:, ab, :], rhs=pts[pi], start=(idx == 0), stop=(idx == len(lst) - 1))
                    return r
                P.add("pe", pv, reads=["vm", "onesAB"] + [("pt", pi) for (_, _, pi) in lst], writes=[bk(nb), bk(db)])
                epi(g, nb, db)
            prev = stageA(0)
            for g in range(4):
                nxt = stageA(g + 1) if g < 3 else None
                stageB(g, prev)
                prev = nxt

        def do_mem(li, w_in, qcol0, mix_chunk0):
            mem_kv(li)
            for c in range(2):
                load_w(wbuf, "wbuf", w_in, qcol0 + c * 128, 128)
                linear_fm(QT, "QT", lambda kc: wbuf[:, kc, 0:128], "wbuf")
                mem_attention_pair(c, mix_chunk0 + c)

        wbuf2 = lnp[:].rearrange("p a b -> p (a b)")[:, 0:1536].bitcast(BF16).rearrange("p (k n) -> p k n", n=384)
        WB = {"i": 0}

        def next_wb():
            WB["i"] += 1
            return (wbuf, "wbuf") if WB["i"] % 2 else (wbuf2, "lnp")
        wo1 = view(27648, [8, 512])
        wos = [wo, wo1]
        WOP = {"done": False}

        def prefetch_wo(w_out, nin_chunks, rowmap=None):
            for hf in range(2):
                for fc in range(nin_chunks):
                    pieces = rowmap(fc) if rowmap else [(0, fc * 128, 128)]
                    for (p0, r0, n) in pieces:
                        dma("pool", wos[hf][p0:p0 + n, fc, :], w_out[r0:r0 + n, hf * 512:(hf + 1) * 512], [], [("wo", hf)] + (["KT", "vAB"] if hf == 1 else []))
            WOP["done"] = True

        def out_proj_ln(li, w_out, nin_chunks, rowmap=None):
            load_ln(li, 1)
            if not WOP["done"]:
                prefetch_wo(w_out, nin_chunks, rowmap)
            WOP["done"] = False
            for hf in range(2):
                wo = wos[hf]
                for t in range(NT):
                    b = acc.next()[t % 2]

                    def f(e, b=b, t=t, wo=wo):
                        r = None
                        for fc in range(nin_chunks):
                            r = e.matmul(banks[b][:], lhsT=mixT[:, fc, t * 128:(t + 1) * 128], rhs=wo[:, fc, :],
                                         start=(fc == 0), stop=(fc == nin_chunks - 1))
                        return r
                    P.add("pe", f, reads=["mixT", ("wo", hf)] + (["KT", "vAB"] if hf == 1 else []), writes=[bk(b)])
                    P.add("dve", lambda e, b=b, hf=hf, t=t: e.scalar_tensor_tensor(
                        out=x_tm[:, t, hf * 512:(hf + 1) * 512], in0=x_tm[:, t, hf * 512:(hf + 1) * 512], scalar=ALPHA,
                        in1=banks[b][:], op0=ALU.mult, op1=ALU.add, accum_out=sttA[:, t, hf, 0:1]), reads=[bk(b), ("x_tm", t)], writes=[("x_tm", t), ("stt", t, hf)])
            if do_moe:
                moe_prefetch(li)
                layer_norm_all(li, 1, after4=lambda n: (MOE["gu_unit"](0, n), MOE["gu_unit"](0, 4 + n)))
            else:
                layer_norm_all(li, 1)

        wr = P.sb([128, 8, 20], BF16, "wr")
        rb = P.sb([128, 20], F32, "rb")
        lg = P.sb([128, NT, 20], F32, "lg")
        gate = P.sb([128, NT, 16], F32, "gate")
        rt = [P.sb([128, NT, 16], F32, f"rt{i}") for i in range(3)]
        rs = P.sb([128, 8, NT], F32, "rs")

        MOE = {}

        def moe_prefetch(li):
            barrier(ATT_KEYS + ACC_KEYS, MOE_KEYS)

            def load_gu(ex):
                k = ex % 2
                dma("pool", wg[k], W["moe_w_gate"][li, ex].rearrange("(kc p) n -> p kc n", p=128), [], [("wg", k)])
                dma("pool", wu[k], W["moe_w_up"][li, ex].rearrange("(kc p) n -> p kc n", p=128), [], [("wu", k)])

            def load_d(ex):
                k = ex % 2
                dma("pool", wd[k], W["moe_w_down"][li, ex].rearrange("(kc p) n -> p kc n", p=128), [], [("wd", k)])

            def gu_unit(ex, i):
                k = ex % 2
                fc, n = i // 4, i % 4
                bg, bu = rot.next(), rot.next()

                def f(e):
                    r = None
                    for kc in range(8):
                        e.matmul(banks[bg][:], lhsT=wg[k][:, kc, fc * 128:(fc + 1) * 128], rhs=xT[:, kc, n * 512:(n + 1) * 512], start=(kc == 0), stop=(kc == 7))
                    for kc in range(8):
                        r = e.matmul(banks[bu][:], lhsT=wu[k][:, kc, fc * 128:(fc + 1) * 128], rhs=xT[:, kc, n * 512:(n + 1) * 512], start=(kc == 0), stop=(kc == 7))
                    return r
                P.add("pe", f, reads=[("xT", t_, hc_) for t_ in range(4 * n, 4 * n + 4) for hc_ in range(2)] + [("wg", k), ("wu", k)], writes=[bk(bg), bk(bu)])
                si = i % 2
                P.add("act", lambda e: e.activation(out=sl[si], in_=banks[bg][:], func=AF.Silu), reads=[bk(bg)], writes=[("sl", si)])
                P.add("dve", lambda e: e.tensor_tensor(out=hT[k][:, fc, n * 512:(n + 1) * 512], in0=banks[bu][:], in1=sl[si], op=ALU.mult),
                      reads=[bk(bu), ("sl", si)], writes=[("hT", k, fc, n)])
            MOE.update(load_gu=load_gu, load_d=load_d, gu_unit=gu_unit)
            load_gu(0)
            load_d(0)
            load_gu(1)

        def moe(li):
            load_gu, load_d, gu_unit = MOE["load_gu"], MOE["load_d"], MOE["gu_unit"]
            load_w(wr, "wr", W["router_g_w"][li], 0, 4, col0=0)
            load_w(wr, "wr", W["router_e_w"][li], 0, 16, col0=4)
            dma("sp", rb[:, 0:4], W["router_g_b"][li].partition_broadcast(128), [], ["rb"])
            dma("sp", rb[:, 4:20], W["router_e_b"][li].partition_broadcast(128), [], ["rb"])
            b = rot.next()

            def f(e):
                r = None
                for t in range(NT):
                    for kc in range(8):
                        r = e.matmul(banks[b][:, t * 20:(t + 1) * 20], lhsT=xT[:, kc, t * 128:(t + 1) * 128], rhs=wr[:, kc, :], start=(kc == 0), stop=(kc == 7))
                return r
            P.add("pe", f, reads=["xT", "wr"], writes=[bk(b)])

            def D(fn, rd, wr):
                P.add("dve", fn, reads=rd, writes=wr)
            gmax, gsum, m1, m2, dd, w1, w2 = (rs[:, i, :] for i in range(7))
            lv = banks[b][:, 0:NT * 20].rearrange("p (t c) -> p t c", c=20)
            gl = lg[:, :, 0:4]
            r0s, r1s = rt[0][:, :, 0:4], rt[1][:, :, 0:4]
            elm = rt[2][:].rearrange("p t (g x) -> p t g x", x=4)
            RK = ["lg", "rs", "rt"]
            D(lambda e: e.tensor_tensor(out=lg[:], in0=lv, in1=rb[:].unsqueeze(1).to_broadcast([128, NT, 20]), op=ALU.add), [bk(b), "rb"], RK)
            D(lambda e: e.tensor_reduce(out=gmax, in_=gl, axis=AX.X, op=ALU.max), RK, RK)
            D(lambda e: e.tensor_tensor(out=r0s, in0=gl, in1=gmax.unsqueeze(2).to_broadcast([128, NT, 4]), op=ALU.is_equal), RK, RK)
            D(lambda e: e.tensor_tensor(out=r1s, in0=gl, in1=gmax.unsqueeze(2).to_broadcast([128, NT, 4]), op=ALU.subtract), RK, RK)
            D(lambda e: e.tensor_scalar(out=r0s, in0=r0s, scalar1=-1.0, scalar2=1e30, op0=ALU.add, op1=ALU.mult), RK, RK)
            P.add("act", lambda e: e.activation(out=r1s, in_=r1s, func=AF.Exp), reads=RK, writes=RK)
            D(lambda e: e.tensor_reduce(out=gsum, in_=r1s, axis=AX.X, op=ALU.add), RK, RK)
            D(lambda e: e.reciprocal(out=gsum, in_=gsum), RK, RK)
            D(lambda e: e.tensor_tensor(out=elm, in0=lg[:, :, 4:20].rearrange("p t (g x) -> p t g x", x=4),
                                        in1=r0s.unsqueeze(3).to_broadcast([128, NT, 4, 4]), op=ALU.add), RK, RK)
            D(lambda e: e.tensor_reduce(out=m1, in_=rt[2][:], axis=AX.X, op=ALU.max), RK, RK)
            D(lambda e: e.tensor_tensor(out=rt[0][:], in0=rt[2][:], in1=m1.unsqueeze(2).to_broadcast([128, NT, 16]), op=ALU.is_equal), RK, RK)
            D(lambda e: e.scalar_tensor_tensor(out=rt[2][:], in0=rt[0][:], scalar=-1e30, in1=rt[2][:], op0=ALU.mult, op1=ALU.add), RK, RK)
            D(lambda e: e.tensor_reduce(out=m2, in_=rt[2][:], axis=AX.X, op=ALU.max), RK, RK)
            D(lambda e: e.tensor_tensor(out=rt[1][:], in0=rt[2][:], in1=m2.unsqueeze(2).to_broadcast([128, NT, 16]), op=ALU.is_equal), RK, RK)
            D(lambda e: e.tensor_tensor(out=dd, in0=m2, in1=m1, op=ALU.subtract), RK, RK)
            P.add("act", lambda e: e.activation(out=dd, in_=dd, func=AF.Exp), reads=RK, writes=RK)
            D(lambda e: e.tensor_scalar(out=w1, in0=dd, scalar1=1.0, scalar2=None, op0=ALU.add), RK, RK)
            D(lambda e: e.reciprocal(out=w1, in_=w1), RK, RK)
            D(lambda e: e.tensor_tensor(out=w1, in0=w1, in1=gsum, op=ALU.mult), RK, RK)
            D(lambda e: e.tensor_tensor(out=w2, in0=w1, in1=dd, op=ALU.mult), RK, RK)
            D(lambda e: e.tensor_tensor(out=rt[0][:], in0=rt[0][:], in1=w1.unsqueeze(2).to_broadcast([128, NT, 16]), op=ALU.mult), RK, RK)
            D(lambda e: e.tensor_tensor(out=rt[1][:], in0=rt[1][:], in1=w2.unsqueeze(2).to_broadcast([128, NT, 16]), op=ALU.mult), RK, RK)
            D(lambda e: e.tensor_tensor(out=gate[:], in0=rt[0][:], in1=rt[1][:], op=ALU.add), RK, ["gate"])

            for t in range(NT):
                P.add("act", lambda e, t=t: e.activation(out=x_tm[:, t, :], in_=x_tm[:, t, :], func=AF.Copy, scale=ALPHA),
                      reads=[("x_tm", t)], writes=[("x_tm", t)])

            def down_unit(ex, t):
                k = ex % 2
                pair = acc.next()
                for hf in range(2):
                    bq = pair[hf]

                    def f(e, bq=bq, hf=hf):
                        r = None
                        for fc in range(2):
                            r = e.matmul(banks[bq][:], lhsT=hT[k][:, fc, t * 128:(t + 1) * 128], rhs=wd[k][:, fc, hf * 512:(hf + 1) * 512], start=(fc == 0), stop=(fc == 1))
                        return r
                    P.add("pe", f, reads=[("hT", k, 0, t // 4), ("hT", k, 1, t // 4), ("wd", k)], writes=[bk(bq)])
                    if ex == 15:
                        P.add("dve", lambda e, bq=bq, hf=hf: e.scalar_tensor_tensor(
                            out=x_tm[:, t, hf * 512:(hf + 1) * 512], in0=banks[bq][:], scalar=gate[:, t, ex:ex + 1],
                            in1=x_tm[:, t, hf * 512:(hf + 1) * 512], op0=ALU.mult, op1=ALU.add, accum_out=sttA[:, t, hf, 0:1]),
                            reads=[bk(bq), "gate", ("x_tm", t)], writes=[("x_tm", t), ("stt", t, hf)])
                    else:
                        P.add("dve", lambda e, bq=bq, hf=hf: e.scalar_tensor_tensor(
                            out=x_tm[:, t, hf * 512:(hf + 1) * 512], in0=banks[bq][:], scalar=gate[:, t, ex:ex + 1],
                            in1=x_tm[:, t, hf * 512:(hf + 1) * 512], op0=ALU.mult, op1=ALU.add),
                            reads=[bk(bq), "gate", ("x_tm", t)], writes=[("x_tm", t)])

            for ex in range(0, 16):
                if ex + 2 < 16:
                    load_gu(ex + 2)
                if ex + 1 < 16:
                    load_d(ex + 1)
                for i in range(8):
                    if ex + 1 < 16:
                        gu_unit(ex + 1, i)
                    down_unit(ex, 2 * i)
                    down_unit(ex, 2 * i + 1)
            load_ln(li, 2)
            layer_norm_all(li, 2, make=(li != layers[-1]))
            barrier(MOE_KEYS, ATT_KEYS + ACC_KEYS)

        def v_padded(lhs_fn, wcols, wbuf=None, wkey="wbuf"):
            wbuf = wbuf if wbuf is not None else view(17408, [8, 384])
            for t in range(NT):
                b = rot.next()

                def f(e, b=b, t=t):
                    r = None
                    for kc in range(8):
                        r = e.matmul(banks[b][:, 0:128], lhsT=lhs_fn(kc, t), rhs=wbuf[:, kc, wcols], start=(kc == 0), stop=(kc == 7))
                    return r
                P.add("pe", f, reads=["xT", wkey], writes=[bk(b)])
                for ab in range(2):
                    P.add("act", lambda e, b=b, t=t, ab=ab: e.activation(out=vAB[:, t, ab, 64 * ab:64 * ab + 64], in_=banks[b][:, 64 * ab:64 * ab + 64], func=AF.Copy),
                          reads=[bk(b)], writes=["vAB"])

        def layer_win(li):
            w_in = W["win_w_in"][0]
            P.add("pool", lambda e: e.memset(vAB, 0.0), writes=["vAB"])
            for a in range(2):
                for g3 in range(3):
                    cc = a * 3 + g3
                    for ab in range(2):
                        h = (2 * a + ab) * 3 + g3
                        dma("sp", esk[64 * ab:64 * ab + 64, cc:cc + 1], W["win_sink"][0, h:h + 1].partition_broadcast(64), [], ["esk"])
            P.add("act", lambda e: e.activation(out=esk[:, 0:6], in_=esk[:, 0:6], func=AF.Exp), reads=["esk"], writes=["esk"])
            for a in range(2):
                dma("pool", masks, CT["c_winmask"][:, a * 18 * 128:(a + 1) * 18 * 128], [], ["masks"])
                wb_, wk_ = next_wb()
                load_w(wb_, wk_, w_in, 768 + a * 128, 128, col0=0)
                load_w(wb_, wk_, w_in, 1024 + a * 128, 128, col0=128)
                linear_fm(KT, "KT", lambda kc, wb_=wb_: wb_[:, kc, 0:128], wk_)
                v_padded(lambda kc, t: xT[:, kc, t * 128:(t + 1) * 128], slice(128, 256), wb_, wk_)
                for g3 in range(3):
                    cc = a * 3 + g3
                    hA, hB = (2 * a) * 3 + g3, (2 * a + 1) * 3 + g3
                    wb_, wk_ = next_wb()
                    load_w(wb_, wk_, w_in, hA * 64, 64, col0=256)
                    load_w(wb_, wk_, w_in, hB * 64, 64, col0=320)
                    linear_fm(QT, "QT", lambda kc, wb_=wb_: wb_[:, kc, 256:384], wk_)

                    def mfn(ab, di, g3=g3):
                        o = ((g3 * 2 + ab) * 3 + di) * 128
                        return masks[:, o:o + 128]
                    dbg("QT", QT, ["QT"])
                    dbg("KT", KT, ["KT"])
                    dbg("vAB", vAB, ["vAB"])
                    dbg("masks", masks, ["masks"])
                    dbg("esk", esk[:], ["esk"])
                    attn_banded(NT, mfn, 0.125, attn_epi_norm(cc, sink_col=cc))
                    dbg("pt0", pts[0], ["pt"])
            def rowmap(fc):
                if fc >= 6:
                    return [(0, fc * 128, 128)]
                a, g3 = fc // 3, fc % 3
                return [(0, ((2 * a) * 3 + g3) * 64, 64), (64, ((2 * a + 1) * 3 + g3) * 64, 64)]
            prefetch_wo(W["win_w_out"][0], 8, rowmap)
            do_mem(li, w_in, 1536 - 256, 6)
            out_proj_ln(li, W["win_w_out"][0], 8, rowmap)

        def layer_dil(li):
            w_in = W["dil_w_in"][0]
            P.add("pool", lambda e: e.memset(vAB, 0.0), writes=["vAB"])
            for c in range(4):
                dma("pool", masks, CT["c_dilmask"][:, c * 18 * 128:(c + 1) * 18 * 128], [], ["masks"])
                for p, dil in enumerate((1, 4, 16)):
                    ls = L // dil
                    xv = xT[:].rearrange("p k (s r) -> p k r s", r=dil)

                    def rhs_fn(kc, n, ls=ls, xv=xv):
                        if ls >= 512:
                            r, s0 = (512 * n) // ls, (512 * n) % ls
                            return xv[:, kc, r, s0:s0 + 512]
                        nr = 512 // ls
                        return xv[:, kc, n * nr:(n + 1) * nr, :]
                    qc = ((0 * 3 + p) * 8 + 2 * c) * 64
                    kc_ = ((1 * 3 + p) * 8 + 2 * c) * 64
                    vc = ((2 * 3 + p) * 8 + 2 * c) * 64
                    wb_, wk_ = next_wb()
                    load_w(wb_, wk_, w_in, qc, 128, col0=0)
                    load_w(wb_, wk_, w_in, kc_, 128, col0=128)
                    load_w(wb_, wk_, w_in, vc, 128, col0=256)
                    linear_fm(QT, "QT", lambda kc, wb_=wb_: wb_[:, kc, 0:128], wk_, perm=dil)
                    linear_fm(KT, "KT", lambda kc, wb_=wb_: wb_[:, kc, 128:256], wk_, perm=dil)
                    v_padded(lambda kc, t, xv=xv, ls=ls: xv[:, kc, (128 * t) // ls, (128 * t) % ls:(128 * t) % ls + 128], slice(256, 384), wb_, wk_)

                    def mfn(ab, di, p=p):
                        o = ((p * 2 + ab) * 3 + di) * 128
                        return masks[:, o:o + 128]

                    def epi(g, nb, db, p=p, dil=dil, ls=ls):
                        def vw(tl):
                            v = tl.rearrange("p (s r) -> p r s", r=dil)
                            if ls >= 512:
                                r, s0 = (512 * g) // ls, (512 * g) % ls
                                return v[:, r, s0:s0 + 512], None
                            nr = 512 // ls
                            return v[:, g * nr:(g + 1) * nr, :], nr
                        for bkk, tl, nm in ((nb, accN, "accN"), (db, accD, "accD")):
                            ov, nr = vw(tl)
                            src = banks[bkk][:] if nr is None else banks[bkk][:].rearrange("p (r s) -> p r s", r=nr)
                            if p == 0:
                                P.add("act", lambda e, ov=ov, src=src: e.activation(out=ov, in_=src, func=AF.Copy), reads=[bk(bkk)], writes=[nm])
                            else:
                                P.add("dve", lambda e, ov=ov, src=src: e.tensor_tensor(out=ov, in0=ov, in1=src, op=ALU.add), reads=[bk(bkk), nm], writes=[nm])
                    attn_banded(max(1, ls // 128), mfn, 0.125, epi)
                P.add("dve", lambda e: e.reciprocal(out=accD, in_=accD), reads=["accD"], writes=["accD"])
                P.add("dve", lambda e, c=c: e.tensor_tensor(out=mixT[:, c, :], in0=accN, in1=accD, op=ALU.mult), reads=["accN", "accD"], writes=[("mixT", c)])
            barrier(ACC_KEYS, ["wo", "wkv", "KTm", "vm", "rec"])
            prefetch_wo(W["dil_w_out"][0], 6)
            do_mem(li, w_in, 4864 - 256, 4)
            out_proj_ln(li, W["dil_w_out"][0], 6)
            barrier(["wo", "wkv", "KTm", "vm", "rec"], ACC_KEYS)

        PI = float(np.pi)
        S5 = {}

        def view_on(base, off, shape, dt=F32):
            n = int(np.prod(shape))
            if dt == BF16:
                v = base[:, off // 2:off // 2 + n]
            else:
                v = base[:, off // 2:off // 2 + 2 * n].bitcast(dt)
            if len(shape) == 1:
                return v
            names = " ".join(f"a{i}" for i in range(len(shape)))
            return v.rearrange(f"p ({names}) -> p {names}", **{f"a{i}": shape[i] for i in range(1, len(shape))})

        def s5_setup():
            MI_d = nc.dram_tensor("s5_MI", [128, 32, 128], BF16, kind="Internal").ap()
            MINP_d = nc.dram_tensor("s5_MINP", [128, 16, 2, 4, 128], BF16, kind="Internal").ap()
            MOUT_d = nc.dram_tensor("s5_MOUT", [128, 16, 2, 2, 128], BF16, kind="Internal").ap()
            MU_d = nc.dram_tensor("s5_MU", [128, 2, 3, 16, 8], F32, kind="Internal").ap()
            S5.update(MI=MI_d, MINP=MINP_d, MOUT=MOUT_d, MU=MU_d)
            xw = xT[:].rearrange("p a b -> p (a b)")
            mw = mixT[:].rearrange("p a b -> p (a b)")
            I32 = mybir.dt.int32

            def vx(off, shape, dt=F32):
                return view_on(xw, off, shape, dt)

            def vmx(off, shape, dt=F32):
                return view_on(mw, off, shape, dt)

            def vs(off, shape, dt=F32):
                return view_on(scr, off, shape, dt)
            PW = {("a", "re"): vx(0, [32, 16]), ("a", "im"): vx(2048, [32, 16]), ("d", "re"): vx(4096, [32, 16]), ("d", "im"): vx(6144, [32, 16])}
            bbre, bbim = vx(8192, [32, 16]), vx(10240, [32, 16])
            Cre, Cim = vx(12288, [32, 16]), vx(14336, [32, 16])
            Bre, Bim = vx(16384, [32, 16]), vx(18432, [32, 16])
            t1, t2, t3 = vx(20480, [32, 16]), vx(22528, [32, 16]), vx(24576, [32, 16])
            ki = vx(26624, [32, 16], I32)
            sm = vx(28672, [24, 32])
            evec = vx(31744, [2, 16])
            stg = vmx(0, [4, 128], BF16)
            L32 = vmx(1024, [2, 64])
            CL = vmx(1536, [2, 64])
            maskFB = vmx(2048, [2, 128])
            identf = vmx(3072, [128])
            D8 = vmx(3584, [8, 16])
            dcol = vmx(4096, [32])
            lsb = vmx(4224, [32])
            MUt = vmx(4352, [3, 16, 8])
            T2re = vmx(8192, [16, 128])
            T2im = vmx(16384, [16, 128])
            TX = vs(0, [32, 128])
            TY = vs(16384, [32, 128])
            MiA = vs(32768, [32, 128])
            K = "s5w"

            def DV(fn, rd=(), wr=()):
                P.add("dve", fn, reads=[K] + list(rd), writes=[K] + list(wr))

            def AC(fn, rd=(), wr=()):
                P.add("act", fn, reads=[K] + list(rd), writes=[K] + list(wr))
            dma("sp", evec, CT["c_evec"].rearrange("p (a b) -> p a b", a=2), [], [K])
            dma("sp", maskFB, CT["c_s5mask"].rearrange("p (a b) -> p a b", a=2), [], [K])
            dma("sp", identf, CT["c_ident"], [], [K])
            P.add("pool", lambda e: e.memset(stg, 0.0), reads=[K], writes=[K])
            for nm, dst in (("s5_c_re", Cre), ("s5_c_im", Cim)):
                src = W[nm][0].rearrange("g c p -> (g c) p")
                for i in range(4):
                    for du in range(2):
                        dma("sp", CL[:, du, :], src[i * 128:(i + 1) * 128, :], [], [K])
                    b = rot.next()
                    P.add("pe", lambda e, b=b: e.transpose(banks[b][:, 0:128], CL.rearrange("p a b -> p (a b)"), identf), reads=[K], writes=[bk(b)])
                    AC(lambda e, b=b, dst=dst, i=i: e.activation(out=dst[:, 8 * i:8 * i + 8, :], in_=banks[b][:, 0:128].rearrange("p (g c) -> p g c", c=16), func=AF.Copy), [bk(b)])
            for nm, dst in (("s5_b_re", Bre), ("s5_b_im", Bim)):
                for du in range(2):
                    dma("sp", dst[64 * du:64 * du + 64], W[nm][0].rearrange("g p c -> p g c"), [], [K])
            for tau in range(8):
                dma("sp", D8[0:32, tau, :], W["s5_d"][0].rearrange("(g c) -> g c", c=16), [], [K])
            b = rot.next()
            P.add("pe", lambda e, b=b: e.transpose(banks[b][:, 0:32], D8[0:32].rearrange("p a b -> p (a b)"), identf[0:32, 0:32]), reads=[K], writes=[bk(b)])
            AC(lambda e, b=b: e.activation(out=dcol, in_=banks[b][:, 0:32], func=AF.Copy), [bk(b)])

            def sml(i):
                return sm[:, i, :]
            lre, lim, st, a_, th, den, cr, ci, nr, u1, u2 = (sml(i) for i in range(11))

            def rr_sin(dst, src, shift):
                DV(lambda e: e.tensor_scalar(out=t3, in0=src, scalar1=1.0 / (2 * PI), scalar2=32.5 + shift / (2 * PI), op0=ALU.mult, op1=ALU.add))
                DV(lambda e: e.tensor_copy(out=ki, in_=t3))
                DV(lambda e: e.tensor_copy(out=t3, in_=ki))
                DV(lambda e: e.tensor_scalar(out=dst, in0=src, scalar1=64 * PI + shift, scalar2=None, op0=ALU.add))
                DV(lambda e: e.scalar_tensor_tensor(out=dst, in0=t3, scalar=-2 * PI, in1=dst, op0=ALU.mult, op1=ALU.add))
                DV(lambda e: e.tensor_scalar(out=t3, in0=dst, scalar1=-PI, scalar2=None, op0=ALU.is_lt))
                DV(lambda e: e.scalar_tensor_tensor(out=dst, in0=t3, scalar=2 * PI, in1=dst, op0=ALU.mult, op1=ALU.add))
                DV(lambda e: e.tensor_scalar(out=dst, in0=dst, scalar1=PI, scalar2=-PI, op0=ALU.min, op1=ALU.max))
                AC(lambda e: e.activation(out=dst, in_=dst, func=AF.Sin))

            def bc_g(x):
                return x.unsqueeze(2).to_broadcast([128, 32, 16])

            def table(TT, half, pr_, pi_, e0, Tr, Ti, kind, gsl=None):
                for j in range(8):
                    if gsl is None:
                        wr_ = pr_[half, :, e0 + j:e0 + j + 1].to_broadcast([64, 32, 16])
                        wi_ = pi_[half, :, e0 + j:e0 + j + 1].to_broadcast([64, 32, 16])
                        xr, xi, q1, q2 = Tr[half], Ti[half], t1[half], t2[half]
                    else:
                        wr_ = pr_[half, gsl, e0 + j:e0 + j + 1].to_broadcast([64, 16, 16])
                        wi_ = pi_[half, gsl, e0 + j:e0 + j + 1].to_broadcast([64, 16, 16])
                        xr, xi, q1, q2 = Tr[half, gsl, :], Ti[half, gsl, :], t1[half, 0:16, :], t2[half, 0:16, :]
                    o = TT[half, :, j, :]
                    if kind == "re":
                        DV(lambda e, wr_=wr_, xr=xr, q1=q1: e.tensor_tensor(out=q1, in0=wr_, in1=xr, op=ALU.mult))
                        DV(lambda e, wi_=wi_, xi=xi, q2=q2: e.tensor_tensor(out=q2, in0=wi_, in1=xi, op=ALU.mult))
                        DV(lambda e, o=o, q1=q1, q2=q2: e.tensor_tensor(out=o, in0=q1, in1=q2, op=ALU.subtract), wr=["s5T"])
                    else:
                        DV(lambda e, wr_=wr_, xi=xi, q1=q1: e.tensor_tensor(out=q1, in0=wr_, in1=xi, op=ALU.mult))
                        DV(lambda e, wi_=wi_, xr=xr, q2=q2: e.tensor_tensor(out=q2, in0=wi_, in1=xr, op=ALU.mult))
                        if kind == "im":
                            DV(lambda e, o=o, q1=q1, q2=q2: e.tensor_tensor(out=o, in0=q1, in1=q2, op=ALU.add), wr=["s5T"])
                        else:
                            DV(lambda e, o=o, q1=q1, q2=q2: e.scalar_tensor_tensor(out=o, in0=q1, scalar=-1.0, in1=q2, op0=ALU.mult, op1=ALU.subtract), wr=["s5T"])

            LO, HI = slice(0, 64), slice(64, 128)
            TX4 = TX.rearrange("p g (j c) -> p g j c", c=16)
            TY4 = TY.rearrange("p g (j c) -> p g j c", c=16)
            T2re4 = T2re.rearrange("p g (j c) -> p g j c", c=16)
            T2im4 = T2im.rearrange("p g (j c) -> p g j c", c=16)
            for d_ in range(2):
                for nm, dst in (("s5_lam_re", lre), ("s5_lam_im", lim)):
                    for du in range(2):
                        dma("sp", L32[0:32, du, :], W[nm][0, d_], [], [K])
                    b = rot.next()
                    P.add("pe", lambda e, b=b: e.transpose(banks[b][:, 0:32], L32[0:32].rearrange("p a b -> p (a b)"), identf[0:32, 0:32]), reads=[K], writes=[bk(b)])
                    AC(lambda e, b=b, dst=dst: e.activation(out=dst, in_=banks[b][:, 0:32], func=AF.Copy), [bk(b)])
                dma("sp", lsb, W["s5_log_step"][0, d_].partition_broadcast(128), [], [K])
                AC(lambda e: e.activation(out=st, in_=lsb, func=AF.Exp))
                DV(lambda e: e.tensor_tensor(out=a_, in0=lre, in1=st, op=ALU.mult))
                DV(lambda e: e.tensor_tensor(out=th, in0=lim, in1=st, op=ALU.mult))
                for oi, on in enumerate(("a", "d")):
                    ev = evec[:, oi, :].unsqueeze(1).to_broadcast([128, 32, 16])
                    pre, pim = PW[(on, "re")], PW[(on, "im")]
                    DV(lambda e, ev=ev: e.tensor_tensor(out=t1, in0=bc_g(a_), in1=ev, op=ALU.mult))
                    AC(lambda e: e.activation(out=t1, in_=t1, func=AF.Exp))
                    DV(lambda e, ev=ev: e.tensor_tensor(out=t2, in0=bc_g(th), in1=ev, op=ALU.mult))
                    rr_sin(pim, t2, 0.0)
                    rr_sin(pre, t2, PI / 2)
                    DV(lambda e, pim=pim: e.tensor_tensor(out=pim, in0=pim, in1=t1, op=ALU.mult))
                    DV(lambda e, pre=pre: e.tensor_tensor(out=pre, in0=pre, in1=t1, op=ALU.mult))
                pAr, pAi, pDr, pDi = PW[("a", "re")], PW[("a", "im")], PW[("d", "re")], PW[("d", "im")]
                DV(lambda e: e.tensor_scalar(out=nr, in0=pAr[:, :, 8], scalar1=-1.0, scalar2=None, op0=ALU.add))
                DV(lambda e: e.tensor_tensor(out=den, in0=lre, in1=lre, op=ALU.mult))
                DV(lambda e: e.tensor_tensor(out=u1, in0=lim, in1=lim, op=ALU.mult))
                DV(lambda e: e.tensor_tensor(out=den, in0=den, in1=u1, op=ALU.add))
                DV(lambda e: e.reciprocal(out=den, in_=den))
                DV(lambda e: e.tensor_tensor(out=u1, in0=nr, in1=lre, op=ALU.mult))
                DV(lambda e: e.tensor_tensor(out=u2, in0=pAi[:, :, 8], in1=lim, op=ALU.mult))
                DV(lambda e: e.tensor_tensor(out=cr, in0=u1, in1=u2, op=ALU.add))
                DV(lambda e: e.tensor_tensor(out=cr, in0=cr, in1=den, op=ALU.mult))
                DV(lambda e: e.tensor_tensor(out=u1, in0=pAi[:, :, 8], in1=lre, op=ALU.mult))
                DV(lambda e: e.tensor_tensor(out=u2, in0=nr, in1=lim, op=ALU.mult))
                DV(lambda e: e.tensor_tensor(out=ci, in0=u1, in1=u2, op=ALU.subtract))
                DV(lambda e: e.tensor_tensor(out=ci, in0=ci, in1=den, op=ALU.mult))
                DV(lambda e: e.tensor_tensor(out=t1, in0=bc_g(cr), in1=Bre, op=ALU.mult))
                DV(lambda e: e.tensor_tensor(out=t2, in0=bc_g(ci), in1=Bim, op=ALU.mult))
                DV(lambda e: e.tensor_tensor(out=bbre, in0=t1, in1=t2, op=ALU.subtract))
                DV(lambda e: e.tensor_tensor(out=t1, in0=bc_g(cr), in1=Bim, op=ALU.mult))
                DV(lambda e: e.tensor_tensor(out=t2, in0=bc_g(ci), in1=Bre, op=ALU.mult))
                DV(lambda e: e.tensor_tensor(out=bbim, in0=t1, in1=t2, op=ALU.add))
                if d_ == 0:
                    xs, ys = (pDr, pDi, 8), (pAr, pAi, 7)
                else:
                    xs, ys = (pAr, pAi, 7), (pDr, pDi, 8)
                table(TX4, LO, xs[0], xs[1], xs[2], bbre, bbim, "re")
                table(TX4, HI, xs[0], xs[1], xs[2], bbre, bbim, "im")
                table(TY4, LO, ys[0], ys[1], ys[2], Cre, Cim, "re")
                table(TY4, HI, ys[0], ys[1], ys[2], Cre, Cim, "imn")
                for g in range(32):
                    b = rot.next()
                    P.add("pe", lambda e, b=b, g=g: e.matmul(banks[b][:, 0:128], lhsT=TX[:, g, :], rhs=TY[:, g, :], start=True, stop=True), reads=["s5T", K], writes=[bk(b)])
                    if d_ == 0:
                        P.add("dve", lambda e, b=b, g=g: e.tensor_tensor(out=MiA[:, g, :], in0=banks[b][:, 0:128], in1=maskFB[:, 0, :], op=ALU.mult), reads=[bk(b), K], writes=[("s5Mi", g)])
                    else:
                        P.add("dve", lambda e, b=b, g=g: e.tensor_tensor(out=t1[:, 0:8, :].rearrange("p a b -> p (a b)"), in0=banks[b][:, 0:128], in1=maskFB[:, 1, :], op=ALU.mult), reads=[bk(b), K], writes=[K])
                        P.add("dve", lambda e, g=g: e.tensor_tensor(out=MiA[:, g, :], in0=MiA[:, g, :], in1=t1[:, 0:8, :].rearrange("p a b -> p (a b)"), op=ALU.add), reads=[K, ("s5Mi", g)], writes=[("s5Mi", g)])
                        P.add("dve", lambda e, g=g: e.scalar_tensor_tensor(out=MiA[:, g, :], in0=identf, scalar=dcol[:, g:g + 1], in1=MiA[:, g, :], op0=ALU.mult, op1=ALU.add), reads=[K, ("s5Mi", g)], writes=[("s5Mi", g)])
                if d_ == 1:
                    dma("pool", MI_d, MiA, ["s5Mi"], ["s5dram"])
                if d_ == 0:
                    table(TX4, LO, pDr, pDi, 1, bbre, bbim, "re")
                    table(TX4, HI, pDr, pDi, 1, bbre, bbim, "im")
                for pr in range(16):
                    for g2 in range(2):
                        g = 2 * pr + g2
                        b = rot.next()
                        P.add("pe", lambda e, b=b, g=g: e.transpose(banks[b][:, 0:128], TX[:, g, :], identf), reads=["s5T", K], writes=[bk(b)])
                        for ri in range(2):
                            P.add("act", lambda e, b=b, ri=ri, g2=g2: e.activation(out=stg[:, ri * 2 + g2, 64 * g2:64 * g2 + 64], in_=banks[b][:, 64 * ri:64 * ri + 64], func=AF.Copy),
                                  reads=[bk(b)], writes=["s5stg"])
                    dma("sp", MINP_d[:, pr, d_], stg, ["s5stg"], ["s5dram"])
                ms = (pAr, pAi, 8) if d_ == 0 else (pDr, pDi, 0)
                for half, gsl in ((LO, slice(0, 32, 2)), (HI, slice(1, 32, 2))):
                    table(T2re4, half, ms[0], ms[1], ms[2], Cre, Cim, "re", gsl=gsl)
                    table(T2im4, half, ms[0], ms[1], ms[2], Cre, Cim, "imn", gsl=gsl)
                dma("pool", MOUT_d[:, :, d_, 0, :], T2re, ["s5T", K], ["s5dram"])
                dma("pool", MOUT_d[:, :, d_, 1, :], T2im, ["s5T", K], ["s5dram"])
                for half, gsl in ((LO, slice(0, 32, 2)), (HI, slice(1, 32, 2))):
                    DV(lambda e, half=half, gsl=gsl: e.tensor_copy(out=MUt[half, 0, :, 0], in_=pAr[half, gsl, 15]))
                    DV(lambda e, half=half, gsl=gsl: e.tensor_copy(out=MUt[half, 1, :, 0], in_=pAi[half, gsl, 15]))
                for j in range(7):
                    r0, i0, r1, i1 = MUt[:, 0, :, j], MUt[:, 1, :, j], MUt[:, 0, :, j + 1], MUt[:, 1, :, j + 1]
                    DV(lambda e, r0=r0: e.tensor_tensor(out=u1[:, 0:16], in0=r0, in1=r0, op=ALU.mult))
                    DV(lambda e, i0=i0: e.tensor_tensor(out=u2[:, 0:16], in0=i0, in1=i0, op=ALU.mult))
                    DV(lambda e, r1=r1: e.tensor_tensor(out=r1, in0=u1[:, 0:16], in1=u2[:, 0:16], op=ALU.subtract))
                    DV(lambda e, r0=r0, i0=i0: e.tensor_tensor(out=u1[:, 0:16], in0=r0, in1=i0, op=ALU.mult))
                    DV(lambda e, i1=i1: e.tensor_scalar(out=i1, in0=u1[:, 0:16], scalar1=2.0, scalar2=None, op0=ALU.mult))
                DV(lambda e: e.tensor_scalar(out=MUt[:, 2, :, :], in0=MUt[:, 1, :, :], scalar1=-1.0, scalar2=None, op0=ALU.mult))
                dma("sp", MU_d[:, d_], MUt, [K], ["s5dram"])
            barrier([K, "s5T", "s5Mi", "s5stg", "xT", "mixT", "scr_all"], ["xT", "mixT"] + ATT_KEYS + ACC_KEYS)

        def layer_s5(li):
            w_in = W["s5_w_in"][0]
            S5K = ["s5sel", "s5uT", "s5U", "s5minp", "s5mout", "s5mi", "s5sbf", "s5mu"]
            barrier(ATT_KEYS + ACC_KEYS, S5K)
            sel = view(0, [64, 128])
            wbu = view(0, [8, 512])
            uT = view(16384, [4, L])
            Uall = view(32768, [32, 256])
            MINP_t = view(49152, [2, 4, 128])
            MOUT_t = view(51200, [2, 2, 128])
            MI_t = view(52224, [2, 128])
            Sbf = view(52736, [2, 2, 256])
            MU_t = view(54784, [2, 3, 8], F32)
            lnf = lnp[:].rearrange("p a b -> p (a b)")
            SSd = [[[lnf[:, 1024 * dd + (2 * ab + ri) * 256:1024 * dd + (2 * ab + ri + 1) * 256] for ri in range(2)] for ab in range(2)] for dd in range(2)]
            wglu = view(0, [4, 512])
            Gall = mixT[:, 4:8, :].rearrange("p a b -> p (a b)").rearrange("p (g k) -> p g k", k=256)
            gtmp = [rt[i][:].rearrange("p a b -> p (a b)") for i in range(3)]
            load_w(wbu, "s5sel", w_in, 0, 512)
            for fc in range(4):
                linear_fm(uT[:, fc, :], "s5uT", lambda kc, fc=fc: wbu[:, kc, fc * 128:(fc + 1) * 128], "s5sel", perm=8)
            dma("pool", sel, CT["c_sel"].rearrange("p (a b) -> p a b", b=128), [], ["s5sel"])
            for g in range(32):
                b = rot.next()

                def f(e, b=b, g=g):
                    r = None
                    uv = uT[:, g // 8, :].rearrange("p (t k) -> p t k", t=8)
                    for tau in range(8):
                        r = e.matmul(banks[b][:, 0:256], lhsT=sel[:, (g % 8) * 8 + tau, :], rhs=uv[:, tau, :], start=(tau == 0), stop=(tau == 7))
                    return r
                P.add("pe", f, reads=["s5sel", "s5uT"], writes=[bk(b)])
                P.add("act", lambda e, b=b, g=g: e.activation(out=Uall[:, g, :], in_=banks[b][:, 0:256], func=AF.Copy), reads=[bk(b)], writes=[("s5U", g)])
            dbg("s5uT", uT, ["s5uT"])
            dbg("s5U", Uall, ["s5U"])
            dma("pool", sel, CT["c_selT"].rearrange("p (a b) -> p a b", b=128), [], ["s5sel"])
            for pr in range(16):
                dma("sp", MINP_t, S5["MINP"][:, pr], ["s5dram"], ["s5minp"])
                dma("sp", MOUT_t, S5["MOUT"][:, pr], ["s5dram"], ["s5mout"])
                dma("sp", MI_t, S5["MI"][:, 2 * pr:2 * pr + 2, :], ["s5dram"], ["s5mi"])
                dma("sp", MU_t, S5["MU"][:, :, :, pr, :], ["s5dram"], ["s5mu"])
                for d_ in range(2):
                    b = rot.next()
                    SS = SSd[d_]

                    def f(e, b=b, d_=d_, pr=pr):
                        r = None
                        for ri in range(2):
                            for g2 in range(2):
                                r = e.matmul(banks[b][:, ri * 256:(ri + 1) * 256], lhsT=MINP_t[:, d_, ri * 2 + g2, :], rhs=Uall[:, 2 * pr + g2, :],
                                             start=(g2 == 0), stop=(g2 == 1))
                        return r
                    P.add("pe", f, reads=["s5minp", ("s5U", 2 * pr), ("s5U", 2 * pr + 1)], writes=[bk(b)])
                    for ri in range(2):
                        P.add("act", lambda e, b=b, ri=ri, SS=SS: e.activation(out=SS[0][ri], in_=banks[b][:, ri * 256:(ri + 1) * 256], func=AF.Copy),
                              reads=[bk(b)], writes=[("lnp", "S", d_, 0, ri)])
                cur = 0
                for j in range(8):
                    sh = 1 << j
                    nx = 1 - cur
                    for d_ in range(2):
                        SS = SSd[d_]
                        if d_ == 0:
                            dst, src, keep = slice(sh, 256), slice(0, 256 - sh), slice(0, sh)
                        else:
                            dst, src, keep = slice(0, 256 - sh), slice(sh, 256), slice(256 - sh, 256)
                        cr_, ci_, nr_, ni_ = SS[cur][0], SS[cur][1], SS[nx][0], SS[nx][1]
                        kc_r, kc_i, kn_r, kn_i = ("lnp", "S", d_, cur, 0), ("lnp", "S", d_, cur, 1), ("lnp", "S", d_, nx, 0), ("lnp", "S", d_, nx, 1)
                        a_ = MU_t[:, d_, 0, j:j + 1]
                        b_ = MU_t[:, d_, 1, j:j + 1]
                        nb_ = MU_t[:, d_, 2, j:j + 1]
                        P.add("dve", lambda e, nr_=nr_, cr_=cr_, a_=a_, dst=dst, src=src: e.scalar_tensor_tensor(out=nr_[:, dst], in0=cr_[:, src], scalar=a_, in1=cr_[:, dst], op0=ALU.mult, op1=ALU.add),
                              reads=[kc_r, kc_r + ("k",), "s5mu"], writes=[kn_r])
                        P.add("dve", lambda e, ni_=ni_, ci_=ci_, a_=a_, dst=dst, src=src: e.scalar_tensor_tensor(out=ni_[:, dst], in0=ci_[:, src], scalar=a_, in1=ci_[:, dst], op0=ALU.mult, op1=ALU.add),
                              reads=[kc_i, kc_i + ("k",), "s5mu"], writes=[kn_i])
                        P.add("act", lambda e, nr_=nr_, cr_=cr_, keep=keep: e.activation(out=nr_[:, keep], in_=cr_[:, keep], func=AF.Copy), reads=[kc_r, kc_r + ("k",)], writes=[kn_r + ("k",)])
                        P.add("act", lambda e, ni_=ni_, ci_=ci_, keep=keep: e.activation(out=ni_[:, keep], in_=ci_[:, keep], func=AF.Copy), reads=[kc_i, kc_i + ("k",)], writes=[kn_i + ("k",)])
                    for d_ in range(2):
                        SS = SSd[d_]
                        if d_ == 0:
                            dst, src = slice(sh, 256), slice(0, 256 - sh)
                        else:
                            dst, src = slice(0, 256 - sh), slice(sh, 256)
                        cr_, ci_, nr_, ni_ = SS[cur][0], SS[cur][1], SS[nx][0], SS[nx][1]
                        kc_r, kc_i, kn_r, kn_i = ("lnp", "S", d_, cur, 0), ("lnp", "S", d_, cur, 1), ("lnp", "S", d_, nx, 0), ("lnp", "S", d_, nx, 1)
                        b_ = MU_t[:, d_, 1, j:j + 1]
                        nb_ = MU_t[:, d_, 2, j:j + 1]
                        P.add("dve", lambda e, nr_=nr_, ci_=ci_, nb_=nb_, dst=dst, src=src: e.scalar_tensor_tensor(out=nr_[:, dst], in0=ci_[:, src], scalar=nb_, in1=nr_[:, dst], op0=ALU.mult, op1=ALU.add),
                              reads=[kc_i, kc_i + ("k",), kn_r, "s5mu"], writes=[kn_r])
                        P.add("dve", lambda e, ni_=ni_, cr_=cr_, b_=b_, dst=dst, src=src: e.scalar_tensor_tensor(out=ni_[:, dst], in0=cr_[:, src], scalar=b_, in1=ni_[:, dst], op0=ALU.mult, op1=ALU.add),
                              reads=[kc_r, kc_r + ("k",), kn_i, "s5mu"], writes=[kn_i])
                    cur = nx
                for d_ in range(2):
                    for ri in range(2):
                        P.add("act", lambda e, ri=ri, d_=d_, cur=cur: e.activation(out=Sbf[:, d_, ri, :], in_=SSd[d_][cur][ri], func=AF.Copy),
                              reads=[("lnp", "S", d_, cur, ri), ("lnp", "S", d_, cur, ri, "k")], writes=[("s5sbf", d_, ri)])
                for g2 in range(2):
                    g = 2 * pr + g2
                    b = rot.next()
                    hp = slice(64 * g2, 64 * g2 + 64)

                    def f(e, b=b, g=g, g2=g2, hp=hp):
                        e.matmul(banks[b][:, 0:256], lhsT=MI_t[:, g2, :], rhs=Uall[:, g, :], start=True, stop=False)
                        e.matmul(banks[b][:, 1:256], lhsT=MOUT_t[hp, 0, 0, :], rhs=Sbf[hp, 0, 0, 0:255], start=False, stop=False)
                        e.matmul(banks[b][:, 1:256], lhsT=MOUT_t[hp, 0, 1, :], rhs=Sbf[hp, 0, 1, 0:255], start=False, stop=False)
                        e.matmul(banks[b][:, 0:255], lhsT=MOUT_t[hp, 1, 0, :], rhs=Sbf[hp, 1, 0, 1:256], start=False, stop=False)
                        return e.matmul(banks[b][:, 0:255], lhsT=MOUT_t[hp, 1, 1, :], rhs=Sbf[hp, 1, 1, 1:256], start=False, stop=True)
                    P.add("pe", f, reads=["s5mi", "s5mout", "s5sbf", ("s5U", g)], writes=[bk(b)])
                    yb = banks[b][:, 0:256]
                    gt = gtmp[g2]
                    gk = ("rt", g2)
                    P.add("act", lambda e, yb=yb, gt=gt: e.activation(out=gt, in_=yb, func=AF.Square), reads=[bk(b)], writes=[gk])
                    P.add("dve", lambda e, gt=gt: e.tensor_scalar(out=gt, in0=gt, scalar1=0.044715, scalar2=1.0, op0=ALU.mult, op1=ALU.add), reads=[gk], writes=[gk])
                    P.add("dve", lambda e, yb=yb, gt=gt: e.tensor_tensor(out=gt, in0=gt, in1=yb, op=ALU.mult), reads=[gk, bk(b)], writes=[gk])
                    P.add("act", lambda e, gt=gt: e.activation(out=gt, in_=gt, func=AF.Sigmoid, scale=1.5957691216057308), reads=[gk], writes=[gk])
                    P.add("dve", lambda e, yb=yb, gt=gt, g=g: e.tensor_tensor(out=Gall[:, g, :], in0=gt, in1=yb, op=ALU.mult), reads=[gk, bk(b)], writes=[("mixT", "s5G", g)])
            dbg("s5G", Gall, ["mixT"])
            dbg("s5Sbf", Sbf, ["s5sbf"])
            gT = uT
            for cc in range(4):
                for tp in range(8):
                    b = rot.next()

                    def f(e, b=b, cc=cc, tp=tp):
                        r = None
                        for j in range(8):
                            r = e.matmul(banks[b][:, 0:256], lhsT=sel[:, j * 8 + tp, :], rhs=Gall[:, 8 * cc + j, :], start=(j == 0), stop=(j == 7))
                        return r
                    P.add("pe", f, reads=["s5sel"] + [("mixT", "s5G", 8 * cc + j) for j in range(8)], writes=[bk(b)])
                    P.add("act", lambda e, b=b, cc=cc, tp=tp: e.activation(out=gT[:, cc, :].rearrange("p (k t) -> p t k", t=8)[:, tp, :], in_=banks[b][:, 0:256], func=AF.Copy),
                          reads=[bk(b)], writes=["s5uT"])
            dbg("s5gT", gT, ["s5uT"])
            dma("pool", wglu, W["s5_w_glu"][0].rearrange("(kc p) n -> p kc n", p=128), [], ["s5sel"])
            for fc in range(4):
                dma("sp", esk[:, fc:fc + 1], W["s5_b_glu"][0, fc * 128:(fc + 1) * 128].rearrange("(p o) -> p o", o=1), [], ["esk"])
            sgt = Uall.rearrange("p g k -> p (g k)")
            for fc in range(4):
                for n in range(4):
                    b = rot.next()

                    def f(e, b=b, fc=fc, n=n):
                        r = None
                        for kc in range(4):
                            r = e.matmul(banks[b][:], lhsT=wglu[:, kc, fc * 128:(fc + 1) * 128], rhs=gT[:, kc, n * 512:(n + 1) * 512], start=(kc == 0), stop=(kc == 3))
                        return r
                    P.add("pe", f, reads=["s5sel", "s5uT"], writes=[bk(b)])
                    si = (fc * 4 + n) % 4
                    sg = sgt[:, si * 512:(si + 1) * 512]
                    P.add("act", lambda e, b=b, sg=sg, fc=fc: e.activation(out=sg, in_=banks[b][:], func=AF.Sigmoid, bias=esk[:, fc:fc + 1]), reads=[bk(b), "esk"], writes=[("s5U", "sg", si)])
                    P.add("dve", lambda e, sg=sg, fc=fc, n=n: e.tensor_tensor(out=mixT[:, fc, n * 512:(n + 1) * 512], in0=gT[:, fc, n * 512:(n + 1) * 512], in1=sg, op=ALU.mult),
                          reads=[("s5U", "sg", si), "s5uT"], writes=[("mixT", fc, n)])
            barrier(S5K + ["mixT", "lnp", "rt", "esk"], ATT_KEYS + ACC_KEYS + ["mixT", "lnp", "rt", "esk"])
            prefetch_wo(W["s5_w_out"][0], 6)
            do_mem(li, w_in, 512, 4)
            out_proj_ln(li, W["s5_w_out"][0], 6)


        def layer_mlstm(li):
            w_in = W["mlstm_w_in"][0]
            do_mem(li, w_in, 3344 - 256, 6)
            MK = ["m_qT", "m_kT", "m_ktm", "m_vaug", "m_vt", "m_hacc", "m_Z", "m_Cbf", "m_tmp", "m_Sm", "m_wb", "m_otok",
                  "m_osig", "m_hn", "m_G", "m_lfn", "m_cum", "m_E", "m_ng", "m_sc", "m_sc2"]
            barrier(ATT_KEYS + ACC_KEYS, MK)
            qT = view(0, [2, L])
            kT = view(8192, [2, L])
            k_tm = view(16384, [NT, 192])
            v_aug = view(22528, [NT, 194])
            vt = view(28736, [NT, 194])
            hacc = view(34944, [NT, 192])
            Z = view(41088, [2, 194], F32)
            Cbf = view(42640, [2, 194])
            tmp = view(43424, [194], F32)
            Sm = [view(44224 + 256 * i, [128]) for i in range(2)]
            wb = view(44736, [8, 192])
            otok = view(47808, [256])
            osig = view(48320, [192], F32)
            hn = view(49088, [192], F32)
            Gtm = view(49856, [NT, 16], F32)
            lfn = view(50880, [NT, 8], F32)
            cumS = view(51392, [NT, 16], F32)
            EB = view(52416, [NT, 8], F32)
            EC = view(52928, [NT, 8], F32)
            EG = view(53440, [NT, 8], F32)
            ng = view(53952, [192], F32)
            gb = view(54720, [16], F32)
            sc = view(54784, [8], F32)
            tri = view(54816, [3, 128], F32)
            dma("sp", tri, CT["c_tri"].rearrange("p (a b) -> p a b", a=3), [], ["m_sc"])
            dma("sp", gb, W["mlstm_gate_b"][0].rearrange("a b -> (a b)").partition_broadcast(128), [], ["m_G"])
            load_w(wb, "m_wb", w_in, 3072, 16)
            b = rot.next()

            def f(e):
                r = None
                for t in range(NT):
                    for kc in range(8):
                        r = e.matmul(banks[b][:, t * 16:(t + 1) * 16], lhsT=xT[:, kc, t * 128:(t + 1) * 128], rhs=wb[:, kc, 0:16], start=(kc == 0), stop=(kc == 7))
                return r
            P.add("pe", f, reads=["xT", "m_wb"], writes=[bk(b)])
            P.add("dve", lambda e: e.tensor_tensor(out=Gtm, in0=banks[b][:, 0:256].rearrange("p (t c) -> p t c", c=16),
                                                   in1=gb.unsqueeze(1).to_broadcast([128, NT, 16]), op=ALU.add), reads=[bk(b), "m_G"], writes=["m_G"])
            P.add("act", lambda e: e.activation(out=lfn[:, :, 0:4], in_=Gtm[:, :, 4:8], func=AF.Exp, scale=-1.0), reads=["m_G"], writes=["m_lfn"])
            P.add("act", lambda e: e.activation(out=lfn[:, :, 4:8], in_=Gtm[:, :, 12:16], func=AF.Exp, scale=-1.0), reads=["m_G"], writes=["m_lfn"])
            P.add("act", lambda e: e.activation(out=lfn, in_=lfn, func=AF.Ln, bias=1.0), reads=["m_lfn"], writes=["m_lfn"])
            b2 = rot.next()

            def f2(e):
                r = None
                cv = banks[b2][:, 0:256].rearrange("p (t c) -> p t c", c=16)
                for t in range(NT):
                    e.matmul(cv[:, t, 0:4], lhsT=tri[:, 0, :], rhs=lfn[:, t, 0:4], start=True, stop=True)
                    e.matmul(cv[:, t, 4:8], lhsT=tri[:, 1, :], rhs=lfn[:, t, 4:8], start=True, stop=True)
                    r = e.matmul(cv[:, t, 8:16], lhsT=tri[:, 2, :], rhs=lfn[:, t, 0:8], start=True, stop=True)
                return r
            P.add("pe", f2, reads=["m_lfn", "m_sc"], writes=[bk(b2)])
            P.add("act", lambda e: e.activation(out=cumS, in_=banks[b2][:, 0:256].rearrange("p (t c) -> p t c", c=16), func=AF.Copy), reads=[bk(b2)], writes=["m_cum"])
            P.add("act", lambda e: e.activation(out=EB, in_=cumS[:, :, 0:8], func=AF.Exp, scale=-1.0), reads=["m_cum"], writes=[("m_E", 0)])
            P.add("act", lambda e: e.activation(out=EG, in_=cumS[:, :, 8:16], func=AF.Exp, scale=-1.0), reads=["m_cum"], writes=[("m_E", 1)])
            P.add("dve", lambda e: e.tensor_tensor(out=EC[:, :, 0:4], in0=Gtm[:, :, 0:4], in1=cumS[:, :, 0:4], op=ALU.add), reads=["m_G", "m_cum"], writes=[("m_E", 2)])
            P.add("dve", lambda e: e.tensor_tensor(out=EC[:, :, 4:8], in0=Gtm[:, :, 8:12], in1=cumS[:, :, 4:8], op=ALU.add), reads=["m_G", "m_cum"], writes=[("m_E", 2)])
            P.add("act", lambda e: e.activation(out=EC, in_=EC, func=AF.Exp), reads=[("m_E", 2)], writes=[("m_E", 2)])
            P.add("pool", lambda e: e.memset(otok, 0.0), writes=["m_otok"])
            for h in range(4):
                load_w(wb, "m_wb", w_in, h * 192, 192)
                linear_fm(qT[:, 0, :], "m_qT", lambda kc: wb[:, kc, 0:128], "m_wb")
                linear_fm(qT[0:64, 1, :], "m_qT", lambda kc: wb[:, kc, 128:192], "m_wb", M=64)
                load_w(wb, "m_wb", w_in, 768 + h * 192, 192)
                ksc = 192.0 ** -0.5
                linear_fm(kT[:, 0, :], "m_kT", lambda kc: wb[:, kc, 0:128], "m_wb", scale=ksc)
                linear_fm(kT[0:64, 1, :], "m_kT", lambda kc: wb[:, kc, 128:192], "m_wb", M=64, scale=ksc)

                def tokmaj(dst_fn, dkey, func=AF.Copy, scale=1.0):
                    for t in range(NT):
                        bb = rot.next()

                        def g(e, bb=bb, t=t):
                            r = None
                            for kc in range(8):
                                r = e.matmul(banks[bb][:, 0:192], lhsT=xT[:, kc, t * 128:(t + 1) * 128], rhs=wb[:, kc, 0:192], start=(kc == 0), stop=(kc == 7))
                            return r
                        P.add("pe", g, reads=["xT", "m_wb"], writes=[bk(bb)])
                        P.add("act", lambda e, bb=bb, t=t: e.activation(out=dst_fn(t), in_=banks[bb][:, 0:192], func=func, scale=scale), reads=[bk(bb)], writes=[dkey])
                tokmaj(lambda t: k_tm[:, t, :], "m_ktm", scale=ksc)
                load_w(wb, "m_wb", w_in, 1536 + h * 192, 192)
                tokmaj(lambda t: v_aug[:, t, 0:192], "m_vaug")
                P.add("pool", lambda e: e.memset(v_aug[:, :, 192:194], 1.0), writes=["m_vaug"])
                lnb = lnp[:].rearrange("p a b -> p (a b)")
                vts = [vt, lnb[:, 0:1552].bitcast(BF16).rearrange("p (t c) -> p t c", c=194)]
                Zs = [Z, lnb[:, 1552:1940].rearrange("p (a c) -> p a c", c=194)]
                Cbs = [Cbf, rt[0][:].rearrange("p a b -> p (a b)").bitcast(BF16)[:, 0:388].rearrange("p (a c) -> p a c", c=194)]
                tmps = [tmp, rt[1][:].rearrange("p a b -> p (a b)")[:, 0:194]]
                smf = rt[2][:].rearrange("p a b -> p (a b)").bitcast(BF16)
                Sms = [Sm, [smf[:, 0:128], smf[:, 128:256]]]
                scs = [sc, esk]
                P.add("pool", lambda e: e.memset(hacc, 0.0), writes=["m_hacc"])
                for d_ in range(2):
                    hd = d_ * 4 + h
                    P.add("dve", lambda e, hd=hd, d_=d_: e.tensor_tensor(out=vts[d_][:, :, 0:193], in0=v_aug[:, :, 0:193],
                                                                     in1=EC[:, :, hd:hd + 1].to_broadcast([128, NT, 193]), op=ALU.mult),
                          reads=["m_vaug", ("m_E", 2), "lnp"], writes=[("m_vt", d_), "lnp"] if d_ == 1 else [("m_vt", d_)])
                    P.add("pool", lambda e, d_=d_: e.memset(Zs[d_], 0.0), reads=["lnp"], writes=[("m_Z", d_), "lnp"] if d_ == 1 else [("m_Z", d_)])
                    P.add("pool", lambda e, d_=d_: e.memset(Cbs[d_], 0.0), reads=["rt"], writes=[("m_Cbf", d_), "rt"] if d_ == 1 else [("m_Cbf", d_)])
                kprevs = [0, NT - 1]
                pend = {}

                def stageA(step, d_):
                    k = step if d_ == 0 else NT - 1 - step
                    vt_ = vts[d_]
                    ts = slice(k * 128, (k + 1) * 128)
                    sb_ = rot.next()
                    ob, ub = acc.next()

                    def fs(e, sb_=sb_, ts=ts, ub=ub, k=k, vt_=vt_):
                        e.matmul(banks[sb_][:, 0:128], lhsT=kT[:, 0, ts], rhs=qT[:, 0, ts], start=True, stop=False)
                        e.matmul(banks[sb_][:, 0:128], lhsT=kT[0:64, 1, ts], rhs=qT[0:64, 1, ts], start=False, stop=True)
                        e.matmul(banks[ub][:, 0:193], lhsT=k_tm[:, k, 0:128], rhs=vt_[:, k, 0:193], start=True, stop=True)
                        return e.matmul(banks[ub][0:64, 256:449], lhsT=k_tm[:, k, 128:192], rhs=vt_[:, k, 0:193], start=True, stop=True)
                    P.add("pe", fs, reads=["m_qT", "m_kT", "m_ktm", ("m_vt", d_)], writes=[bk(sb_), bk(ub)])
                    si = step % 2
                    Sm_ = Sms[d_][si]
                    P.add("dve", lambda e, sb_=sb_, Sm_=Sm_, d_=d_: e.tensor_tensor(out=Sm_, in0=banks[sb_][:, 0:128], in1=tri[:, d_, :], op=ALU.mult),
                          reads=[bk(sb_), "m_sc"], writes=[("m_Sm", d_, si)])
                    pend[(step, d_)] = (k, ts, ob, ub, si, Sm_)

                def stageB(step, d_):
                    hd = d_ * 4 + h
                    k, ts, ob, ub, si, Sm_ = pend.pop((step, d_))
                    kprev = kprevs[d_]
                    vt_, Z_, Cb_, tmp_, sc_ = vts[d_], Zs[d_], Cbs[d_], tmps[d_], scs[d_]

                    def fo(e, ob=ob, Sm_=Sm_, k=k, ts=ts, vt_=vt_, Cb_=Cb_):
                        e.matmul(banks[ob][:, 0:193], lhsT=Sm_, rhs=vt_[:, k, 0:193], start=True, stop=False)
                        e.matmul(banks[ob][:, 0:193], lhsT=qT[:, 0, ts], rhs=Cb_[:, 0, 0:193], start=False, stop=False)
                        return e.matmul(banks[ob][:, 0:193], lhsT=qT[0:64, 1, ts], rhs=Cb_[0:64, 1, 0:193], start=False, stop=True)
                    P.add("pe", fo, reads=[("m_Sm", d_, si), ("m_vt", d_), "m_qT", ("m_Cbf", d_)], writes=[bk(ob)])
                    P.add("dve", lambda e, ub=ub, kprev=kprev, hd=hd, Z_=Z_: e.scalar_tensor_tensor(out=Z_[:, 0, 0:193], in0=Z_[:, 0, 0:193], scalar=EG[:, kprev, hd:hd + 1],
                                                                                              in1=banks[ub][:, 0:193], op0=ALU.mult, op1=ALU.add),
                          reads=[bk(ub), ("m_E", 1), ("m_Z", d_, 0)], writes=[("m_Z", d_, 0)])
                    P.add("dve", lambda e, ub=ub, kprev=kprev, hd=hd, Z_=Z_: e.scalar_tensor_tensor(out=Z_[0:64, 1, 0:193], in0=Z_[0:64, 1, 0:193], scalar=EG[0:64, kprev, hd:hd + 1],
                                                                                              in1=banks[ub][0:64, 256:449], op0=ALU.mult, op1=ALU.add),
                          reads=[bk(ub), ("m_E", 1), ("m_Z", d_, 1)], writes=[("m_Z", d_, 1)])
                    P.add("act", lambda e, k=k, hd=hd, Z_=Z_, Cb_=Cb_: e.activation(out=Cb_[:, 0, 0:193], in_=Z_[:, 0, 0:193], func=AF.Copy, scale=EG[:, k, hd:hd + 1]),
                          reads=[("m_Z", d_, 0), ("m_E", 1)], writes=[("m_Cbf", d_, 0)])
                    P.add("act", lambda e, k=k, hd=hd, Z_=Z_, Cb_=Cb_: e.activation(out=Cb_[0:64, 1, 0:193], in_=Z_[0:64, 1, 0:193], func=AF.Copy, scale=EG[0:64, k, hd:hd + 1]),
                          reads=[("m_Z", d_, 1), ("m_E", 1)], writes=[("m_Cbf", d_, 1)])
                    P.add("dve", lambda e, ob=ob, k=k, hd=hd, tmp_=tmp_: e.tensor_scalar(out=tmp_[:, 0:193], in0=banks[ob][:, 0:193], scalar1=EB[:, k, hd:hd + 1], scalar2=None, op0=ALU.mult),
                          reads=[bk(ob), ("m_E", 0)], writes=[("m_tmp", d_)])
                    P.add("act", lambda e, tmp_=tmp_, sc_=sc_: e.activation(out=sc_[:, 0:1], in_=tmp_[:, 192:193], func=AF.Abs), reads=[("m_tmp", d_)], writes=[("m_sc2", d_)])
                    P.add("dve", lambda e, sc_=sc_: e.tensor_scalar(out=sc_[:, 0:1], in0=sc_[:, 0:1], scalar1=1.0, scalar2=None, op0=ALU.max), reads=[("m_sc2", d_)], writes=[("m_sc2", d_)])
                    P.add("dve", lambda e, sc_=sc_: e.reciprocal(out=sc_[:, 1:2], in_=sc_[:, 0:1]), reads=[("m_sc2", d_)], writes=[("m_sc2", d_)])
                    P.add("dve", lambda e, k=k, tmp_=tmp_, sc_=sc_: e.scalar_tensor_tensor(out=hacc[:, k, :], in0=tmp_[:, 0:192], scalar=sc_[:, 1:2], in1=hacc[:, k, :], op0=ALU.mult, op1=ALU.add),
                          reads=[("m_tmp", d_), ("m_sc2", d_), ("m_hacc", k)], writes=[("m_hacc", k)])
                    kprevs[d_] = k

                stageA(0, 0)
                stageA(0, 1)
                for step in range(NT):
                    for d_ in range(2):
                        stageB(step, d_)
                        if step + 1 < NT:
                            stageA(step + 1, d_)
                barrier([("m_vt", 1), ("m_Z", 1), ("m_Cbf", 1), ("m_tmp", 1), ("m_Sm", 1), ("m_sc2", 1)], ["lnp", "rt", "esk"])
                load_w(wb, "m_wb", w_in, 2304 + h * 192, 192)
                dma("sp", ng, W["mlstm_norm_g"][0, h * 192:(h + 1) * 192].partition_broadcast(128), [], ["m_ng"])
                lnq = lnp[:].rearrange("p a b -> p (a b)")
                hns = [hn, lnq[:, 0:192]]
                osigs = [osig, lnq[:, 192:384]]
                otoks = [otok, lnq[:, 384:512].bitcast(BF16)]
                if h == 0:
                    P.add("pool", lambda e: e.memset(otoks[1], 0.0), reads=["lnp"], writes=["lnp", ("m_otok", 1)])
                for t in range(NT):
                    stt, mv = sttA[:, t], mvA[:, t]
                    P.add("dve", lambda e, t=t, stt=stt: e.bn_stats(out=stt[:, 0, :], in_=hacc[:, t, :]), reads=[("m_hacc", t)], writes=[("stt", t)])
                    P.add("dve", lambda e, stt=stt, mv=mv: e.bn_aggr(out=mv[:, 0:2], in_=stt[:, 0, :]), reads=[("stt", t)], writes=["mv"])
                P.add("dve", lambda e: e.tensor_scalar(out=mvA[:, :, 1], in0=mvA[:, :, 1], scalar1=EPS, scalar2=None, op0=ALU.add), reads=["mv"], writes=["mv"])
                P.add("act", lambda e: e.activation(out=mvA[:, :, 2], in_=mvA[:, :, 1], func=AF.Sqrt), reads=["mv"], writes=["mv"])
                P.add("dve", lambda e: e.reciprocal(out=mvA[:, :, 2], in_=mvA[:, :, 2]), reads=["mv"], writes=["mv"])

                def stage_o(t):
                    bb = rot.next()
                    q = t % 2

                    def g(e, bb=bb, t=t):
                        r = None
                        for kc in range(8):
                            r = e.matmul(banks[bb][:, 0:192], lhsT=xT[:, kc, t * 128:(t + 1) * 128], rhs=wb[:, kc, 0:192], start=(kc == 0), stop=(kc == 7))
                        return r
                    P.add("pe", g, reads=["xT", "m_wb"], writes=[bk(bb)])
                    P.add("act", lambda e, bb=bb, q=q: e.activation(out=osigs[q], in_=banks[bb][:, 0:192], func=AF.Sigmoid), reads=[bk(bb)], writes=[("m_osig", q)])

                def stage_h(t):
                    q = t % 2
                    hn_, os_, ot_ = hns[q], osigs[q], otoks[q]
                    P.add("dve", lambda e, t=t, hn_=hn_: e.scalar_tensor_tensor(out=hn_, in0=hacc[:, t, :], scalar=mvA[:, t, 0:1], in1=ng, op0=ALU.subtract, op1=ALU.mult),
                          reads=[("m_hacc", t), "mv", "m_ng"], writes=[("m_hn", q)])
                    P.add("dve", lambda e, t=t, hn_=hn_, os_=os_, ot_=ot_: e.scalar_tensor_tensor(out=ot_[:, 0:128], in0=hn_[:, 0:128], scalar=mvA[:, t, 2:3], in1=os_[:, 0:128], op0=ALU.mult, op1=ALU.mult),
                          reads=[("m_hn", q), ("m_osig", q), "mv"], writes=[("m_otok", q)])
                    ro = 128 + 64 * (h % 2)
                    P.add("dve", lambda e, t=t, hn_=hn_, os_=os_, ot_=ot_, ro=ro: e.scalar_tensor_tensor(out=ot_[:, ro:ro + 64], in0=hn_[:, 128:192], scalar=mvA[:, t, 2:3], in1=os_[:, 128:192], op0=ALU.mult, op1=ALU.mult),
                          reads=[("m_hn", q), ("m_osig", q), "mv"], writes=[("m_otok", q)])
                    tb = rot.next()
                    pv = banks[tb][:].bitcast(BF16)

                    def ft(e, pv=pv, ot_=ot_):
                        e.transpose(pv[:, 0:128], ot_[:, 0:128], ident[:])
                        return e.transpose(pv[:, 128:256], ot_[:, 128:256], ident[:])
                    P.add("pe", ft, reads=[("m_otok", q), "ident"], writes=[bk(tb)])
                    P.add("act", lambda e, pv=pv, t=t, h=h: e.activation(out=mixT[:, h, t * 128:(t + 1) * 128], in_=pv[:, 0:128], func=AF.Copy), reads=[bk(tb)], writes=[("mixT", h, t)])
                    p0 = 64 * (h % 2)
                    P.add("act", lambda e, pv=pv, t=t, h=h, p0=p0: e.activation(out=mixT[p0:p0 + 64, 4 + h // 2, t * 128:(t + 1) * 128], in_=pv[p0:p0 + 64, 128:256], func=AF.Copy),
                          reads=[bk(tb)], writes=[("mixT", 4 + h // 2, t, h % 2)])
                stage_o(0)
                for t in range(NT):
                    if t + 1 < NT:
                        stage_o(t + 1)
                    stage_h(t)
                barrier(["m_hn", "m_osig", "m_otok"], ["lnp"])
            barrier(MK, ATT_KEYS + ACC_KEYS)

            def rowmap(fc):
                if fc >= 6:
                    return [(0, fc * 128, 128)]
                if fc < 4:
                    return [(0, fc * 192, 128)]
                h0 = (fc - 4) * 2
                return [(0, h0 * 192 + 128, 64), (64, (h0 + 1) * 192 + 128, 64)]
            out_proj_ln(li, W["mlstm_w_out"][0], 8, rowmap)


        mixers = [layer_s5, layer_dil, layer_win, layer_mlstm]

        if 0 in layers:
            s5_setup()
            dbg("s5MI", S5["MI"], ["s5dram"])
            dbg("s5MINP", S5["MINP"], ["s5dram"])
            dbg("s5MOUT", S5["MOUT"], ["s5dram"])
            dbg("s5MU", S5["MU"], ["s5dram"])
        for s in range(nseq if "setup_only" not in debug else 0):
            for t4 in range(4):
                dma("sp", x_tm[:, 4 * t4:4 * t4 + 4, :], x_d[s, 512 * t4:512 * (t4 + 1), :].rearrange("(t p) d -> p t d", p=128),
                    [], [("x_tm", 4 * t4 + i) for i in range(4)])
            for j in range(2):
                dma("pool", xb[0][:], mem_d[s, j * 128:(j + 1) * 128, :], [], ["xb"])
                for hc in range(2):
                    transposes_to(xb[0][:, 512 * hc:512 * hc + 512], 4, lambda j=j, hc=hc: memT[:, 4 * hc:4 * hc + 4, j * 128:(j + 1) * 128], ("xb", hc), ("memT", j, hc))
            for t in range(NT):
                make_xT_tile(t)
            dbg("xT", xT[:], ["xT"])
            dbg("memT", memT[:], ["memT"])
            for li in layers:
                mixers[li % 4](li)
                dbg("mixT", mixT[:], ["mixT"])
                if do_moe:
                    moe(li)
            for t4 in range(4):
                dma("sp", out_d[s, 512 * t4:512 * (t4 + 1), :].rearrange("(t p) d -> p t d", p=128), x_tm[:, 4 * t4:4 * t4 + 4, :],
                    [("x_tm", 4 * t4 + i) for i in range(4)], [("out", s, t4)])
        P.add("sp", lambda e: None, reads=["out", "dbgout"])
        P.emit()
        print("ops per engine:", P.stats)
    return nc


_NC_CACHE = {}


def kernel(**inputs):
    nseq = 32 // NCORES
    if "nc" not in _NC_CACHE:
        _NC_CACHE["nc"] = build(nseq)
    nc = _NC_CACHE["nc"]
    consts = _consts()
    x = np.ascontiguousarray(inputs["x"], dtype=np.float32)
    mem = np.ascontiguousarray(inputs["mem"], dtype=np.float32)
    shared = {k: np.ascontiguousarray(inputs[k], dtype=np.float32) for k in WEIGHT_SHAPES}
    shared.update(consts)
    in_maps = []
    for c in range(NCORES):
        m = dict(shared)
        m["x"] = x[c * nseq:(c + 1) * nseq]
        m["mem"] = mem[c * nseq:(c + 1) * nseq]
        in_maps.append(m)
    res = run_bass_kernel_spmd(nc, in_maps, core_ids=list(range(NCORES)))
    return np.concatenate([r["out"] for r in res.results], axis=0)
```
